# Optimizing a Trainium2 kernel written in Bass

```python
import math
import jax, jax.numpy as jnp
from jax import lax
import numpy as np

D_MODEL = 2048
BATCH = 4
SEQ = 2048
DEPTH = 4

MEM_LEN = 256
EPS = 1e-5

SG_CHUNK = 128
N_SG = 4
D_SG = 512
SG_GROUP = D_SG // N_SG

N_MLA = 4
MLA_Q_RANK = 384
MLA_KV_RANK = 256
MLA_NOPE = 128
MLA_ROPE = 64
MLA_V = 128
ROPE_BASE = 10000.0
Q_BLOCK = 128

N_GLA = 4
GLA_DK = 64
GLA_DV = 128
GLA_GATE_RANK = 16
GLA_TAU = 16.0
GLA_CHUNK = 64
GLA_QK = N_GLA * GLA_DK
GLA_VW = N_GLA * GLA_DV

N_ML = 4
ML_DK = 64
ML_DV = 128
ML_CONV = 4
ML_CHUNK = 64
ML_QK = N_ML * ML_DK
ML_VW = N_ML * ML_DV

N_BRANCH = 4
BRANCH_W = 512

N_X = 4
X_HEAD = 128

N_EXPERTS = 16
N_GROUPS = 4
TOP_K = 2
D_EXPERT = 512

SG_IN = 2 * D_SG
MLA_IN = MLA_Q_RANK + MLA_KV_RANK + MLA_ROPE
GLA_IN = 2 * GLA_QK + 2 * GLA_VW + GLA_GATE_RANK
ML_IN = 2 * ML_QK + 2 * ML_VW + 2 * N_ML
GATE_IN = N_BRANCH * D_MODEL
IN_SPLITS = (SG_IN, MLA_IN, GLA_IN, ML_IN, GATE_IN)
D_IN = SG_IN + MLA_IN + GLA_IN + ML_IN + GATE_IN

kernel_name = "hybrid_gated_mixer_deepnorm_moe"


def _split(x, sizes):
    out, off = [], 0
    for s in sizes:
        out.append(x[..., off:off + s])
        off += s
    return out


def layer_norm(x, g, b):
    xf = x.astype(jnp.float32)
    mu = jnp.mean(xf, -1, keepdims=True)
    var = jnp.mean(jnp.square(xf - mu), -1, keepdims=True)
    y = (xf - mu) * lax.rsqrt(var + EPS) * g.astype(jnp.float32) + b.astype(jnp.float32)
    return y.astype(x.dtype)


def rms_norm(x, g):
    xf = x.astype(jnp.float32)
    y = xf * lax.rsqrt(jnp.mean(xf * xf, -1, keepdims=True) + EPS) * g.astype(jnp.float32)
    return y.astype(x.dtype)


def rope(x, pos):
    half = x.shape[-1] // 2
    freqs = ROPE_BASE ** (-jnp.arange(half, dtype=jnp.float32) / half)
    ang = pos.astype(jnp.float32)[..., None] * freqs
    cos, sin = jnp.cos(ang)[:, :, None, :], jnp.sin(ang)[:, :, None, :]
    xf = x.astype(jnp.float32)
    x1, x2 = xf[..., :half], xf[..., half:]
    return jnp.concatenate([x1 * cos - x2 * sin, x1 * sin + x2 * cos], -1).astype(x.dtype)


def spatial_gating(z, v_g, v_b, w_s, b_s):
    B, S, _ = z.shape
    u, v = z[..., :D_SG], z[..., D_SG:]
    v = layer_norm(v, v_g, v_b).reshape(B, S // SG_CHUNK, SG_CHUNK, N_SG, SG_GROUP)
    causal = jnp.tril(jnp.ones((SG_CHUNK, SG_CHUNK), dtype=bool))
    w = jnp.where(causal, w_s, 0.0).astype(v.dtype)
    mixed = jnp.einsum('gts,bcsgd->bctgd', w, v) + b_s.T[:, :, None]
    return u * mixed.reshape(B, S, D_SG)


def causal_block_attention(q, k, v, scale):
    B, S, H, Dq = q.shape
    nb = S // Q_BLOCK
    qb = q.reshape(B, nb, Q_BLOCK, H, Dq).transpose(1, 0, 2, 3, 4)
    k_idx = jnp.arange(S)

    def one_block(args):
        q_blk, i = args
        s = jnp.einsum('bthd,bshd->bhts', q_blk, k).astype(jnp.float32) * scale
        q_idx = i * Q_BLOCK + jnp.arange(Q_BLOCK)
        s = jnp.where(k_idx[None, :] <= q_idx[:, None], s, -jnp.inf)
        p = jax.nn.softmax(s, axis=-1).astype(v.dtype)
        return jnp.einsum('bhts,bshd->bthd', p, v)

    out = lax.map(one_block, (qb, jnp.arange(nb)))
    return out.transpose(1, 0, 2, 3, 4).reshape(B, S, H * v.shape[-1])


def mla_attention(c_q, c_kv, k_rope, pos, q_g, kv_g, w_uq, w_ukv):
    B, S, _ = c_q.shape
    q = (rms_norm(c_q, q_g) @ w_uq).reshape(B, S, N_MLA, MLA_NOPE + MLA_ROPE)
    kv = (rms_norm(c_kv, kv_g) @ w_ukv).reshape(B, S, N_MLA, MLA_NOPE + MLA_V)
    q = jnp.concatenate([q[..., :MLA_NOPE], rope(q[..., MLA_NOPE:], pos)], -1)
    k_r = jnp.broadcast_to(rope(k_rope[:, :, None, :], pos), (B, S, N_MLA, MLA_ROPE))
    k = jnp.concatenate([kv[..., :MLA_NOPE], k_r], -1)
    v = kv[..., MLA_NOPE:]
    return causal_block_attention(q, k, v, (MLA_NOPE + MLA_ROPE) ** -0.5)


def gla(q, k, v, o_gate, gate_lr, w_gate, b_gate, norm_g):
    B, S, _ = q.shape
    L, nc = GLA_CHUNK, S // GLA_CHUNK
    log_a = jax.nn.log_sigmoid((gate_lr @ w_gate + b_gate).astype(jnp.float32)) / GLA_TAU

    def chunks(t, d):
        return t.reshape(B, nc, L, N_GLA, d).transpose(1, 0, 3, 2, 4).astype(jnp.float32)

    qc = chunks(q, GLA_DK) * (GLA_DK ** -0.5)
    kc, vc, ac = chunks(k, GLA_DK), chunks(v, GLA_DV), chunks(log_a, GLA_DK)
    causal = jnp.tril(jnp.ones((L, L), dtype=bool))

    def step(state, inp):
        qi, ki, vi, ai = inp
        b = jnp.cumsum(ai, axis=2)
        b_last = b[:, :, -1:, :]
        q_t = qi * jnp.exp(b)
        att = jnp.where(causal, jnp.einsum('bhtd,bhsd->bhts', q_t, ki * jnp.exp(-b)), 0.0)
        o = jnp.einsum('bhts,bhsv->bhtv', att, vi) + jnp.einsum('bhtd,bhdv->bhtv', q_t, state)
        state = state * jnp.exp(b_last[:, :, 0, :])[..., None] + \
            jnp.einsum('bhsd,bhsv->bhdv', ki * jnp.exp(b_last - b), vi)
        return state, o

    init = jnp.zeros((B, N_GLA, GLA_DK, GLA_DV), jnp.float32)
    _, o = lax.scan(step, init, (qc, kc, vc, ac))
    o = rms_norm(o.transpose(1, 0, 3, 2, 4).reshape(B, S, N_GLA, GLA_DV), norm_g)
    return (o.reshape(B, S, GLA_VW) * jax.nn.silu(o_gate.astype(jnp.float32))).astype(v.dtype)


def causal_depthwise_conv(x, w, b):
    K, C = w.shape
    y = lax.conv_general_dilated(x, w[:, None, :], window_strides=(1,), padding=[(K - 1, 0)],
                                 dimension_numbers=('NWC', 'WIO', 'NWC'), feature_group_count=C)
    return y + b


def mlstm(q, k, v, o_pre, i_pre, f_pre, norm_g):
    B, S, _ = q.shape
    L, nc = ML_CHUNK, S // ML_CHUNK

    def chunks(t, d):
        return t.reshape(B, nc, L, N_ML, d).transpose(1, 0, 3, 2, 4).astype(jnp.float32)

    def gchunks(t):
        return t.reshape(B, nc, L, N_ML).transpose(1, 0, 3, 2).astype(jnp.float32)

    qc = chunks(q, ML_DK) * (ML_DK ** -0.5)
    kc, vc = chunks(k, ML_DK), chunks(v, ML_DV)
    fc = gchunks(jax.nn.log_sigmoid(f_pre.astype(jnp.float32)))
    ic = gchunks(i_pre)
    causal = jnp.tril(jnp.ones((L, L), dtype=bool))

    def step(carry, inp):
        C, n, m = carry
        qi, ki, vi, fi, ii = inp
        b = jnp.cumsum(fi, axis=-1)
        log_inter = b + m[..., None]
        log_d = jnp.where(causal, b[..., :, None] - b[..., None, :] + ii[..., None, :], -jnp.inf)
        m_t = jnp.maximum(log_inter, jnp.max(log_d, -1))
        w_inter = jnp.exp(log_inter - m_t)
        s = jnp.einsum('bhtd,bhsd->bhts', qi, ki) * jnp.exp(log_d - m_t[..., None])
        num = jnp.einsum('bhts,bhsv->bhtv', s, vi) + \
            w_inter[..., None] * jnp.einsum('bhtd,bhdv->bhtv', qi, C)
        den = jnp.sum(s, -1) + w_inter * jnp.einsum('bhtd,bhd->bht', qi, n)
        h = num / jnp.maximum(jnp.abs(den), jnp.exp(-m_t))[..., None]
        b_last = b[..., -1]
        log_w = b_last[..., None] - b + ii
        m_new = jnp.maximum(b_last + m, jnp.max(log_w, -1))
        decay = jnp.exp(b_last + m - m_new)
        w = jnp.exp(log_w - m_new[..., None])
        C = decay[..., None, None] * C + jnp.einsum('bhs,bhsd,bhsv->bhdv', w, ki, vi)
        n = decay[..., None] * n + jnp.einsum('bhs,bhsd->bhd', w, ki)
        return (C, n, m_new), h

    init = (jnp.zeros((B, N_ML, ML_DK, ML_DV), jnp.float32),
            jnp.zeros((B, N_ML, ML_DK), jnp.float32),
            jnp.zeros((B, N_ML), jnp.float32))
    _, h = lax.scan(step, init, (qc, kc, vc, fc, ic))
    h = rms_norm(h.transpose(1, 0, 3, 2, 4).reshape(B, S, N_ML, ML_DV), norm_g)
    return (h.reshape(B, S, ML_VW) * jax.nn.sigmoid(o_pre.astype(jnp.float32))).astype(v.dtype)


def hybrid_mixer(h, positions, w_in, sg_vnorm_g, sg_vnorm_b, sg_w_s, sg_b_s,
                 mla_qnorm_g, mla_kvnorm_g, mla_w_uq, mla_w_ukv,
                 gla_w_gate, gla_b_gate, gla_norm_g,
                 ml_conv_w, ml_conv_b, ml_gate_b, ml_norm_g, w_branch, w_out):
    B, S, D = h.shape
    z_sg, z_mla, z_gla, z_ml, z_gate = _split(h @ w_in, IN_SPLITS)
    y_a = spatial_gating(jax.nn.gelu(z_sg), sg_vnorm_g, sg_vnorm_b, sg_w_s, sg_b_s)
    c_q, c_kv, k_rope = _split(z_mla, (MLA_Q_RANK, MLA_KV_RANK, MLA_ROPE))
    y_b = mla_attention(c_q, c_kv, k_rope, positions, mla_qnorm_g, mla_kvnorm_g, mla_w_uq, mla_w_ukv)
    g_q, g_k, g_v, g_o, g_lr = _split(z_gla, (GLA_QK, GLA_QK, GLA_VW, GLA_VW, GLA_GATE_RANK))
    y_c = gla(g_q, g_k, g_v, g_o, g_lr, gla_w_gate, gla_b_gate, gla_norm_g)
    m_qk, m_v, m_o, m_if = _split(z_ml, (2 * ML_QK, ML_VW, ML_VW, 2 * N_ML))
    m_q, m_k = _split(jax.nn.silu(causal_depthwise_conv(m_qk, ml_conv_w, ml_conv_b)), (ML_QK, ML_QK))
    m_i, m_f = _split(m_if + ml_gate_b, (N_ML, N_ML))
    y_d = mlstm(m_q, m_k, m_v, m_o, m_i, m_f, ml_norm_g)
    branches = jnp.stack([y_a, y_b, y_c, y_d], axis=2)
    gates = jax.nn.sigmoid(z_gate).reshape(B, S, N_BRANCH, D)
    merged = jnp.einsum('bsnd,bsnd->bsd', gates, jnp.einsum('bsnw,nwd->bsnd', branches, w_branch))
    return merged @ w_out


def memory_cross_attention(h, mem, w_q, w_kv, w_o):
    B, S, _ = h.shape
    M = mem.shape[1]
    q = (h @ w_q).reshape(B, S, N_X, X_HEAD)
    kv = (mem @ w_kv).reshape(B, M, 2, N_X, X_HEAD)
    s = jnp.einsum('bthd,bshd->bhts', q, kv[:, :, 0]).astype(jnp.float32) * (X_HEAD ** -0.5)
    p = jax.nn.softmax(s, axis=-1).astype(h.dtype)
    o = jnp.einsum('bhts,bshd->bthd', p, kv[:, :, 1]).reshape(B, S, N_X * X_HEAD)
    return o @ w_o


def grouped_moe(h, w_router, router_bias, w_gate, w_up, w_down):
    B, S, D = h.shape
    t = h.reshape(B * S, D)
    aff = jax.nn.sigmoid((t @ w_router).astype(jnp.float32))
    per_group = N_EXPERTS // N_GROUPS
    biased = (aff + router_bias.astype(jnp.float32)).reshape(-1, N_GROUPS, per_group)
    group_score = lax.top_k(biased, TOP_K)[0].sum(-1)
    g_idx = jnp.argmax(group_score, axis=-1)
    in_group = jnp.take_along_axis(biased, g_idx[:, None, None], axis=1)[:, 0]
    _, local = lax.top_k(in_group, TOP_K)
    exp_idx = g_idx[:, None] * per_group + local
    sel = jnp.take_along_axis(aff, exp_idx, axis=1)
    wts = sel / jnp.sum(sel, -1, keepdims=True)
    gates = jnp.sum(jax.nn.one_hot(exp_idx, N_EXPERTS, dtype=jnp.float32) * wts[..., None], axis=1)
    hid = jax.nn.silu(jnp.einsum('nd,edf->nef', t, w_gate)) * jnp.einsum('nd,edf->nef', t, w_up)
    hid = hid * gates.astype(hid.dtype)[..., None]
    return jnp.einsum('nef,efd->nd', hid, w_down).reshape(B, S, D)


def setup_inputs(seed: int = 0) -> dict:
    key = jax.random.key(seed)
    ks = iter(jax.random.split(key, 48))
    f32 = jnp.float32

    def nrm(shape, scale):
        return jax.random.normal(next(ks), shape, f32) * scale

    Ld, D = DEPTH, D_MODEL
    beta = (8.0 * DEPTH) ** -0.25
    x = nrm((BATCH, SEQ, D), 1.0)
    mem = nrm((BATCH, MEM_LEN, D), 1.0)
    positions = jax.random.randint(next(ks), (BATCH, 1), 0, 4096, dtype=jnp.int32) + \
        jnp.arange(SEQ, dtype=jnp.int32)[None, :]
    return {
        "x": x,
        "mem": mem,
        "positions": positions,
        "ln_in_g": 1.0 + nrm((D,), 0.05),
        "ln_in_b": nrm((D,), 0.02),
        "w_in": nrm((Ld, D, D_IN), D ** -0.5),
        "sg_vnorm_g": 1.0 + nrm((Ld, D_SG), 0.05),
        "sg_vnorm_b": nrm((Ld, D_SG), 0.02),
        "sg_w_s": nrm((Ld, N_SG, SG_CHUNK, SG_CHUNK), 0.5 * SG_CHUNK ** -0.5),
        "sg_b_s": 1.0 + nrm((Ld, N_SG, SG_CHUNK), 0.1),
        "mla_qnorm_g": 1.0 + nrm((Ld, MLA_Q_RANK), 0.05),
        "mla_kvnorm_g": 1.0 + nrm((Ld, MLA_KV_RANK), 0.05),
        "mla_w_uq": nrm((Ld, MLA_Q_RANK, N_MLA * (MLA_NOPE + MLA_ROPE)), MLA_Q_RANK ** -0.5),
        "mla_w_ukv": nrm((Ld, MLA_KV_RANK, N_MLA * (MLA_NOPE + MLA_V)), MLA_KV_RANK ** -0.5),
        "gla_w_gate": nrm((Ld, GLA_GATE_RANK, GLA_QK), GLA_GATE_RANK ** -0.5),
        "gla_b_gate": nrm((Ld, GLA_QK), 0.1),
        "gla_norm_g": 1.0 + nrm((Ld, GLA_DV), 0.05),
        "ml_conv_w": nrm((Ld, ML_CONV, 2 * ML_QK), ML_CONV ** -0.5),
        "ml_conv_b": nrm((Ld, 2 * ML_QK), 0.02),
        "ml_gate_b": jnp.concatenate(
            [nrm((Ld, N_ML), 0.1),
             jnp.broadcast_to(jnp.linspace(3.0, 6.0, N_ML, dtype=f32), (Ld, N_ML)) + nrm((Ld, N_ML), 0.1)],
            axis=-1),
        "ml_norm_g": 1.0 + nrm((Ld, ML_DV), 0.05),
        "w_branch": nrm((Ld, N_BRANCH, BRANCH_W, D), beta * BRANCH_W ** -0.5),
        "w_out": nrm((Ld, D, D), beta * D ** -0.5),
        "ln1_g": 1.0 + nrm((Ld, D), 0.05),
        "ln1_b": nrm((Ld, D), 0.02),
        "x_w_q": nrm((Ld, D, N_X * X_HEAD), D ** -0.5),
        "x_w_kv": nrm((Ld, D, 2 * N_X * X_HEAD), D ** -0.5),
        "x_w_o": nrm((Ld, N_X * X_HEAD, D), beta * (N_X * X_HEAD) ** -0.5),
        "ln2_g": 1.0 + nrm((Ld, D), 0.05),
        "ln2_b": nrm((Ld, D), 0.02),
        "w_router": nrm((D, N_EXPERTS), D ** -0.5),
        "router_bias": nrm((N_EXPERTS,), 0.01),
        "moe_w_gate": nrm((Ld, N_EXPERTS, D, D_EXPERT), D ** -0.5),
        "moe_w_up": nrm((Ld, N_EXPERTS, D, D_EXPERT), D ** -0.5),
        "moe_w_down": nrm((Ld, N_EXPERTS, D_EXPERT, D), beta * D_EXPERT ** -0.5),
        "ln3_g": 1.0 + nrm((Ld, D), 0.05),
        "ln3_b": nrm((Ld, D), 0.02),
    }


def reference(x, mem, positions, ln_in_g, ln_in_b, w_in, sg_vnorm_g, sg_vnorm_b, sg_w_s, sg_b_s,
              mla_qnorm_g, mla_kvnorm_g, mla_w_uq, mla_w_ukv, gla_w_gate, gla_b_gate, gla_norm_g,
              ml_conv_w, ml_conv_b, ml_gate_b, ml_norm_g, w_branch, w_out, ln1_g, ln1_b,
              x_w_q, x_w_kv, x_w_o, ln2_g, ln2_b, w_router, router_bias,
              moe_w_gate, moe_w_up, moe_w_down, ln3_g, ln3_b):
    alpha = (2.0 * DEPTH) ** 0.25
    h = layer_norm(x, ln_in_g, ln_in_b)
    for l in range(DEPTH):
        y = hybrid_mixer(h, positions, w_in[l], sg_vnorm_g[l], sg_vnorm_b[l], sg_w_s[l], sg_b_s[l],
                         mla_qnorm_g[l], mla_kvnorm_g[l], mla_w_uq[l], mla_w_ukv[l],
                         gla_w_gate[l], gla_b_gate[l], gla_norm_g[l],
                         ml_conv_w[l], ml_conv_b[l], ml_gate_b[l], ml_norm_g[l], w_branch[l], w_out[l])
        h = layer_norm(alpha * h + y, ln1_g[l], ln1_b[l])
        y = memory_cross_attention(h, mem, x_w_q[l], x_w_kv[l], x_w_o[l])
        h = layer_norm(alpha * h + y, ln2_g[l], ln2_b[l])
        y = grouped_moe(h, w_router, router_bias, moe_w_gate[l], moe_w_up[l], moe_w_down[l])
        h = layer_norm(alpha * h + y, ln3_g[l], ln3_b[l])
    return h
```

```python
import math
from concourse.bass_utils import run_bass_kernel_spmd
import numpy as np
import concourse.bass as bass
import concourse.mybir as mybir

F32 = mybir.dt.float32
BF16 = mybir.dt.bfloat16
I32 = mybir.dt.int32
AF = mybir.ActivationFunctionType
ALU = mybir.AluOpType
AX = mybir.AxisListType


def _region(ap):
    t = ap.tensor
    esz = mybir.dt.size(ap.dtype)
    pat = ap.ap
    off = ap.offset
    space = str(ap.space)
    if space == "DRAM" or "DRAM" in space.upper() or "HBM" in space.upper():
        lo = off
        hi = off + sum((c - 1) * abs(s) for s, c in pat) + 1
        return (t.name, 0, 1, lo * esz, hi * esz)
    pstride, pcount = pat[0]
    if pstride == 0:
        pstride = 1 << 40
    p0 = off // pstride if pstride < (1 << 40) else 0
    lo = off - p0 * pstride if pstride < (1 << 40) else off
    hi = lo + sum((c - 1) * abs(s) for s, c in pat[1:]) + 1
    if space == "PSUM":
        return (t.name, 0, 128, (lo * esz) // 2048 * 2048, ((hi * esz) + 2047) // 2048 * 2048)
    return (t.name, p0, p0 + pcount, lo * esz, hi * esz)


class _Op:
    __slots__ = ("e", "fn", "r", "w", "dma", "deps", "need", "tok", "slot")

    def __init__(self, e, fn, r, w, dma):
        self.e, self.fn, self.r, self.w, self.dma = e, fn, r, w, dma
        self.deps = None
        self.need = False
        self.tok = None
        self.slot = None


class Sched:
    GEN = 20000

    def __init__(self, nc, n_dma_slots=6):
        self.nc = nc
        self.engs = {"pe": nc.tensor, "act": nc.scalar, "dve": nc.vector,
                     "pool": nc.gpsimd, "sp": nc.sync}
        self.ops = []
        self.n_dma_slots = n_dma_slots

    def add(self, e, fn, reads=(), writes=(), dma=False):
        r = [_region(a) for a in reads if a is not None and hasattr(a, "ap")]
        w = [_region(a) for a in writes if a is not None and hasattr(a, "ap")]
        self.ops.append(_Op(e, fn, r, w, dma))

    def mm(self, out, lhsT, rhs, start=True, stop=True, **kw):
        self.add("pe", lambda g: g.matmul(out, lhsT, rhs, start=start, stop=stop, **kw),
                 [lhsT, rhs], [out])

    def tr(self, out, in_, ident):
        self.add("pe", lambda g: g.transpose(out, in_, ident), [in_, ident], [out])

    def act(self, out, in_, func, bias=None, scale=None, accum_out=None, e="act"):
        kw = {}
        if bias is not None:
            kw["bias"] = bias
        if scale is not None:
            kw["scale"] = scale
        if accum_out is not None:
            kw["accum_out"] = accum_out
        self.add(e, lambda g: g.activation(out, in_, func, **kw),
                 [in_, bias, scale], [out, accum_out])

    def tt(self, out, in0, in1, op, e="dve"):
        self.add(e, lambda g: g.tensor_tensor(out, in0, in1, op), [in0, in1], [out])

    def ts(self, out, in0, s1, s2=None, op0=ALU.mult, op1=None, e="dve", accum_out=None):
        kw = {}
        if op1 is not None:
            kw["op1"] = op1
        if accum_out is not None:
            kw["accum_out"] = accum_out
        self.add(e, lambda g: g.tensor_scalar(out, in0, s1, s2, op0, **kw),
                 [in0, s1, s2], [out, accum_out])

    def stt(self, out, in0, scalar, in1, op0, op1, e="dve"):
        self.add(e, lambda g: g.scalar_tensor_tensor(out, in0, scalar, in1, op0, op1),
                 [in0, scalar, in1], [out])

    def copy(self, out, in_, e="dve"):
        if e == "act":
            self.add(e, lambda g: g.copy(out, in_), [in_], [out])
        else:
            self.add(e, lambda g: g.tensor_copy(out, in_), [in_], [out])

    def memset(self, out, val, e="pool"):
        self.add(e, lambda g: g.memset(out, val), [], [out])

    def dma(self, out, in_, e="sp", **kw):
        self.add(e, lambda g: g.dma_start(out=out, in_=in_, **kw), [in_], [out], dma=True)

    def finish(self):
        nc = self.nc
        ops = self.ops
        recs = {}
        for i, op in enumerate(ops):
            deps = set()
            for (name, p0, p1, lo, hi) in op.r:
                for rc in recs.get(name, ()):
                    if rc[5] and rc[0] < p1 and p0 < rc[1] and rc[2] < hi and lo < rc[3]:
                        deps.add(rc[4])
            for (name, p0, p1, lo, hi) in op.w:
                for rc in recs.get(name, ()):
                    if rc[0] < p1 and p0 < rc[1] and rc[2] < hi and lo < rc[3]:
                        deps.add(rc[4])
            deps.discard(i)
            if op.e == "pe":
                deps = {d for d in deps if ops[d].e != "pe" or ops[d].dma}
            op.deps = deps
            for d in deps:
                ops[d].need = True
            for (name, p0, p1, lo, hi) in op.w:
                L = recs.setdefault(name, [])
                L[:] = [rc for rc in L if not (p0 <= rc[0] and rc[1] <= p1 and lo <= rc[2] and rc[3] <= hi)]
                L.append([p0, p1, lo, hi, i, True, op.e])
            for (name, p0, p1, lo, hi) in op.r:
                L = recs.setdefault(name, [])
                if not op.dma:
                    L[:] = [rc for rc in L if not ((not rc[5]) and rc[6] == op.e and not ops[rc[4]].dma
                                                   and p0 <= rc[0] and rc[1] <= p1 and lo <= rc[2] and rc[3] <= hi)]
                L.append([p0, p1, lo, hi, i, False, op.e])
        ticks = {e: 0 for e in self.engs}
        ndma = {e: 0 for e in self.engs}
        ncc = 0
        for op in ops:
            if op.dma == "cc":
                op.tok = ("k", "cc", ncc, 1)
                ncc += 1
            elif op.dma:
                k = ndma[op.e]
                ndma[op.e] += 1
                op.slot = (op.e, k % self.n_dma_slots)
                op.tok = ("d", op.e, k % self.n_dma_slots, 16 * (k // self.n_dma_slots + 1))
            elif op.need:
                ticks[op.e] += 1
                t = ticks[op.e]
                op.tok = ("c", op.e, (t - 1) // self.GEN, (t - 1) % self.GEN + 1)
        self.sems = {}
        import contextlib
        self._stack = contextlib.ExitStack()

        def sem(key):
            if key not in self.sems:
                self.sems[key] = self._stack.enter_context(nc.semaphore("s_%s_%s_%s" % key))
            return self.sems[key]

        waited = {e: {} for e in self.engs}
        last_on_slot = {}
        self.n_waits = 0
        for op in ops:
            g = self.engs[op.e]
            need = {}
            for d in op.deps:
                tk = ops[d].tok
                key = tk[:3]
                if need.get(key, 0) < tk[3]:
                    need[key] = tk[3]
            if op.dma and op.dma != "cc":
                prev = last_on_slot.get(op.slot)
                if prev is not None:
                    key = prev[:3]
                    if need.get(key, 0) < prev[3]:
                        need[key] = prev[3]
            for key, v in need.items():
                if waited[op.e].get(key, 0) < v:
                    g.wait_ge(sem(key), v)
                    waited[op.e][key] = v
                    self.n_waits += 1
            ins = op.fn(g)
            if op.dma == "cc":
                ins.then_inc(sem(op.tok[:3]), 1)
            elif op.dma:
                ins.then_inc(sem(op.tok[:3]), 16)
                last_on_slot[op.slot] = op.tok
            elif op.tok is not None:
                ins.then_inc(sem(op.tok[:3]), 1)
        self.ticks = ticks
        return self

    def wait_all_dma_on(self, e="sp"):
        raise NotImplementedError


def _finish_tail(self, e="sp"):
    g = self.engs[e]
    seen = {}
    for op in self.ops:
        if op.dma:
            seen[op.tok[:3]] = max(seen.get(op.tok[:3], 0), op.tok[3])
    for key, v in seen.items():
        g.wait_ge(self.sems[key], v)


Sched.final_wait = _finish_tail


def _recip(self, out, in_, e="dve"):
    self.add(e, lambda g: g.reciprocal(out, in_), [in_], [out])


def _scan(self, out, d0, d1, init, op0, op1):
    self.add("dve", lambda g: g.tensor_tensor_scan(out, d0, d1, init, op0, op1), [d0, d1, init], [out])


def _bnstats(self, out, in_):
    self.add("dve", lambda g: g.bn_stats(out, in_), [in_], [out])


def _bnaggr(self, out, in_):
    self.add("dve", lambda g: g.bn_aggr(out, in_), [in_], [out])


def _cc(self, out, in_, groups):
    self.add("pool", lambda g: g.collective_compute("AllGather", ALU.bypass, replica_groups=groups,
                                                    ins=[in_], outs=[out]), [in_], [out], dma="cc")


Sched.recip = _recip
Sched.scan = _scan
Sched.bnstats = _bnstats
Sched.bnaggr = _bnaggr
Sched.cc = _cc


class Arena:
    def __init__(self, nc, stack, nbytes):
        self.nb = nbytes
        self.t = stack.enter_context(nc.sbuf_tensor("arena", [128, nbytes // 2], BF16))
        self.free_list = [(0, nbytes)]
        self.live = {}

    def alloc(self, name, shape, dt, parts=None):
        P = shape[0]
        n = 1
        for s in shape[1:]:
            n *= s
        nbytes = n * mybir.dt.size(dt)
        nbytes = (nbytes + 63) // 64 * 64
        for i, (o, sz) in enumerate(self.free_list):
            if sz >= nbytes:
                if sz == nbytes:
                    self.free_list.pop(i)
                else:
                    self.free_list[i] = (o + nbytes, sz - nbytes)
                self.live[name] = (o, nbytes)
                ap = self.t[0:P, o // 2:(o + n * mybir.dt.size(dt)) // 2]
                if dt != BF16:
                    ap = ap.bitcast(dt)
                if len(shape) == 3:
                    ap = ap.rearrange("p (a b) -> p a b", a=shape[1])
                elif len(shape) == 4:
                    ap = ap.rearrange("p (a b c) -> p a b c", a=shape[1], b=shape[2])
                return ap
        raise RuntimeError("arena full: %s %d free=%s" % (name, nbytes, self.free_list))

    def free(self, *names):
        for name in names:
            o, sz = self.live.pop(name)
            self.free_list.append((o, sz))
        self.free_list.sort()
        m = []
        for o, sz in self.free_list:
            if m and m[-1][0] + m[-1][1] == o:
                m[-1] = (m[-1][0], m[-1][1] + sz)
            else:
                m.append((o, sz))
        self.free_list = m


T = 1024
DEPTH = 4
D = 2048
ALPHA = (2.0 * DEPTH) ** 0.25
EPS = 1e-5
PAIRS = [[0, 1], [2, 3], [4, 5], [6, 7]]
NCST = 128 * 3 + 1024 + 256 + 1
ARENA_BYTES = 202 * 1024

PARAM_SHAPES = dict(
    ln_in_g=[2048], ln_in_b=[2048], w_in=[4, 2048, 13016], sg_vnorm_g=[4, 512], sg_vnorm_b=[4, 512],
    sg_w_s=[4, 4, 128, 128], sg_b_s=[4, 4, 128], mla_qnorm_g=[4, 384], mla_kvnorm_g=[4, 256],
    mla_w_uq=[4, 384, 768], mla_w_ukv=[4, 256, 1024], gla_w_gate=[4, 16, 256], gla_b_gate=[4, 256],
    gla_norm_g=[4, 128], ml_conv_w=[4, 4, 512], ml_conv_b=[4, 512], ml_gate_b=[4, 8], ml_norm_g=[4, 128],
    w_branch=[4, 4, 512, 2048], w_out=[4, 2048, 2048], ln1_g=[4, 2048], ln1_b=[4, 2048],
    x_w_q=[4, 2048, 512], x_w_kv=[4, 2048, 1024], x_w_o=[4, 512, 2048], ln2_g=[4, 2048], ln2_b=[4, 2048],
    w_router=[2048, 16], router_bias=[16], moe_w_gate=[4, 16, 2048, 512], moe_w_up=[4, 16, 2048, 512],
    moe_w_down=[4, 16, 512, 2048], ln3_g=[4, 2048], ln3_b=[4, 2048])


LAYERED = [k for k, v in PARAM_SHAPES.items() if v[0] == 4 and len(v) >= 2 and k not in ('w_router',)]


def make_consts():
    c = np.zeros((128, NCST), np.float32)
    p = np.arange(128)[:, None]
    f = np.arange(128)[None, :]
    c[:, 0:128] = (p == f)
    c[:, 128:256] = (p <= f)
    c[:, 256:384] = (p <= f) & ((p // 64) == (f // 64))
    t = np.arange(1024)
    c[:, 384:1408] = (t % 64 != 0)[None, :]
    for k in range(4):
        for fc in range(2):
            for m in range(128):
                c[k, 1408 + fc * 128 + m] = 1.0 if k == 2 * fc + m // 64 else 0.0
    fr = (np.float32(10000.0) ** (-np.arange(32, dtype=np.float32) / np.float32(32))).astype(np.float32)
    c[0:32, 1664] = fr
    c[32:64, 1664] = fr
    return c


def build(depth=DEPTH, stop_after=None, dumps=()):
    import contextlib
    nc = bass.Bass("TRN2", target_bir_lowering=False)

    def din(name, shape, dt=F32):
        return nc.dram_tensor(name, list(shape), dt, kind="ExternalInput").ap()

    x_d = din("x", [T, D])
    mem_d = din("mem", [256, D])
    pos_d = din("pos", [1, T], I32)
    pf_d = din("pflag", [128, 1])
    cst_d = din("cst", [128, NCST])
    class _W(dict):
        def __missing__(self, k):
            shp = list(PARAM_SHAPES[k])
            if k in LAYERED:
                shp[0] = depth
            self[k] = din(k, shp)
            return self[k]
    W = _W()
    out_d = nc.dram_tensor("out", [T, D], F32, kind="ExternalOutput").ap()
    dump_d = {}

    def internal(name, shape, dt=F32):
        return nc.dram_tensor(name, list(shape), dt, kind="Internal").ap()

    cin_tail, cout_tail = internal("cin_tail", [128, 16]), internal("cout_tail", [256, 16])
    cin_kv, cout_kv = internal("cin_kv", [320, T], BF16), internal("cout_kv", [640, T], BF16)
    cin_g, cout_g = internal("cin_g", [256, 128]), internal("cout_g", [512, 128])
    cin_m, cout_m = internal("cin_m", [256, 256]), internal("cout_m", [512, 256])

    S = Sched(nc)
    st = contextlib.ExitStack()
    A = Arena(nc, st, ARENA_BYTES)
    pst = st.enter_context(nc.psum_tensor("ps", [128, 8, 512], F32))
    psn = [0]

    def PS():
        b = psn[0] % 4
        psn[0] += 1
        return pst[:, b, :]

    pxn = [0]

    def PSX():
        b = 4 + pxn[0] % 4
        pxn[0] += 1
        return pst[:, b, :]

    def dump(name, ap):
        if name in dumps:
            dd = nc.dram_tensor("dbg_" + name, list(ap.shape), ap.dtype, kind="ExternalOutput").ap()
            dump_d[name] = dd
            S.dma(dd, ap)

    def wload(dst, src):
        S.dma(dst, src, e="pool")

    HS = [slice(0, 512), slice(512, 1024)]

    hT = A.alloc("hT", [128, 16, T], F32)
    hb = A.alloc("hb", [128, 16, T], BF16)
    ident_f = A.alloc("ident_f", [128, 128], F32)
    ident_b = A.alloc("ident_b", [128, 128], BF16)
    ones_b = A.alloc("ones_b", [128, 128], BF16)
    tri_b = A.alloc("tri_b", [128, 128], BF16)
    bd_b = A.alloc("bd_b", [128, 128], BF16)
    rmask = A.alloc("rmask", [128, T], BF16)
    sel_f = A.alloc("sel_f", [4, 2, 128], F32)
    small = A.alloc("small", [128, 8], F32)
    pf, nb, epsc, freq, onec = (small[:, i:i + 1] for i in range(5))
    lnp = A.alloc("lnp", [128, 512], F32)
    cosT = A.alloc("cosT", [64, T], BF16)
    sinT = A.alloc("sinT", [64, T], BF16)

    cst = A.alloc("cst", [128, NCST], F32)
    S.dma(cst, cst_d)
    S.dma(pf, pf_d)
    S.copy(ident_f, cst[:, 0:128])
    S.copy(ident_b, cst[:, 0:128])
    S.copy(tri_b, cst[:, 128:256])
    S.copy(bd_b, cst[:, 256:384])
    S.copy(rmask, cst[:, 384:1408])
    S.copy(sel_f, cst[0:4, 1408:1664].rearrange("p (a b) -> p a b", a=2))
    S.copy(freq, cst[:, 1664:1665])
    S.memset(ones_b, 1.0, e="dve")
    S.memset(epsc, EPS, e="dve")
    S.memset(onec, 1.0, e="dve")
    S.ts(nb, pf, 30000.0, -30000.0, op0=ALU.mult, op1=ALU.add)

    posi = A.alloc("posi", [64, T], I32)
    S.dma(posi, pos_d.partition_broadcast(64) if False else pos_d[0:1, :].to_broadcast([64, T]))
    ang = A.alloc("ang", [64, T], F32)
    S.copy(ang, posi)
    S.ts(ang, ang, freq[0:64, :], 1.0 / (2.0 * math.pi), op0=ALU.mult, op1=ALU.mult)
    ki = A.alloc("ki", [64, T], I32)
    kf = A.alloc("kf", [64, T], F32)
    for tab, shift in ((sinT, 0.0), (cosT, 0.25)):
        uu = A.alloc("uu", [64, T], F32)
        S.ts(uu, ang, shift, None, op0=ALU.add)
        S.copy(ki, uu)
        S.copy(kf, ki)
        S.tt(uu, uu, kf, ALU.subtract)
        S.ts(kf, uu, 0.5, None, op0=ALU.is_gt)
        S.tt(uu, uu, kf, ALU.subtract)
        S.ts(kf, uu, -0.5, None, op0=ALU.is_lt)
        S.tt(uu, uu, kf, ALU.add)
        S.act(tab, uu, AF.Sin, scale=2.0 * math.pi)
        A.free("uu")
    A.free("posi", "ang", "ki", "kf")

    rows = A.alloc("rows", [128, 4, 128], F32)
    S.memset(rows, 0.0, e="dve")

    def ln_src(li, which):
        if li == 0:
            return W["ln_in_" + which]
        l, j = divmod(li - 1, 3)
        return W["ln%d_%s" % (j + 1, which)][l]
    for wi, which in enumerate("gb"):
        for li in range(1 + 3 * depth):
            q = li * 16
            slot, r = divmod(q, 128)
            S.dma(rows[r:r + 16, 2 * wi + slot, :], ln_src(li, which).rearrange("(c p) -> c p", p=128))
    ps = PS()
    for s4 in range(4):
        S.tr(ps[:, s4 * 128:(s4 + 1) * 128], rows[:, s4, :], ident_f)
    S.copy(lnp, ps)
    A.free("rows")
    A.free("cst")

    def lng(li, c):
        return lnp[:, li * 16 + c: li * 16 + c + 1]

    def lnb(li, c):
        return lnp[:, 256 + li * 16 + c: 256 + li * 16 + c + 1]

    for i in range(8):
        xt = A.alloc("xt%d" % (i % 2), [128, D], F32)
        S.dma(xt, x_d[i * 128:(i + 1) * 128, :], e="sp" if i % 2 == 0 else "act")
        for c4 in range(4):
            ps = PS()
            for j in range(4):
                c = c4 * 4 + j
                S.tr(ps[:, j * 128:(j + 1) * 128], xt[:, c * 128:(c + 1) * 128], ident_f)
            S.copy(hT[:, c4 * 4:(c4 + 1) * 4, i * 128:(i + 1) * 128],
                   ps.rearrange("p (a b) -> p a b", a=4), e="act" if c4 % 2 else "dve")
        A.free("xt%d" % (i % 2))

    def ln_fm(li):
        sq = A.alloc("ln_sq", [128, 16, 512], BF16)
        mean = A.alloc("ln_mean", [128, 1, 512], F32)
        rstd = A.alloc("ln_rstd", [128, 1, 512], F32)
        m2 = A.alloc("ln_m2", [128, 512], F32)
        for hf in range(2):
            hs = HS[hf]
            S.copy(hb[:, :, hs], hT[:, :, hs], e="dve")
            S.act(sq, hT[:, :, hs], AF.Square)
            pm, pq = PS(), PS()
            for c in range(16):
                S.mm(pm, ones_b, hb[:, c, hs], start=(c == 0), stop=(c == 15))
            for c in range(16):
                S.mm(pq, ones_b, sq[:, c, :], start=(c == 0), stop=(c == 15))
            S.act(mean[:, 0, :], pm, AF.Identity, scale=1.0 / D)
            S.tt(m2, mean[:, 0, :], mean[:, 0, :], ALU.mult)
            S.stt(m2, pq, 1.0 / D, m2, ALU.mult, ALU.subtract)
            S.act(m2, m2, AF.Sqrt, bias=epsc)
            S.recip(rstd[:, 0, :], m2)
            S.tt(hT[:, :, hs], hT[:, :, hs], mean.to_broadcast([128, 16, 512]), ALU.subtract)
            S.tt(hT[:, :, hs], hT[:, :, hs], rstd.to_broadcast([128, 16, 512]), ALU.mult, e="pool")
            for c in range(16):
                S.act(hb[:, c, hs], hT[:, c, hs], AF.Identity, scale=lng(li, c), bias=lnb(li, c))
                S.ts(hT[:, c, hs], hT[:, c, hs], lng(li, c), lnb(li, c), op0=ALU.mult, op1=ALU.add)
        A.free("ln_sq", "ln_mean", "ln_rstd", "ln_m2")

    ln_fm(0)
    dump("h0", hT)

    def win_blk(l, c0, c1, name):
        wb = A.alloc(name, [128, 16, c1 - c0], BF16)
        wload(wb, W["w_in"][l, :, c0:c1].rearrange("(kc p) n -> p kc n", p=128))
        return wb

    def win_blk_c(l, c0, c1, name):
        n = c1 - c0
        nch = (n + 127) // 128
        wb = A.alloc(name, [128, nch, 16, 128], BF16)
        for ch in range(nch):
            w = min(128, n - ch * 128)
            wload(wb[:, ch, :, 0:w], W["w_in"][l, :, c0 + ch * 128:c0 + ch * 128 + w].rearrange("(kc p) n -> p kc n", p=128))
        return wb

    def wsl(wb, kc, col0, ncols):
        if len(wb.shape) == 4:
            return wb[:, col0 // 128, kc, col0 % 128: col0 % 128 + ncols]
        return wb[:, kc, col0:col0 + ncols]

    def proj_fm(wb, col0, ncols, evac, kcn=16, rhs=None):
        rhs = hb if rhs is None else rhs
        for hf in range(2):
            ps = PS()
            for kc in range(kcn):
                S.mm(ps[0:ncols, :], wsl(wb, kc, col0, ncols), rhs[:, kc, HS[hf]],
                     start=(kc == 0), stop=(kc == kcn - 1))
            evac(ps[0:ncols, :], hf)

    def bcast_load(name, src1d, n):
        t = A.alloc(name, [128, n], F32)
        S.dma(t, src1d.rearrange("(o n) -> o n", o=1).to_broadcast([128, n]))
        return t

    def layer_params(l):
        rows = A.alloc("prow", [32, 128], F32)
        S.memset(rows, 0.0, e="dve")
        srcs = [(W["mla_qnorm_g"][l], 3), (W["mla_kvnorm_g"][l], 2), (W["gla_b_gate"][l], 2),
                (W["gla_norm_g"][l], 1), (W["ml_norm_g"][l], 1)]
        r = 0
        for src, n in srcs:
            S.dma(rows[r:r + n, :], src.rearrange("(c p) -> c p", p=128))
            r += n
        S.dma(rows[9:25, :], W["ml_conv_w"][l].rearrange("k (c p) -> (k c) p", p=128))
        S.dma(rows[25:29, :], W["ml_conv_b"][l].rearrange("(c p) -> c p", p=128))
        ps = PS()
        S.tr(ps[:, 0:32], rows, ident_f[0:32, 0:32])
        pc = A.alloc("pcol", [128, 32], F32)
        S.copy(pc, ps[:, 0:32])
        A.free("prow")
        return pc

    def mixer_sg(l, yT):
        wu = win_blk_c(l, 0, 512, "wblk0")
        wv = win_blk(l, 512, 1024, "wblk1")
        uT = A.alloc("sg_uT", [128, 4, T], BF16)
        for j in range(4):
            proj_fm(wu, j * 128, 128, lambda ps, hf, j=j: S.act(uT[:, j, HS[hf]], ps, AF.Gelu_apprx_tanh))
        vg = bcast_load("sg_vg", W["sg_vnorm_g"][l], 512)
        vb = bcast_load("sg_vb", W["sg_vnorm_b"][l], 512)
        bsr = bcast_load("sg_bs", W["sg_b_s"][l].rearrange("g t -> (g t)"), 512)
        ws = A.alloc("sg_ws", [128, 4, 128], F32)
        S.dma(ws, W["sg_w_s"][l].rearrange("g t s -> t g s"))
        wT = A.alloc("sg_wT", [128, 4, 128], BF16)
        ps = PS()
        for g in range(4):
            S.tr(ps[:, g * 128:(g + 1) * 128], ws[:, g, :], ident_f)
        S.tt(wT, ps.rearrange("p (a b) -> p a b", a=4), tri_b[:, None, :].to_broadcast([128, 4, 128]), ALU.mult)
        A.free("sg_ws")
        st6 = A.alloc("sg_st", [128, 8], F32)
        mv = A.alloc("sg_mv", [128, 4], F32)
        for i in range(8):
            ts_ = slice(i * 128, (i + 1) * 128)
            ps = PS()
            for kc in range(16):
                S.mm(ps, hb[:, kc, ts_], wv[:, kc, :], start=(kc == 0), stop=(kc == 15))
            vt = A.alloc("sg_vt", [128, 512], F32)
            S.act(vt, ps, AF.Gelu_apprx_tanh)
            S.bnstats(st6[:, 0:6], vt)
            S.bnaggr(mv[:, 0:2], st6[:, 0:6])
            S.act(mv[:, 2:3], mv[:, 1:2], AF.Sqrt, bias=epsc)
            S.recip(mv[:, 2:3], mv[:, 2:3])
            S.stt(mv[:, 3:4], mv[:, 0:1], -1.0, mv[:, 2:3], ALU.mult, ALU.mult)
            S.act(vt, vt, AF.Identity, scale=mv[:, 2:3], bias=mv[:, 3:4])
            S.tt(vt, vt, vg, ALU.mult)
            vnb = A.alloc("sg_vnb", [128, 512], BF16)
            S.tt(vnb, vt, vb, ALU.add)
            ps2 = PS()
            for g in range(4):
                S.mm(ps2[:, g * 128:(g + 1) * 128], vnb[:, g * 128:(g + 1) * 128], wT[:, g, :])
            mx = A.alloc("sg_mx", [128, 4, 128], F32)
            S.tt(mx, ps2.rearrange("p (a b) -> p a b", a=4), bsr.rearrange("p (a b) -> p a b", a=4), ALU.add)
            S.tt(yT[:, :, ts_], mx, uT[:, :, ts_], ALU.mult)
            A.free("sg_vt", "sg_vnb", "sg_mx")
        A.free("wblk0", "wblk1", "sg_uT", "sg_vg", "sg_vb", "sg_bs", "sg_wT", "sg_st", "sg_mv")

    def proj_ps(wb, col0, ncols, hf, kcn=16, rhs=None, ps=None):
        rhs = hb if rhs is None else rhs
        ps = PS() if ps is None else ps
        for kc in range(kcn):
            S.mm(ps[0:ncols, :], wsl(wb, kc, col0, ncols), rhs[:, kc, HS[hf]],
                 start=(kc == 0), stop=(kc == kcn - 1))
        return ps[0:ncols, :]

    def rms_fm(src, dst, nch, gcols, width, dsl=None):
        sq = A.alloc("rms_sq", [128, nch, 512], BF16)
        rs = A.alloc("rms_rs", [128, 512], F32)
        for hf in range(2):
            S.tt(sq, src[:, :, HS[hf]], src[:, :, HS[hf]], ALU.mult, e="pool")
            ps = PS()
            for j in range(nch):
                S.mm(ps, ones_b, sq[:, j, :], start=(j == 0), stop=(j == nch - 1))
            S.act(rs, ps, AF.Sqrt, bias=epsc, scale=1.0 / width)
            S.recip(rs, rs)
            for j in range(nch):
                d = dst[:, j, HS[hf]] if dsl is None else dst[:, j, dsl + hf * 512: dsl + (hf + 1) * 512]
                S.stt(d, src[:, j, HS[hf]], gcols[:, j:j + 1], rs, ALU.mult, ALU.mult)
        A.free("rms_sq", "rms_rs")

    def rope_evac(ps, psr, dst, hf):
        t1 = A.alloc("rp_t1", [64, 512], F32)
        t2 = A.alloc("rp_t2", [64, 512], F32)
        S.tt(t1, psr, sinT[:, HS[hf]], ALU.mult)
        S.tt(t2, ps, cosT[:, HS[hf]], ALU.mult)
        S.tt(dst, t1, t2, ALU.add, e="pool")
        A.free("rp_t1", "rp_t2")

    import os
    MSTOP = int(os.environ.get("MSTOP", "99"))

    def mstop(k):
        if MSTOP == k:
            for n in [n for n in A.live if n.startswith("mla_") or n.startswith("wblk")]:
                A.free(n)
            return True
        return False

    def mixer_mla(l, yT, pcol):
        wb = win_blk_c(l, 1024, 1728, "wblk0")
        wkr = A.alloc("mla_wkr", [128, 16, 64], BF16)
        S.ts(wkr[:, :, 0:32], wb[:, 5, :, 32:64], -1.0, None, op0=ALU.mult, e="pool")
        S.copy(wkr[:, :, 32:64], wb[:, 5, :, 0:32], e="pool")
        ckvn = A.alloc("mla_ckvn", [128, 2, 2 * T], BF16)
        krall = A.alloc("mla_kr", [64, 2 * T], BF16)
        cqn = A.alloc("mla_cqn", [128, 3, T], BF16)
        cq = A.alloc("mla_cq", [128, 3, T], F32)
        for j in range(3):
            proj_fm(wb, j * 128, 128, lambda ps, hf, j=j: S.copy(cq[:, j, HS[hf]], ps, e="act"))
        rms_fm(cq, cqn, 3, pcol[:, 0:3], 384.0)
        A.free("mla_cq")
        ckv = A.alloc("mla_ckv", [128, 2, T], F32)
        for j in range(2):
            proj_fm(wb, 384 + j * 128, 128, lambda ps, hf, j=j: S.copy(ckv[:, j, HS[hf]], ps, e="act"))
        rms_fm(ckv, ckvn, 2, pcol[:, 3:5], 256.0, dsl=T)
        A.free("mla_ckv")
        for hf in range(2):
            ps = proj_ps(wb, 640, 64, hf)
            psr = proj_ps(wkr, 0, 64, hf)
            rope_evac(ps, psr, krall[:, T + hf * 512: T + (hf + 1) * 512], hf)
        A.free("wblk0", "mla_wkr")
        if mstop(1):
            return
        S.dma(cin_kv[0:128, :], ckvn[:, 0, T:2 * T])
        S.dma(cin_kv[128:256, :], ckvn[:, 1, T:2 * T])
        S.dma(cin_kv[256:320, :], krall[:, T:2 * T])
        S.cc(cout_kv, cin_kv, PAIRS)
        S.dma(ckvn[:, 0, 0:T], cout_kv[0:128, :])
        S.dma(ckvn[:, 1, 0:T], cout_kv[128:256, :])
        S.dma(krall[:, 0:T], cout_kv[256:320, :])
        if mstop(2):
            return
        wq = A.alloc("mla_wq", [128, 3, 768], BF16)
        wload(wq, W["mla_w_uq"][l].rearrange("(kc p) n -> p kc n", p=128))
        wqr = A.alloc("mla_wqr", [128, 3, 256], BF16)
        wq4 = wq.rearrange("p k (h d) -> p k h d", h=4)
        wqr4 = wqr.rearrange("p k (h d) -> p k h d", h=4)
        for kc in range(3):
            S.ts(wqr4[:, kc, :, 0:32], wq4[:, kc, :, 160:192], -1.0, None, op0=ALU.mult, e="pool")
            S.copy(wqr4[:, kc, :, 32:64], wq4[:, kc, :, 128:160], e="pool")
        qT = A.alloc("mla_qT", [128, 4, T], BF16)
        qrT = A.alloc("mla_qrT", [64, 4, T], BF16)
        for h in range(4):
            proj_fm(wq, h * 192, 128, lambda ps, hf, h=h: S.copy(qT[:, h, HS[hf]], ps, e="act"), kcn=3, rhs=cqn)
            for hf in range(2):
                ps = proj_ps(wq, h * 192 + 128, 64, hf, kcn=3, rhs=cqn)
                psr = proj_ps(wqr, h * 64, 64, hf, kcn=3, rhs=cqn)
                rope_evac(ps, psr, qrT[:, h, HS[hf]], hf)
        A.free("mla_wq", "mla_wqr", "mla_cqn")
        if mstop(3):
            return
        wkv = A.alloc("mla_wkv", [128, 2, 1024], BF16)
        wload(wkv, W["mla_w_ukv"][l].rearrange("(kc p) n -> p kc n", p=128))
        wv = A.alloc("mla_wv", [128, 2, 512], BF16)
        for kc in range(2):
            S.copy(wv[:, kc, :].rearrange("p (h d) -> p h d", h=4),
                   wkv[:, kc, :].rearrange("p (h t d) -> p h t d", h=4, t=2)[:, :, 1, :], e="pool")
        KT = A.alloc("mla_KT", [128, 4, 2 * T], BF16)
        V = A.alloc("mla_V", [128, 16, 512], BF16)
        for h in range(4):
            for blk in range(4):
                ps = PS()
                for kc in range(2):
                    S.mm(ps, wkv[:, kc, h * 256:h * 256 + 128], ckvn[:, kc, blk * 512:(blk + 1) * 512],
                         start=(kc == 0), stop=(kc == 1))
                S.copy(KT[:, h, blk * 512:(blk + 1) * 512], ps, e="act" if blk % 2 else "dve")
        for j in range(16):
            ps = PS()
            for kc in range(2):
                S.mm(ps, ckvn[:, kc, j * 128:(j + 1) * 128], wv[:, kc, :], start=(kc == 0), stop=(kc == 1))
            S.copy(V[:, j, :], ps, e="act" if j % 2 else "dve")
        A.free("mla_wkv", "mla_wv", "mla_ckvn")
        if mstop(4):
            return
        sc = 192.0 ** -0.5
        rd = A.alloc("mla_rd", [128, 512], F32)
        items = [(h, hf, j) for h in range(4) for hf in range(2) for j in range(8 + (hf + 1) * 4)]

        def q0_of(hf, j):
            return 0 if j < 8 else max((j - 8) * 128 - hf * 512, 0)

        def scores(it):
            h, hf, j = it
            q0 = q0_of(hf, j)
            qs = slice(hf * 512 + q0, (hf + 1) * 512)
            ps = PS()
            S.mm(ps[:, q0:512], KT[:, h, j * 128:(j + 1) * 128], qT[:, h, qs], start=True, stop=False)
            S.mm(ps[:, q0:512], krall[:, j * 128:(j + 1) * 128], qrT[:, h, qs], start=False, stop=True)
            return ps
        pend = scores(items[0])
        po = pd = None
        for idx, (h, hf, j) in enumerate(items):
            nkt = 8 + (hf + 1) * 4
            jl = j - 8
            q0 = q0_of(hf, j)
            ps = pend
            if idx + 1 < len(items):
                pend = scores(items[idx + 1])
            if j == 0:
                po, pd = PSX(), PSX()
            PT = A.alloc("mla_PT%d" % (idx % 3), [128, 512], BF16)
            if j < 8:
                S.act(PT[:, q0:512], ps[:, q0:512], AF.Exp, scale=sc, bias=nb)
            else:
                S.act(PT[:, q0:512], ps[:, q0:512], AF.Exp, scale=sc)
                if jl * 128 >= hf * 512:
                    S.tt(PT[:, q0:q0 + 128], PT[:, q0:q0 + 128], tri_b, ALU.mult, e="pool")
            S.mm(po[:, q0:512], V[:, j, h * 128:(h + 1) * 128], PT[:, q0:512], start=(j == 0), stop=(j == nkt - 1))
            S.mm(pd[:, q0:512], ones_b, PT[:, q0:512], start=(j == 0), stop=(j == nkt - 1))
            A.free("mla_PT%d" % (idx % 3))
            if j == nkt - 1:
                S.recip(rd, pd)
                S.tt(yT[:, h, HS[hf]], po, rd, ALU.mult)
        A.free("mla_rd", "mla_KT", "mla_V", "mla_qT", "mla_qrT", "mla_kr")

    GSTOP = int(os.environ.get("GSTOP", "99"))

    def cla(tag, qtT, ktT, khT, Edec, v_tok, Wd, cin, cout, onum):
        nwb = Wd // 128
        if GSTOP <= 3:
            return
        kh_tok = A.alloc(tag + "khtok", [128, 8, 256], BF16)
        for i in range(8):
            psb = PS().bitcast(BF16)
            for fc in range(2):
                S.tr(psb[:, fc * 128:(fc + 1) * 128], khT[:, fc, i * 128:(i + 1) * 128], ident_b)
            S.copy(kh_tok[:, i, :], psb[:, 0:256], e="act" if i % 2 else "dve")
        St = A.alloc(tag + "S", [128, 2, Wd], F32)
        Sb = A.alloc(tag + "Sb", [128, 2, Wd], BF16)
        S.memset(St, 0.0, e="dve")

        def upd(c, fc):
            i, r0 = c // 2, (c % 2) * 64
            ps = PS()
            S.mm(ps[:, 0:2 * Wd], kh_tok[r0:r0 + 64, i, fc * 128:(fc + 1) * 128],
                 v_tok[r0:r0 + 64, i, 2 * fc:2 * fc + 2, :].rearrange("p a b -> p (a b)"))
            for hh in range(2):
                pr = slice(hh * 64, (hh + 1) * 64)
                S.stt(St[pr, fc, :], St[pr, fc, :], Edec[pr, fc, c:c + 1], ps[pr, hh * Wd:(hh + 1) * Wd],
                      ALU.mult, ALU.add)

        if GSTOP <= 4:
            A.free(tag + "khtok", tag + "S", tag + "Sb")
            return
        for c in range(16):
            for fc in range(2):
                upd(c, fc)
        if GSTOP <= 5:
            A.free(tag + "khtok", tag + "S", tag + "Sb")
            return
        for fc in range(2):
            S.dma(cin[fc * 128:(fc + 1) * 128, :], St[:, fc, :])
        S.cc(cout, cin, PAIRS)
        for fc in range(2):
            S.dma(St[:, fc, :], cout[fc * 128:(fc + 1) * 128, :])
        S.ts(St, St, pf, None, op0=ALU.mult)
        S.copy(Sb, St, e="act")
        if GSTOP <= 6:
            A.free(tag + "khtok", tag + "S", tag + "Sb")
            return
        for i in range(8):
            tsl = slice(i * 128, (i + 1) * 128)
            attm = [A.alloc(tag + "attm%d" % fc, [128, 2, 128], BF16) for fc in range(2)]
            for fc in range(2):
                for hh in range(2):
                    pr = slice(hh * 64, (hh + 1) * 64)
                    pa = PS()
                    S.mm(pa[:, 0:128], ktT[pr, fc, tsl], qtT[pr, fc, tsl])
                    S.tt(attm[fc][:, hh, :], pa[:, 0:128], bd_b, ALU.mult)
            po = [[PSX() for hh in range(2)] for fc in range(2)]
            for fc in range(2):
                for hh in range(2):
                    for wb in range(nwb):
                        S.mm(po[fc][hh][:, wb * 128:(wb + 1) * 128], v_tok[:, i, 2 * fc + hh, wb * 128:(wb + 1) * 128],
                             attm[fc][:, hh, :], start=(wb == 0), stop=False)
            for cc in range(2):
                c = 2 * i + cc
                qsl = slice(i * 128 + cc * 64, i * 128 + cc * 64 + 64)
                for fc in range(2):
                    for hh in range(2):
                        pr = slice(hh * 64, (hh + 1) * 64)
                        for wb in range(nwb):
                            S.mm(po[fc][hh][:, wb * 128 + cc * 64: wb * 128 + (cc + 1) * 64],
                                 Sb[pr, fc, wb * 128:(wb + 1) * 128], qtT[pr, fc, qsl], start=False,
                                 stop=(cc == 1 and wb == nwb - 1))
                for fc in range(2):
                    upd(c, fc)
                for fc in range(2):
                    S.copy(Sb[:, fc, :], St[:, fc, :], e="act")
            for fc in range(2):
                for hh in range(2):
                    h = 2 * fc + hh
                    if nwb == 1:
                        S.copy(onum[:, h, tsl], po[fc][hh][:, 0:128], e="dve")
                    else:
                        dd = A.alloc(tag + "dd", [128, 128], F32)
                        S.act(dd, po[fc][hh][:, 128:256], AF.Abs)
                        S.ts(dd, dd, 1.0, None, op0=ALU.max)
                        S.recip(dd, dd)
                        S.tt(onum[:, h, tsl], po[fc][hh][:, 0:128], dd, ALU.mult)
                        A.free(tag + "dd")
            A.free(tag + "attm0", tag + "attm1")
        A.free(tag + "khtok", tag + "S", tag + "Sb")

    def headnorm_gate(onum, gcol, gateT, yT):
        sq = A.alloc("hn_sq", [128, 512], BF16)
        rs = A.alloc("hn_rs", [128, 512], F32)
        for h in range(4):
            for hf in range(2):
                S.act(sq, onum[:, h, HS[hf]], AF.Square)
                ps = PS()
                S.mm(ps, ones_b, sq)
                S.act(rs, ps, AF.Sqrt, bias=epsc, scale=1.0 / 128.0)
                S.recip(rs, rs)
                S.tt(rs, rs, onum[:, h, HS[hf]], ALU.mult)
                S.stt(yT[:, h, HS[hf]], rs, gcol, gateT[:, h, HS[hf]], ALU.mult, ALU.mult)
        A.free("hn_sq", "hn_rs")

    def mixer_gla(l, yT, pcol):
        wb1 = win_blk_c(l, 1728, 2240, "wblk0")
        qk = A.alloc("gla_qk", [128, 4, T], F32)
        for j in range(4):
            proj_fm(wb1, j * 128, 128, lambda ps, hf, j=j: S.copy(qk[:, j, HS[hf]], ps, e="act"))
        A.free("wblk0")
        wb3 = win_blk_c(l, 2752, 3280, "wblk1")
        soT = A.alloc("gla_so", [128, 4, T], BF16)
        for j in range(4):
            proj_fm(wb3, j * 128, 128, lambda ps, hf, j=j: S.act(soT[:, j, HS[hf]], ps, AF.Silu))
        glr = A.alloc("gla_lr", [16, T], BF16)
        proj_fm(wb3, 512, 16, lambda ps, hf: S.copy(glr[:, HS[hf]], ps, e="act"))
        A.free("wblk1")
        wg = A.alloc("gla_wg", [16, 256], BF16)
        wload(wg, W["gla_w_gate"][l])
        nbg = A.alloc("gla_nbg", [128, 2], F32)
        S.ts(nbg, pcol[:, 5:7], -1.0, None, op0=ALU.mult)
        bneg = A.alloc("gla_bneg", [128, 2, T], F32)
        et = A.alloc("gla_et", [128, 2, T], F32)
        for fc in range(2):
            for hf in range(2):
                ps = PS()
                S.mm(ps, wg[:, fc * 128:(fc + 1) * 128], glr[:, HS[hf]])
                S.act(et[:, fc, HS[hf]], ps, AF.Exp, scale=-1.0, bias=nbg[:, fc:fc + 1])
            S.act(et[:, fc, :], et[:, fc, :], AF.Ln, bias=onec)
            S.ts(et[:, fc, :], et[:, fc, :], 1.0 / 16.0, None, op0=ALU.mult)
            S.scan(bneg[:, fc, :], rmask, et[:, fc, :], 0.0, ALU.mult, ALU.add)
        A.free("gla_wg", "gla_nbg", "gla_lr")
        Edec = A.alloc("gla_Edec", [128, 2, 16], F32)
        qtT = A.alloc("gla_qt", [128, 2, T], BF16)
        ktT = A.alloc("gla_kt", [128, 2, T], BF16)
        khT = A.alloc("gla_kh", [128, 2, T], BF16)
        for fc in range(2):
            bl = bneg[:, fc, :].rearrange("p (c t) -> p c t", t=64)[:, :, 63:64]
            S.act(Edec[:, fc, :], bl.rearrange("p c o -> p (c o)"), AF.Exp, scale=-1.0)
            S.act(et[:, fc, :], bneg[:, fc, :], AF.Exp, scale=-1.0)
            S.stt(qtT[:, fc, :], qk[:, fc, :], 0.125, et[:, fc, :], ALU.mult, ALU.mult)
            S.act(et[:, fc, :], bneg[:, fc, :], AF.Exp)
            S.tt(ktT[:, fc, :], qk[:, 2 + fc, :], et[:, fc, :], ALU.mult)
            S.tt(et[:, fc, :].rearrange("p (c t) -> p c t", t=64), bneg[:, fc, :].rearrange("p (c t) -> p c t", t=64),
                 bl.to_broadcast([128, 16, 64]), ALU.subtract)
            S.act(et[:, fc, :], et[:, fc, :], AF.Exp)
            S.tt(khT[:, fc, :], qk[:, 2 + fc, :], et[:, fc, :], ALU.mult)
        A.free("gla_qk", "gla_bneg", "gla_et")
        wb2 = win_blk(l, 2240, 2752, "wblk0")
        v_tok = A.alloc("gla_v", [128, 8, 4, 128], BF16)
        for i in range(8):
            ps = PS()
            for kc in range(16):
                S.mm(ps, hb[:, kc, i * 128:(i + 1) * 128], wb2[:, kc, :], start=(kc == 0), stop=(kc == 15))
            S.copy(v_tok[:, i, :, :].rearrange("p a b -> p (a b)"), ps, e="act" if i % 2 else "dve")
        A.free("wblk0")
        onum = A.alloc("gla_on", [128, 4, T], F32)
        cla("gla_", qtT, ktT, khT, Edec, v_tok, 128, cin_g, cout_g, onum)
        A.free("gla_qt", "gla_kt", "gla_kh", "gla_v", "gla_Edec")
        headnorm_gate(onum, pcol[:, 7:8], soT, yT)
        A.free("gla_on", "gla_so")

    def mixer_ml(l, yT, pcol):
        wbA = win_blk_c(l, 3280, 3792, "wblk0")
        mqk = A.alloc("ml_mqk", [128, 4, T + 4], F32)
        for j in range(4):
            proj_fm(wbA, j * 128, 128, lambda ps, hf, j=j: S.copy(mqk[:, j, 3 + hf * 512: 3 + (hf + 1) * 512], ps, e="act"))
        A.free("wblk0")
        for j in range(4):
            S.dma(cin_tail[:, j * 4:(j + 1) * 4], mqk[:, j, T - 1:T + 3])
        S.cc(cout_tail, cin_tail, PAIRS)
        for j in range(4):
            S.dma(mqk[:, j, 0:3], cout_tail[0:128, j * 4 + 1:j * 4 + 4])
        for j in range(4):
            S.ts(mqk[:, j, 0:3], mqk[:, j, 0:3], pf, None, op0=ALU.mult)
        cv = A.alloc("ml_cv", [128, 4, T], F32)
        for j in range(4):
            S.ts(cv[:, j, :], mqk[:, j, 3:3 + T], pcol[:, 9 + 12 + j: 9 + 12 + j + 1], pcol[:, 25 + j:25 + j + 1],
                 op0=ALU.mult, op1=ALU.add, e="dve" if j % 2 else "pool")
            for k in range(3):
                S.stt(cv[:, j, :], mqk[:, j, k:k + T], pcol[:, 9 + 4 * k + j: 9 + 4 * k + j + 1], cv[:, j, :],
                      ALU.mult, ALU.add)
            S.act(cv[:, j, :], cv[:, j, :], AF.Silu)
        A.free("ml_mqk")
        wbC = win_blk_c(l, 4304, 4824, "wblk1")
        soT = A.alloc("ml_so", [128, 4, T], BF16)
        for j in range(4):
            proj_fm(wbC, j * 128, 128, lambda ps, hf, j=j: S.act(soT[:, j, HS[hf]], ps, AF.Sigmoid))
        gb = A.alloc("ml_gb", [4, 4], F32)
        S.dma(gb[:, 0:2], W["ml_gate_b"][l].rearrange("(two h) -> h two", two=2), allow_slow_non_contiguous=True)
        S.ts(gb[:, 2:3], gb[:, 1:2], -1.0, None, op0=ALU.mult)
        r_b = A.alloc("ml_rb", [4, T], F32)
        r_t = A.alloc("ml_rt", [4, T], F32)
        r_e = A.alloc("ml_re", [4, T], F32)
        for hf in range(2):
            ps = proj_ps(wbC, 512, 4, hf)
            S.act(r_t[:, HS[hf]], ps, AF.Identity, bias=gb[:, 0:1])
            ps = proj_ps(wbC, 516, 4, hf)
            S.act(r_e[:, HS[hf]], ps, AF.Exp, scale=-1.0, bias=gb[:, 2:3])
        A.free("wblk1")
        S.act(r_e, r_e, AF.Ln, bias=onec[0:4, :])
        S.scan(r_b, rmask[0:4, :], r_e, 0.0, ALU.mult, ALU.add)
        S.tt(r_t, r_t, r_b, ALU.add)
        bl = r_b.rearrange("p (c t) -> p c t", t=64)[:, :, 63:64]
        Edec = A.alloc("ml_Edec", [128, 2, 16], F32)
        rd = A.alloc("ml_rd", [4, 16], F32)
        S.act(rd, bl.rearrange("p c o -> p (c o)"), AF.Exp, scale=-1.0)
        for fc in range(2):
            ps = PS()
            S.mm(ps[:, 0:16], sel_f[:, fc, :], rd)
            S.copy(Edec[:, fc, :], ps[:, 0:16])
        qtT = A.alloc("ml_qt", [128, 2, T], BF16)
        ktT = A.alloc("ml_kt", [128, 2, T], BF16)
        khT = A.alloc("ml_kh", [128, 2, T], BF16)

        def bc_mul(row, dst, src, scale=None):
            for fc in range(2):
                for hf in range(2):
                    ps = PS()
                    S.mm(ps, sel_f[:, fc, :], row[:, HS[hf]])
                    if scale is None:
                        S.tt(dst[:, fc, HS[hf]], ps, src[:, fc, HS[hf]], ALU.mult)
                    else:
                        S.stt(dst[:, fc, HS[hf]], src[:, fc, HS[hf]], scale, ps, ALU.mult, ALU.mult)

        S.act(r_e, r_b, AF.Exp, scale=-1.0)
        bc_mul(r_e, qtT, cv[:, 0:2, :], scale=0.125)
        S.act(r_e, r_t, AF.Exp)
        bc_mul(r_e, ktT, cv[:, 2:4, :])
        S.tt(r_e.rearrange("p (c t) -> p c t", t=64), r_t.rearrange("p (c t) -> p c t", t=64),
             bl.to_broadcast([4, 16, 64]), ALU.subtract)
        S.act(r_e, r_e, AF.Exp)
        bc_mul(r_e, khT, cv[:, 2:4, :])
        A.free("ml_cv", "ml_rb", "ml_rt", "ml_re", "ml_rd", "ml_gb")
        wbB = win_blk(l, 3792, 4304, "wblk0")
        v_tok = A.alloc("ml_v", [128, 8, 4, 256], BF16)
        for i in range(8):
            S.memset(v_tok[:, i, :, 128:256], 1.0, e="pool")
            ps = PS()
            for kc in range(16):
                S.mm(ps, hb[:, kc, i * 128:(i + 1) * 128], wbB[:, kc, :], start=(kc == 0), stop=(kc == 15))
            S.copy(v_tok[:, i, :, 0:128], ps.rearrange("p (a b) -> p a b", a=4), e="act" if i % 2 else "dve")
        A.free("wblk0")
        onum = A.alloc("ml_on", [128, 4, T], F32)
        cla("ml_", qtT, ktT, khT, Edec, v_tok, 256, cin_m, cout_m, onum)
        A.free("ml_qt", "ml_kt", "ml_kh", "ml_v", "ml_Edec")
        headnorm_gate(onum, pcol[:, 8:9], soT, yT)
        A.free("ml_on", "ml_so")

    def pipeline(steps):
        n = len(steps)
        hs_ = [None] * n
        if n:
            hs_[0] = steps[0][0](0)
        for s in range(n):
            if s + 1 < n:
                hs_[s + 1] = steps[s + 1][0]((s + 1) % 2)
            steps[s][1](hs_[s])
            A.free(*steps[s][2](s % 2))

    def resid_evac(dt):
        return lambda ps, hf: S.stt(hT[:, dt, HS[hf]], hT[:, dt, HS[hf]], ALPHA, ps, ALU.mult, ALU.add)

    def merge_out(l, y):
        mT = A.alloc("mT", [128, 16, T], BF16)
        macc = A.alloc("macc", [128, 2, T], F32)
        steps = []
        for G in range(8):
            for n in range(4):
                def load(slot, G=G, n=n):
                    gw = A.alloc("gw%d" % slot, [128, 16, 256], BF16)
                    c0 = 4824 + n * 2048 + G * 256
                    wload(gw, W["w_in"][l, :, c0:c0 + 256].rearrange("(kc p) n -> p kc n", p=128))
                    bw = A.alloc("bw%d" % slot, [128, 4, 256], BF16)
                    wload(bw, W["w_branch"][l, n, :, G * 256:(G + 1) * 256].rearrange("(kc p) n -> p kc n", p=128))
                    return gw, bw

                def comp(hd, G=G, n=n):
                    gw, bw = hd
                    for dt in range(2):
                        for hf in range(2):
                            pg = proj_ps(gw, dt * 128, 128, hf)
                            pp = proj_ps(bw, dt * 128, 128, hf, kcn=4, rhs=y[n])
                            sg = A.alloc("mg_sg", [128, 512], F32)
                            S.act(sg, pg, AF.Sigmoid)
                            if n == 0:
                                S.tt(macc[:, dt, HS[hf]], sg, pp, ALU.mult)
                            else:
                                S.tt(sg, sg, pp, ALU.mult)
                                S.tt(macc[:, dt, HS[hf]], macc[:, dt, HS[hf]], sg, ALU.add, e="pool")
                            A.free("mg_sg")
                    if n == 3:
                        S.copy(mT[:, 2 * G:2 * G + 2, :], macc, e="act")
                steps.append((load, comp, lambda slot: ("gw%d" % slot, "bw%d" % slot)))
        pipeline(steps)
        A.free("macc", "yA", "yB", "yC", "yD")
        steps = []
        for blk in range(4):
            def load(slot, blk=blk):
                wo = A.alloc("wo%d" % slot, [128, 16, 512], BF16)
                wload(wo, W["w_out"][l, :, blk * 512:(blk + 1) * 512].rearrange("(kc p) n -> p kc n", p=128))
                return wo

            def comp(wo, blk=blk):
                for j in range(4):
                    proj_fm(wo, j * 128, 128, resid_evac(blk * 4 + j), rhs=mT)
            steps.append((load, comp, lambda slot: ("wo%d" % slot,)))
        pipeline(steps)
        A.free("mT")

    def xattn(l):
        memT = A.alloc("xa_memT", [128, 16, 256], BF16)
        for i in range(2):
            mt = A.alloc("xa_mt", [128, D], F32)
            S.dma(mt, mem_d[i * 128:(i + 1) * 128, :])
            for c4 in range(4):
                ps = PS()
                for j in range(4):
                    S.tr(ps[:, j * 128:(j + 1) * 128], mt[:, (c4 * 4 + j) * 128:(c4 * 4 + j + 1) * 128], ident_f)
                S.copy(memT[:, c4 * 4:(c4 + 1) * 4, i * 128:(i + 1) * 128], ps.rearrange("p (a b) -> p a b", a=4),
                       e="act" if c4 % 2 else "dve")
            A.free("xa_mt")

        def wl(name, src):
            wb = A.alloc(name, [128, 16, 512], BF16)
            wload(wb, src.rearrange("(kc p) n -> p kc n", p=128))
            return wb
        wq = wl("xa_wq", W["x_w_q"][l])
        wk = wl("xa_wk", W["x_w_kv"][l, :, 0:512])
        qT = A.alloc("xa_qT", [128, 4, T], BF16)
        for h in range(4):
            proj_fm(wq, h * 128, 128, lambda ps, hf, h=h: S.copy(qT[:, h, HS[hf]], ps, e="act"))
        A.free("xa_wq")
        wv = wl("xa_wv", W["x_w_kv"][l, :, 512:1024])
        KT = A.alloc("xa_KT", [128, 4, 256], BF16)
        for h in range(4):
            ps = PS()
            for kc in range(16):
                S.mm(ps[:, 0:256], wk[:, kc, h * 128:(h + 1) * 128], memT[:, kc, :], start=(kc == 0), stop=(kc == 15))
            S.copy(KT[:, h, :], ps[:, 0:256])
        A.free("xa_wk")
        wo = A.alloc("xa_wo", [128, 4, D], BF16)
        wload(wo, W["x_w_o"][l].rearrange("(kc p) n -> p kc n", p=128))
        V = A.alloc("xa_V", [128, 2, 512], BF16)
        for jc in range(2):
            ps = PS()
            for kc in range(16):
                S.mm(ps, memT[:, kc, jc * 128:(jc + 1) * 128], wv[:, kc, :], start=(kc == 0), stop=(kc == 15))
            S.copy(V[:, jc, :], ps, e="act")
        A.free("xa_wv", "xa_memT")
        oT = A.alloc("xa_oT", [128, 4, T], BF16)
        rd = A.alloc("xa_rd", [128, 512], F32)
        sc = 128.0 ** -0.5
        for h in range(4):
            for hf in range(2):
                po, pd = PSX(), PSX()
                for jc in range(2):
                    ps = PS()
                    S.mm(ps, KT[:, h, jc * 128:(jc + 1) * 128], qT[:, h, HS[hf]])
                    PT = A.alloc("xa_PT%d" % jc, [128, 512], BF16)
                    S.act(PT, ps, AF.Exp, scale=sc)
                    S.mm(po, V[:, jc, h * 128:(h + 1) * 128], PT, start=(jc == 0), stop=(jc == 1))
                    S.mm(pd, ones_b, PT, start=(jc == 0), stop=(jc == 1))
                    A.free("xa_PT%d" % jc)
                S.recip(rd, pd)
                S.tt(oT[:, h, HS[hf]], po, rd, ALU.mult)
        A.free("xa_rd", "xa_KT", "xa_V", "xa_qT")
        for dt in range(16):
            proj_fm(wo, dt * 128, 128, resid_evac(dt), kcn=4, rhs=oT)
        A.free("xa_wo", "xa_oT")

    def moe(l):
        wr = A.alloc("mo_wr", [128, 16, 16], F32)
        S.dma(wr, W["w_router"].rearrange("(kc p) e -> p kc e", p=128))
        rb = bcast_load("mo_rb", W["router_bias"], 16)
        aff = A.alloc("mo_aff", [128, 8, 16], F32)
        for i in range(8):
            ps = PS()
            for kc in range(16):
                S.mm(ps[:, 0:16], hT[:, kc, i * 128:(i + 1) * 128], wr[:, kc, :], start=(kc == 0), stop=(kc == 15))
            S.act(aff[:, i, :], ps[:, 0:16], AF.Sigmoid)
        Bt = A.alloc("mo_B", [128, 8, 16], F32)
        B2 = A.alloc("mo_B2", [128, 8, 16], F32)
        eq = A.alloc("mo_eq", [128, 8, 16], F32)
        m1 = A.alloc("mo_m1", [128, 32, 1], F32)
        m2 = A.alloc("mo_m2", [128, 32, 1], F32)
        gm = A.alloc("mo_gm", [128, 8, 1], F32)

        def g4(t):
            return t.rearrange("p a (g e) -> p (a g) e", e=4)

        def red(out, in_, op):
            S.add("dve", lambda g: g.tensor_reduce(out, in_, AX.X, op), [in_], [out])
        S.tt(Bt, aff, rb[:, None, :].to_broadcast([128, 8, 16]), ALU.add)
        red(m1.rearrange("p a o -> p (a o)"), g4(Bt), ALU.max)
        S.tt(g4(eq), g4(Bt), m1.to_broadcast([128, 32, 4]), ALU.is_equal)
        S.stt(B2, eq, -1.0e9, Bt, ALU.mult, ALU.add)
        red(m2.rearrange("p a o -> p (a o)"), g4(B2), ALU.max)
        S.tt(m1, m1, m2, ALU.add)
        red(gm.rearrange("p a o -> p (a o)"), m1.rearrange("p (a g) o -> p a (g o)", g=4), ALU.max)
        S.tt(m1.rearrange("p (a g) o -> p a (g o)", g=4), m1.rearrange("p (a g) o -> p a (g o)", g=4),
             gm.to_broadcast([128, 8, 4]), ALU.is_equal)
        S.tt(g4(eq), g4(Bt), m2.to_broadcast([128, 32, 4]), ALU.is_ge)
        S.tt(g4(eq), g4(eq), m1.to_broadcast([128, 32, 4]), ALU.mult)
        S.tt(eq, eq, aff, ALU.mult)
        red(gm.rearrange("p a o -> p (a o)"), eq, ALU.add)
        S.recip(gm, gm)
        S.tt(eq, eq, gm.to_broadcast([128, 8, 16]), ALU.mult)
        gT = A.alloc("mo_gT", [16, T], F32)
        for i4 in range(2):
            ps = PS()
            for j in range(4):
                S.tr(ps[0:16, j * 128:(j + 1) * 128], eq[:, i4 * 4 + j, :], ident_f)
            S.copy(gT[:, i4 * 512:(i4 + 1) * 512], ps[0:16, :])
        A.free("mo_wr", "mo_rb", "mo_aff", "mo_B", "mo_B2", "mo_eq", "mo_m1", "mo_m2", "mo_gm")
        hid = A.alloc("mo_hid", [128, 4, T], BF16)
        gbc = A.alloc("mo_gbc", [128, T], F32)
        selt = A.alloc("mo_sel", [16, 128], F32)
        steps = []
        for e in range(16):
            for fp in range(2):
                def load(slot, e=e, fp=fp):
                    wg = A.alloc("mo_wg%d" % slot, [128, 16, 256], BF16)
                    wload(wg, W["moe_w_gate"][l, e, :, fp * 256:(fp + 1) * 256].rearrange("(kc p) n -> p kc n", p=128))
                    wu = A.alloc("mo_wu%d" % slot, [128, 16, 256], BF16)
                    wload(wu, W["moe_w_up"][l, e, :, fp * 256:(fp + 1) * 256].rearrange("(kc p) n -> p kc n", p=128))
                    return wg, wu

                def comp(hd, e=e, fp=fp):
                    wg, wu = hd
                    if fp == 0:
                        S.copy(selt, ident_f[0:16, e:e + 1].to_broadcast([16, 128]))
                        for hf in range(2):
                            ps = PS()
                            S.mm(ps, selt, gT[:, HS[hf]])
                            S.copy(gbc[:, HS[hf]], ps, e="act")
                    for j in range(2):
                        for hf in range(2):
                            pg = proj_ps(wg, j * 128, 128, hf)
                            pu = proj_ps(wu, j * 128, 128, hf)
                            sg = A.alloc("mo_sg", [128, 512], F32)
                            S.act(sg, pg, AF.Silu)
                            S.tt(sg, sg, pu, ALU.mult)
                            S.tt(hid[:, fp * 2 + j, HS[hf]], sg, gbc[:, HS[hf]], ALU.mult, e="pool")
                            A.free("mo_sg")
                steps.append((load, comp, lambda slot: ("mo_wg%d" % slot, "mo_wu%d" % slot)))
            for dh in range(2):
                def load(slot, e=e, dh=dh):
                    wd = A.alloc("mo_wd%d" % slot, [128, 4, 1024], BF16)
                    wload(wd, W["moe_w_down"][l, e, :, dh * 1024:(dh + 1) * 1024].rearrange("(kc p) n -> p kc n", p=128))
                    return wd

                def comp(wd, e=e, dh=dh):
                    for j in range(8):
                        dt = dh * 8 + j
                        proj_fm(wd, j * 128, 128,
                                (resid_evac(dt) if e == 0 else
                                 (lambda ps, hf, dt=dt: S.tt(hT[:, dt, HS[hf]], hT[:, dt, HS[hf]], ps, ALU.add))),
                                kcn=4, rhs=hid)
                steps.append((load, comp, lambda slot: ("mo_wd%d" % slot,)))
        pipeline(steps)
        A.free("mo_hid", "mo_gbc", "mo_sel", "mo_gT")

    for l in range(depth):
        pcol = layer_params(l)
        y = [None] * 4
        y[1] = A.alloc("yB", [128, 4, T], BF16)
        mixer_mla(l, y[1], pcol)
        dump("y_b", y[1])
        y[2] = A.alloc("yC", [128, 4, T], BF16)
        mixer_gla(l, y[2], pcol)
        dump("y_c", y[2])
        y[3] = A.alloc("yD", [128, 4, T], BF16)
        mixer_ml(l, y[3], pcol)
        dump("y_d", y[3])
        y[0] = A.alloc("yA", [128, 4, T], BF16)
        mixer_sg(l, y[0])
        dump("y_a", y[0])
        A.free("pcol")
        merge_out(l, y)
        if l == 0:
            dump("pre1", hT)
        ln_fm(1 + 3 * l)
        if l == 0:
            dump("h1", hT)
        xattn(l)
        ln_fm(2 + 3 * l)
        if l == 0:
            dump("h2", hT)
        moe(l)
        ln_fm(3 + 3 * l)

    def write_out():
        for i in range(8):
            ot = A.alloc("ot%d" % (i % 2), [128, D], F32)
            for c4 in range(4):
                ps = PS()
                for j in range(4):
                    c = c4 * 4 + j
                    S.tr(ps[:, j * 128:(j + 1) * 128], hT[:, c, i * 128:(i + 1) * 128], ident_f)
                S.copy(ot[:, c4 * 512:(c4 + 1) * 512], ps, e="act" if c4 % 2 else "dve")
            S.dma(out_d[i * 128:(i + 1) * 128, :], ot, e="sp" if i % 2 == 0 else "act")
            A.free("ot%d" % (i % 2))
    write_out()
    S.finish()
    S.final_wait()
    nc._keep = (st, S)
    nc._depth = depth
    return nc, list(W.keys()), dump_d


_CACHE = {}


def kernel(**inputs):
    depth = DEPTH
    if "prog" not in _CACHE:
        _CACHE["prog"] = build(depth)
    nc, wnames, _ = _CACHE["prog"]
    return run_prog(nc, wnames, inputs)[0]


def run_prog(nc, wnames, inputs, dump_names=()):
    cst = make_consts()
    x = np.ascontiguousarray(inputs["x"], dtype=np.float32)
    mem = np.ascontiguousarray(inputs["mem"], dtype=np.float32)
    pos = np.ascontiguousarray(inputs["positions"], dtype=np.int32)
    in_maps = []
    depth = nc._depth
    wcache = {}
    for k in wnames:
        a = inputs[k]
        if k in LAYERED and depth < 4:
            a = a[:depth]
        wcache[k] = np.ascontiguousarray(a, dtype=np.float32)
    for c in range(8):
        b, hf = divmod(c, 2)
        m = {"x": x[b, hf * T:(hf + 1) * T], "mem": mem[b], "pos": pos[b:b + 1, hf * T:(hf + 1) * T],
             "pflag": np.full((128, 1), float(hf), np.float32), "cst": cst}
        for k in wnames:
            m[k] = wcache[k]
        in_maps.append(m)
    res = run_bass_kernel_spmd(nc, in_maps, core_ids=list(range(8)))
    out = np.empty((4, 2 * T, D), np.float32)
    for c in range(8):
        b, hf = divmod(c, 2)
        out[b, hf * T:(hf + 1) * T] = res.results[c]["out"]
    dumps = {n: [res.results[c]["dbg_" + n] for c in range(8)] for n in dump_names}
    return out, dumps
```

```python
import math
from concourse.bass_utils import run_bass_kernel_spmd
import numpy as np
import concourse.bass as bass
import concourse.mybir as mybir

F32 = mybir.dt.float32
BF16 = mybir.dt.bfloat16
I32 = mybir.dt.int32
AF = mybir.ActivationFunctionType
ALU = mybir.AluOpType
AX = mybir.AxisListType


def _region(ap):
    t = ap.tensor
    esz = mybir.dt.size(ap.dtype)
    pat = ap.ap
    off = ap.offset
    space = str(ap.space)
    if space == "DRAM" or "DRAM" in space.upper() or "HBM" in space.upper():
        lo = off
        hi = off + sum((c - 1) * abs(s) for s, c in pat) + 1
        return (t.name, 0, 1, lo * esz, hi * esz)
    pstride, pcount = pat[0]
    if pstride == 0:
        pstride = 1 << 40
    p0 = off // pstride if pstride < (1 << 40) else 0
    lo = off - p0 * pstride if pstride < (1 << 40) else off
    hi = lo + sum((c - 1) * abs(s) for s, c in pat[1:]) + 1
    if space == "PSUM":
        return (t.name, 0, 128, (lo * esz) // 2048 * 2048, ((hi * esz) + 2047) // 2048 * 2048)
    return (t.name, p0, p0 + pcount, lo * esz, hi * esz)


class _Op:
    __slots__ = ("e", "fn", "r", "w", "dma", "deps", "need", "tok", "slot")

    def __init__(self, e, fn, r, w, dma):
        self.e, self.fn, self.r, self.w, self.dma = e, fn, r, w, dma
        self.deps = None
        self.need = False
        self.tok = None
        self.slot = None


class Sched:
    GEN = 20000

    def __init__(self, nc, n_dma_slots=6):
        self.nc = nc
        self.engs = {"pe": nc.tensor, "act": nc.scalar, "dve": nc.vector,
                     "pool": nc.gpsimd, "sp": nc.sync}
        self.ops = []
        self.n_dma_slots = n_dma_slots

    def add(self, e, fn, reads=(), writes=(), dma=False):
        r = [_region(a) for a in reads if a is not None and hasattr(a, "ap")]
        w = [_region(a) for a in writes if a is not None and hasattr(a, "ap")]
        self.ops.append(_Op(e, fn, r, w, dma))

    def mm(self, out, lhsT, rhs, start=True, stop=True, **kw):
        self.add("pe", lambda g: g.matmul(out, lhsT, rhs, start=start, stop=stop, **kw),
                 [lhsT, rhs], [out])

    def tr(self, out, in_, ident):
        self.add("pe", lambda g: g.transpose(out, in_, ident), [in_, ident], [out])

    def act(self, out, in_, func, bias=None, scale=None, accum_out=None, e="act"):
        kw = {}
        if bias is not None:
            kw["bias"] = bias
        if scale is not None:
            kw["scale"] = scale
        if accum_out is not None:
            kw["accum_out"] = accum_out
        self.add(e, lambda g: g.activation(out, in_, func, **kw),
                 [in_, bias, scale], [out, accum_out])

    def tt(self, out, in0, in1, op, e="dve"):
        self.add(e, lambda g: g.tensor_tensor(out, in0, in1, op), [in0, in1], [out])

    def ts(self, out, in0, s1, s2=None, op0=ALU.mult, op1=None, e="dve", accum_out=None):
        kw = {}
        if op1 is not None:
            kw["op1"] = op1
        if accum_out is not None:
            kw["accum_out"] = accum_out
        self.add(e, lambda g: g.tensor_scalar(out, in0, s1, s2, op0, **kw),
                 [in0, s1, s2], [out, accum_out])

    def stt(self, out, in0, scalar, in1, op0, op1, e="dve"):
        self.add(e, lambda g: g.scalar_tensor_tensor(out, in0, scalar, in1, op0, op1),
                 [in0, scalar, in1], [out])

    def copy(self, out, in_, e="dve"):
        if e == "act":
            self.add(e, lambda g: g.copy(out, in_), [in_], [out])
        else:
            self.add(e, lambda g: g.tensor_copy(out, in_), [in_], [out])

    def memset(self, out, val, e="pool"):
        self.add(e, lambda g: g.memset(out, val), [], [out])

    def dma(self, out, in_, e="sp", **kw):
        self.add(e, lambda g: g.dma_start(out=out, in_=in_, **kw), [in_], [out], dma=True)

    def finish(self):
        nc = self.nc
        ops = self.ops
        recs = {}
        for i, op in enumerate(ops):
            deps = set()
            for (name, p0, p1, lo, hi) in op.r:
                for rc in recs.get(name, ()):
                    if rc[5] and rc[0] < p1 and p0 < rc[1] and rc[2] < hi and lo < rc[3]:
                        deps.add(rc[4])
            for (name, p0, p1, lo, hi) in op.w:
                for rc in recs.get(name, ()):
                    if rc[0] < p1 and p0 < rc[1] and rc[2] < hi and lo < rc[3]:
                        deps.add(rc[4])
            deps.discard(i)
            if op.e == "pe":
                deps = {d for d in deps if ops[d].e != "pe" or ops[d].dma}
            op.deps = deps
            for d in deps:
                ops[d].need = True
            for (name, p0, p1, lo, hi) in op.w:
                L = recs.setdefault(name, [])
                L[:] = [rc for rc in L if not (p0 <= rc[0] and rc[1] <= p1 and lo <= rc[2] and rc[3] <= hi)]
                L.append([p0, p1, lo, hi, i, True, op.e])
            for (name, p0, p1, lo, hi) in op.r:
                L = recs.setdefault(name, [])
                if not op.dma:
                    L[:] = [rc for rc in L if not ((not rc[5]) and rc[6] == op.e and not ops[rc[4]].dma
                                                   and p0 <= rc[0] and rc[1] <= p1 and lo <= rc[2] and rc[3] <= hi)]
                L.append([p0, p1, lo, hi, i, False, op.e])
        ticks = {e: 0 for e in self.engs}
        ndma = {e: 0 for e in self.engs}
        ncc = 0
        for op in ops:
            if op.dma == "cc":
                op.tok = ("k", "cc", ncc, 1)
                ncc += 1
            elif op.dma:
                k = ndma[op.e]
                ndma[op.e] += 1
                op.slot = (op.e, k % self.n_dma_slots)
                op.tok = ("d", op.e, k % self.n_dma_slots, 16 * (k // self.n_dma_slots + 1))
            elif op.need:
                ticks[op.e] += 1
                t = ticks[op.e]
                op.tok = ("c", op.e, (t - 1) // self.GEN, (t - 1) % self.GEN + 1)
        self.sems = {}
        import contextlib
        self._stack = contextlib.ExitStack()

        def sem(key):
            if key not in self.sems:
                self.sems[key] = self._stack.enter_context(nc.semaphore("s_%s_%s_%s" % key))
            return self.sems[key]

        waited = {e: {} for e in self.engs}
        last_on_slot = {}
        self.n_waits = 0
        for op in ops:
            g = self.engs[op.e]
            need = {}
            for d in op.deps:
                tk = ops[d].tok
                key = tk[:3]
                if need.get(key, 0) < tk[3]:
                    need[key] = tk[3]
            if op.dma and op.dma != "cc":
                prev = last_on_slot.get(op.slot)
                if prev is not None:
                    key = prev[:3]
                    if need.get(key, 0) < prev[3]:
                        need[key] = prev[3]
            for key, v in need.items():
                if waited[op.e].get(key, 0) < v:
                    g.wait_ge(sem(key), v)
                    waited[op.e][key] = v
                    self.n_waits += 1
            ins = op.fn(g)
            if op.dma == "cc":
                ins.then_inc(sem(op.tok[:3]), 1)
            elif op.dma:
                ins.then_inc(sem(op.tok[:3]), 16)
                last_on_slot[op.slot] = op.tok
            elif op.tok is not None:
                ins.then_inc(sem(op.tok[:3]), 1)
        self.ticks = ticks
        return self

    def wait_all_dma_on(self, e="sp"):
        raise NotImplementedError


def _finish_tail(self, e="sp"):
    g = self.engs[e]
    seen = {}
    for op in self.ops:
        if op.dma:
            seen[op.tok[:3]] = max(seen.get(op.tok[:3], 0), op.tok[3])
    for key, v in seen.items():
        g.wait_ge(self.sems[key], v)


Sched.final_wait = _finish_tail


def _recip(self, out, in_, e="dve"):
    self.add(e, lambda g: g.reciprocal(out, in_), [in_], [out])


def _scan(self, out, d0, d1, init, op0, op1):
    self.add("dve", lambda g: g.tensor_tensor_scan(out, d0, d1, init, op0, op1), [d0, d1, init], [out])


def _bnstats(self, out, in_):
    self.add("dve", lambda g: g.bn_stats(out, in_), [in_], [out])


def _bnaggr(self, out, in_):
    self.add("dve", lambda g: g.bn_aggr(out, in_), [in_], [out])


def _cc(self, out, in_, groups):
    self.add("pool", lambda g: g.collective_compute("AllGather", ALU.bypass, replica_groups=groups,
                                                    ins=[in_], outs=[out]), [in_], [out], dma="cc")


Sched.recip = _recip
Sched.scan = _scan
Sched.bnstats = _bnstats
Sched.bnaggr = _bnaggr
Sched.cc = _cc


class Arena:
    def __init__(self, nc, stack, nbytes):
        self.nb = nbytes
        self.t = stack.enter_context(nc.sbuf_tensor("arena", [128, nbytes // 2], BF16))
        self.free_list = [(0, nbytes)]
        self.live = {}

    def alloc(self, name, shape, dt, parts=None):
        P = shape[0]
        n = 1
        for s in shape[1:]:
            n *= s
        nbytes = n * mybir.dt.size(dt)
        nbytes = (nbytes + 63) // 64 * 64
        for i, (o, sz) in enumerate(self.free_list):
            if sz >= nbytes:
                if sz == nbytes:
                    self.free_list.pop(i)
                else:
                    self.free_list[i] = (o + nbytes, sz - nbytes)
                self.live[name] = (o, nbytes)
                ap = self.t[0:P, o // 2:(o + n * mybir.dt.size(dt)) // 2]
                if dt != BF16:
                    ap = ap.bitcast(dt)
                if len(shape) == 3:
                    ap = ap.rearrange("p (a b) -> p a b", a=shape[1])
                elif len(shape) == 4:
                    ap = ap.rearrange("p (a b c) -> p a b c", a=shape[1], b=shape[2])
                return ap
        raise RuntimeError("arena full: %s %d free=%s" % (name, nbytes, self.free_list))

    def free(self, *names):
        for name in names:
            o, sz = self.live.pop(name)
            self.free_list.append((o, sz))
        self.free_list.sort()
        m = []
        for o, sz in self.free_list:
            if m and m[-1][0] + m[-1][1] == o:
                m[-1] = (m[-1][0], m[-1][1] + sz)
            else:
                m.append((o, sz))
        self.free_list = m


T = 1024
DEPTH = 4
D = 2048
ALPHA = (2.0 * DEPTH) ** 0.25
EPS = 1e-5
PAIRS = [[0, 1], [2, 3], [4, 5], [6, 7]]
NCST = 128 * 3 + 1024 + 256 + 1
ARENA_BYTES = 202 * 1024

PARAM_SHAPES = dict(
    ln_in_g=[2048], ln_in_b=[2048], w_in=[4, 2048, 13016], sg_vnorm_g=[4, 512], sg_vnorm_b=[4, 512],
    sg_w_s=[4, 4, 128, 128], sg_b_s=[4, 4, 128], mla_qnorm_g=[4, 384], mla_kvnorm_g=[4, 256],
    mla_w_uq=[4, 384, 768], mla_w_ukv=[4, 256, 1024], gla_w_gate=[4, 16, 256], gla_b_gate=[4, 256],
    gla_norm_g=[4, 128], ml_conv_w=[4, 4, 512], ml_conv_b=[4, 512], ml_gate_b=[4, 8], ml_norm_g=[4, 128],
    w_branch=[4, 4, 512, 2048], w_out=[4, 2048, 2048], ln1_g=[4, 2048], ln1_b=[4, 2048],
    x_w_q=[4, 2048, 512], x_w_kv=[4, 2048, 1024], x_w_o=[4, 512, 2048], ln2_g=[4, 2048], ln2_b=[4, 2048],
    w_router=[2048, 16], router_bias=[16], moe_w_gate=[4, 16, 2048, 512], moe_w_up=[4, 16, 2048, 512],
    moe_w_down=[4, 16, 512, 2048], ln3_g=[4, 2048], ln3_b=[4, 2048])


LAYERED = [k for k, v in PARAM_SHAPES.items() if v[0] == 4 and len(v) >= 2 and k not in ('w_router',)]


def make_consts():
    c = np.zeros((128, NCST), np.float32)
    p = np.arange(128)[:, None]
    f = np.arange(128)[None, :]
    c[:, 0:128] = (p == f)
    c[:, 128:256] = (p <= f)
    c[:, 256:384] = (p <= f) & ((p // 64) == (f // 64))
    t = np.arange(1024)
    c[:, 384:1408] = (t % 64 != 0)[None, :]
    for k in range(4):
        for fc in range(2):
            for m in range(128):
                c[k, 1408 + fc * 128 + m] = 1.0 if k == 2 * fc + m // 64 else 0.0
    fr = (np.float32(10000.0) ** (-np.arange(32, dtype=np.float32) / np.float32(32))).astype(np.float32)
    c[0:32, 1664] = fr
    c[32:64, 1664] = fr
    return c


def build(depth=DEPTH, stop_after=None, dumps=()):
    import contextlib
    nc = bass.Bass("TRN2", target_bir_lowering=False)

    def din(name, shape, dt=F32):
        return nc.dram_tensor(name, list(shape), dt, kind="ExternalInput").ap()

    x_d = din("x", [T, D])
    mem_d = din("mem", [256, D])
    pos_d = din("pos", [1, T], I32)
    pf_d = din("pflag", [128, 1])
    cst_d = din("cst", [128, NCST])
    class _W(dict):
        def __missing__(self, k):
            shp = list(PARAM_SHAPES[k])
            if k in LAYERED:
                shp[0] = depth
            self[k] = din(k, shp)
            return self[k]
    W = _W()
    out_d = nc.dram_tensor("out", [T, D], F32, kind="ExternalOutput").ap()
    dump_d = {}

    def internal(name, shape, dt=F32):
        return nc.dram_tensor(name, list(shape), dt, kind="Internal").ap()

    cin_tail, cout_tail = internal("cin_tail", [128, 16]), internal("cout_tail", [256, 16])
    cin_kv, cout_kv = internal("cin_kv", [320, T], BF16), internal("cout_kv", [640, T], BF16)
    cin_g, cout_g = internal("cin_g", [256, 128]), internal("cout_g", [512, 128])
    cin_m, cout_m = internal("cin_m", [256, 256]), internal("cout_m", [512, 256])

    S = Sched(nc)
    st = contextlib.ExitStack()
    A = Arena(nc, st, ARENA_BYTES)
    pst = st.enter_context(nc.psum_tensor("ps", [128, 8, 512], F32))
    psn = [0]

    def PS():
        b = psn[0] % 4
        psn[0] += 1
        return pst[:, b, :]

    pxn = [0]

    def PSX():
        b = 4 + pxn[0] % 4
        pxn[0] += 1
        return pst[:, b, :]

    def dump(name, ap):
        if name in dumps:
            dd = nc.dram_tensor("dbg_" + name, list(ap.shape), ap.dtype, kind="ExternalOutput").ap()
            dump_d[name] = dd
            S.dma(dd, ap)

    def wload(dst, src):
        S.dma(dst, src, e="pool")

    HS = [slice(0, 512), slice(512, 1024)]

    hT = A.alloc("hT", [128, 16, T], F32)
    hb = A.alloc("hb", [128, 16, T], BF16)
    ident_f = A.alloc("ident_f", [128, 128], F32)
    ident_b = A.alloc("ident_b", [128, 128], BF16)
    ones_b = A.alloc("ones_b", [128, 128], BF16)
    tri_b = A.alloc("tri_b", [128, 128], BF16)
    bd_b = A.alloc("bd_b", [128, 128], BF16)
    rmask = A.alloc("rmask", [128, T], BF16)
    sel_f = A.alloc("sel_f", [4, 2, 128], F32)
    small = A.alloc("small", [128, 8], F32)
    pf, nb, epsc, freq, onec = (small[:, i:i + 1] for i in range(5))
    lnp = A.alloc("lnp", [128, 512], F32)
    cosT = A.alloc("cosT", [64, T], BF16)
    sinT = A.alloc("sinT", [64, T], BF16)

    cst = A.alloc("cst", [128, NCST], F32)
    S.dma(cst, cst_d)
    S.dma(pf, pf_d)
    S.copy(ident_f, cst[:, 0:128])
    S.copy(ident_b, cst[:, 0:128])
    S.copy(tri_b, cst[:, 128:256])
    S.copy(bd_b, cst[:, 256:384])
    S.copy(rmask, cst[:, 384:1408])
    S.copy(sel_f, cst[0:4, 1408:1664].rearrange("p (a b) -> p a b", a=2))
    S.copy(freq, cst[:, 1664:1665])
    S.memset(ones_b, 1.0, e="dve")
    S.memset(epsc, EPS, e="dve")
    S.memset(onec, 1.0, e="dve")
    S.ts(nb, pf, 30000.0, -30000.0, op0=ALU.mult, op1=ALU.add)

    posi = A.alloc("posi", [64, T], I32)
    S.dma(posi, pos_d.partition_broadcast(64) if False else pos_d[0:1, :].to_broadcast([64, T]))
    ang = A.alloc("ang", [64, T], F32)
    S.copy(ang, posi)
    S.ts(ang, ang, freq[0:64, :], 1.0 / (2.0 * math.pi), op0=ALU.mult, op1=ALU.mult)
    ki = A.alloc("ki", [64, T], I32)
    kf = A.alloc("kf", [64, T], F32)
    for tab, shift in ((sinT, 0.0), (cosT, 0.25)):
        uu = A.alloc("uu", [64, T], F32)
        S.ts(uu, ang, shift, None, op0=ALU.add)
        S.copy(ki, uu)
        S.copy(kf, ki)
        S.tt(uu, uu, kf, ALU.subtract)
        S.ts(kf, uu, 0.5, None, op0=ALU.is_gt)
        S.tt(uu, uu, kf, ALU.subtract)
        S.ts(kf, uu, -0.5, None, op0=ALU.is_lt)
        S.tt(uu, uu, kf, ALU.add)
        S.act(tab, uu, AF.Sin, scale=2.0 * math.pi)
        A.free("uu")
    A.free("posi", "ang", "ki", "kf")

    rows = A.alloc("rows", [128, 4, 128], F32)
    S.memset(rows, 0.0, e="dve")

    def ln_src(li, which):
        if li == 0:
            return W["ln_in_" + which]
        l, j = divmod(li - 1, 3)
        return W["ln%d_%s" % (j + 1, which)][l]
    for wi, which in enumerate("gb"):
        for li in range(1 + 3 * depth):
            q = li * 16
            slot, r = divmod(q, 128)
            S.dma(rows[r:r + 16, 2 * wi + slot, :], ln_src(li, which).rearrange("(c p) -> c p", p=128))
    ps = PS()
    for s4 in range(4):
        S.tr(ps[:, s4 * 128:(s4 + 1) * 128], rows[:, s4, :], ident_f)
    S.copy(lnp, ps)
    A.free("rows")
    A.free("cst")

    def lng(li, c):
        return lnp[:, li * 16 + c: li * 16 + c + 1]

    def lnb(li, c):
        return lnp[:, 256 + li * 16 + c: 256 + li * 16 + c + 1]

    for i in range(8):
        xt = A.alloc("xt%d" % (i % 2), [128, D], F32)
        S.dma(xt, x_d[i * 128:(i + 1) * 128, :], e="sp" if i % 2 == 0 else "act")
        for c4 in range(4):
            ps = PS()
            for j in range(4):
                c = c4 * 4 + j
                S.tr(ps[:, j * 128:(j + 1) * 128], xt[:, c * 128:(c + 1) * 128], ident_f)
            S.copy(hT[:, c4 * 4:(c4 + 1) * 4, i * 128:(i + 1) * 128],
                   ps.rearrange("p (a b) -> p a b", a=4), e="act" if c4 % 2 else "dve")
        A.free("xt%d" % (i % 2))

    def ln_fm(li):
        sq = A.alloc("ln_sq", [128, 16, 512], BF16)
        mean = A.alloc("ln_mean", [128, 1, 512], F32)
        rstd = A.alloc("ln_rstd", [128, 1, 512], F32)
        m2 = A.alloc("ln_m2", [128, 512], F32)
        for hf in range(2):
            hs = HS[hf]
            S.copy(hb[:, :, hs], hT[:, :, hs], e="dve")
            S.act(sq, hT[:, :, hs], AF.Square)
            pm, pq = PS(), PS()
            for c in range(16):
                S.mm(pm, ones_b, hb[:, c, hs], start=(c == 0), stop=(c == 15))
            for c in range(16):
                S.mm(pq, ones_b, sq[:, c, :], start=(c == 0), stop=(c == 15))
            S.act(mean[:, 0, :], pm, AF.Identity, scale=1.0 / D)
            S.tt(m2, mean[:, 0, :], mean[:, 0, :], ALU.mult)
            S.stt(m2, pq, 1.0 / D, m2, ALU.mult, ALU.subtract)
            S.act(m2, m2, AF.Sqrt, bias=epsc)
            S.recip(rstd[:, 0, :], m2)
            S.tt(hT[:, :, hs], hT[:, :, hs], mean.to_broadcast([128, 16, 512]), ALU.subtract)
            S.tt(hT[:, :, hs], hT[:, :, hs], rstd.to_broadcast([128, 16, 512]), ALU.mult, e="pool")
            for c in range(16):
                S.act(hb[:, c, hs], hT[:, c, hs], AF.Identity, scale=lng(li, c), bias=lnb(li, c))
                S.ts(hT[:, c, hs], hT[:, c, hs], lng(li, c), lnb(li, c), op0=ALU.mult, op1=ALU.add)
        A.free("ln_sq", "ln_mean", "ln_rstd", "ln_m2")

    ln_fm(0)
    dump("h0", hT)

    def win_blk(l, c0, c1, name):
        wb = A.alloc(name, [128, 16, c1 - c0], BF16)
        wload(wb, W["w_in"][l, :, c0:c1].rearrange("(kc p) n -> p kc n", p=128))
        return wb

    def win_blk_c(l, c0, c1, name):
        n = c1 - c0
        nch = (n + 127) // 128
        wb = A.alloc(name, [128, nch, 16, 128], BF16)
        for ch in range(nch):
            w = min(128, n - ch * 128)
            wload(wb[:, ch, :, 0:w], W["w_in"][l, :, c0 + ch * 128:c0 + ch * 128 + w].rearrange("(kc p) n -> p kc n", p=128))
        return wb

    def wsl(wb, kc, col0, ncols):
        if len(wb.shape) == 4:
            return wb[:, col0 // 128, kc, col0 % 128: col0 % 128 + ncols]
        return wb[:, kc, col0:col0 + ncols]

    def proj_fm(wb, col0, ncols, evac, kcn=16, rhs=None):
        rhs = hb if rhs is None else rhs
        for hf in range(2):
            ps = PS()
            for kc in range(kcn):
                S.mm(ps[0:ncols, :], wsl(wb, kc, col0, ncols), rhs[:, kc, HS[hf]],
                     start=(kc == 0), stop=(kc == kcn - 1))
            evac(ps[0:ncols, :], hf)

    def bcast_load(name, src1d, n):
        t = A.alloc(name, [128, n], F32)
        S.dma(t, src1d.rearrange("(o n) -> o n", o=1).to_broadcast([128, n]))
        return t

    def layer_params(l):
        rows = A.alloc("prow", [32, 128], F32)
        S.memset(rows, 0.0, e="dve")
        srcs = [(W["mla_qnorm_g"][l], 3), (W["mla_kvnorm_g"][l], 2), (W["gla_b_gate"][l], 2),
                (W["gla_norm_g"][l], 1), (W["ml_norm_g"][l], 1)]
        r = 0
        for src, n in srcs:
            S.dma(rows[r:r + n, :], src.rearrange("(c p) -> c p", p=128))
            r += n
        S.dma(rows[9:25, :], W["ml_conv_w"][l].rearrange("k (c p) -> (k c) p", p=128))
        S.dma(rows[25:29, :], W["ml_conv_b"][l].rearrange("(c p) -> c p", p=128))
        ps = PS()
        S.tr(ps[:, 0:32], rows, ident_f[0:32, 0:32])
        pc = A.alloc("pcol", [128, 32], F32)
        S.copy(pc, ps[:, 0:32])
        A.free("prow")
        return pc

    def mixer_sg(l, yT):
        wu = win_blk_c(l, 0, 512, "wblk0")
        wv = win_blk(l, 512, 1024, "wblk1")
        uT = A.alloc("sg_uT", [128, 4, T], BF16)
        for j in range(4):
            proj_fm(wu, j * 128, 128, lambda ps, hf, j=j: S.act(uT[:, j, HS[hf]], ps, AF.Gelu_apprx_tanh))
        vg = bcast_load("sg_vg", W["sg_vnorm_g"][l], 512)
        vb = bcast_load("sg_vb", W["sg_vnorm_b"][l], 512)
        bsr = bcast_load("sg_bs", W["sg_b_s"][l].rearrange("g t -> (g t)"), 512)
        ws = A.alloc("sg_ws", [128, 4, 128], F32)
        S.dma(ws, W["sg_w_s"][l].rearrange("g t s -> t g s"))
        wT = A.alloc("sg_wT", [128, 4, 128], BF16)
        ps = PS()
        for g in range(4):
            S.tr(ps[:, g * 128:(g + 1) * 128], ws[:, g, :], ident_f)
        S.tt(wT, ps.rearrange("p (a b) -> p a b", a=4), tri_b[:, None, :].to_broadcast([128, 4, 128]), ALU.mult)
        A.free("sg_ws")
        st6 = A.alloc("sg_st", [128, 8], F32)
        mv = A.alloc("sg_mv", [128, 4], F32)
        for i in range(8):
            ts_ = slice(i * 128, (i + 1) * 128)
            ps = PS()
            for kc in range(16):
                S.mm(ps, hb[:, kc, ts_], wv[:, kc, :], start=(kc == 0), stop=(kc == 15))
            vt = A.alloc("sg_vt", [128, 512], F32)
            S.act(vt, ps, AF.Gelu_apprx_tanh)
            S.bnstats(st6[:, 0:6], vt)
            S.bnaggr(mv[:, 0:2], st6[:, 0:6])
            S.act(mv[:, 2:3], mv[:, 1:2], AF.Sqrt, bias=epsc)
            S.recip(mv[:, 2:3], mv[:, 2:3])
            S.stt(mv[:, 3:4], mv[:, 0:1], -1.0, mv[:, 2:3], ALU.mult, ALU.mult)
            S.act(vt, vt, AF.Identity, scale=mv[:, 2:3], bias=mv[:, 3:4])
            S.tt(vt, vt, vg, ALU.mult)
            vnb = A.alloc("sg_vnb", [128, 512], BF16)
            S.tt(vnb, vt, vb, ALU.add)
            ps2 = PS()
            for g in range(4):
                S.mm(ps2[:, g * 128:(g + 1) * 128], vnb[:, g * 128:(g + 1) * 128], wT[:, g, :])
            mx = A.alloc("sg_mx", [128, 4, 128], F32)
            S.tt(mx, ps2.rearrange("p (a b) -> p a b", a=4), bsr.rearrange("p (a b) -> p a b", a=4), ALU.add)
            S.tt(yT[:, :, ts_], mx, uT[:, :, ts_], ALU.mult)
            A.free("sg_vt", "sg_vnb", "sg_mx")
        A.free("wblk0", "wblk1", "sg_uT", "sg_vg", "sg_vb", "sg_bs", "sg_wT", "sg_st", "sg_mv")

    def proj_ps(wb, col0, ncols, hf, kcn=16, rhs=None, ps=None):
        rhs = hb if rhs is None else rhs
        ps = PS() if ps is None else ps
        for kc in range(kcn):
            S.mm(ps[0:ncols, :], wsl(wb, kc, col0, ncols), rhs[:, kc, HS[hf]],
                 start=(kc == 0), stop=(kc == kcn - 1))
        return ps[0:ncols, :]

    def rms_fm(src, dst, nch, gcols, width, dsl=None):
        sq = A.alloc("rms_sq", [128, nch, 512], BF16)
        rs = A.alloc("rms_rs", [128, 512], F32)
        for hf in range(2):
            S.tt(sq, src[:, :, HS[hf]], src[:, :, HS[hf]], ALU.mult, e="pool")
            ps = PS()
            for j in range(nch):
                S.mm(ps, ones_b, sq[:, j, :], start=(j == 0), stop=(j == nch - 1))
            S.act(rs, ps, AF.Sqrt, bias=epsc, scale=1.0 / width)
            S.recip(rs, rs)
            for j in range(nch):
                d = dst[:, j, HS[hf]] if dsl is None else dst[:, j, dsl + hf * 512: dsl + (hf + 1) * 512]
                S.stt(d, src[:, j, HS[hf]], gcols[:, j:j + 1], rs, ALU.mult, ALU.mult)
        A.free("rms_sq", "rms_rs")

    def rope_evac(ps, psr, dst, hf):
        t1 = A.alloc("rp_t1", [64, 512], F32)
        t2 = A.alloc("rp_t2", [64, 512], F32)
        S.tt(t1, psr, sinT[:, HS[hf]], ALU.mult)
        S.tt(t2, ps, cosT[:, HS[hf]], ALU.mult)
        S.tt(dst, t1, t2, ALU.add, e="pool")
        A.free("rp_t1", "rp_t2")

    import os
    MSTOP = int(os.environ.get("MSTOP", "99"))

    def mstop(k):
        if MSTOP == k:
            for n in [n for n in A.live if n.startswith("mla_") or n.startswith("wblk")]:
                A.free(n)
            return True
        return False

    def mixer_mla(l, yT, pcol):
        wb = win_blk_c(l, 1024, 1728, "wblk0")
        wkr = A.alloc("mla_wkr", [128, 16, 64], BF16)
        S.ts(wkr[:, :, 0:32], wb[:, 5, :, 32:64], -1.0, None, op0=ALU.mult, e="pool")
        S.copy(wkr[:, :, 32:64], wb[:, 5, :, 0:32], e="pool")
        ckvn = A.alloc("mla_ckvn", [128, 2, 2 * T], BF16)
        krall = A.alloc("mla_kr", [64, 2 * T], BF16)
        cqn = A.alloc("mla_cqn", [128, 3, T], BF16)
        cq = A.alloc("mla_cq", [128, 3, T], F32)
        for j in range(3):
            proj_fm(wb, j * 128, 128, lambda ps, hf, j=j: S.copy(cq[:, j, HS[hf]], ps, e="act"))
        rms_fm(cq, cqn, 3, pcol[:, 0:3], 384.0)
        A.free("mla_cq")
        ckv = A.alloc("mla_ckv", [128, 2, T], F32)
        for j in range(2):
            proj_fm(wb, 384 + j * 128, 128, lambda ps, hf, j=j: S.copy(ckv[:, j, HS[hf]], ps, e="act"))
        rms_fm(ckv, ckvn, 2, pcol[:, 3:5], 256.0, dsl=T)
        A.free("mla_ckv")
        for hf in range(2):
            ps = proj_ps(wb, 640, 64, hf)
            psr = proj_ps(wkr, 0, 64, hf)
            rope_evac(ps, psr, krall[:, T + hf * 512: T + (hf + 1) * 512], hf)
        A.free("wblk0", "mla_wkr")
        if mstop(1):
            return
        S.dma(cin_kv[0:128, :], ckvn[:, 0, T:2 * T])
        S.dma(cin_kv[128:256, :], ckvn[:, 1, T:2 * T])
        S.dma(cin_kv[256:320, :], krall[:, T:2 * T])
        S.cc(cout_kv, cin_kv, PAIRS)
        S.dma(ckvn[:, 0, 0:T], cout_kv[0:128, :])
        S.dma(ckvn[:, 1, 0:T], cout_kv[128:256, :])
        S.dma(krall[:, 0:T], cout_kv[256:320, :])
        if mstop(2):
            return
        wq = A.alloc("mla_wq", [128, 3, 768], BF16)
        wload(wq, W["mla_w_uq"][l].rearrange("(kc p) n -> p kc n", p=128))
        wqr = A.alloc("mla_wqr", [128, 3, 256], BF16)
        wq4 = wq.rearrange("p k (h d) -> p k h d", h=4)
        wqr4 = wqr.rearrange("p k (h d) -> p k h d", h=4)
        for kc in range(3):
            S.ts(wqr4[:, kc, :, 0:32], wq4[:, kc, :, 160:192], -1.0, None, op0=ALU.mult, e="pool")
            S.copy(wqr4[:, kc, :, 32:64], wq4[:, kc, :, 128:160], e="pool")
        qT = A.alloc("mla_qT", [128, 4, T], BF16)
        qrT = A.alloc("mla_qrT", [64, 4, T], BF16)
        for h in range(4):
            proj_fm(wq, h * 192, 128, lambda ps, hf, h=h: S.copy(qT[:, h, HS[hf]], ps, e="act"), kcn=3, rhs=cqn)
            for hf in range(2):
                ps = proj_ps(wq, h * 192 + 128, 64, hf, kcn=3, rhs=cqn)
                psr = proj_ps(wqr, h * 64, 64, hf, kcn=3, rhs=cqn)
                rope_evac(ps, psr, qrT[:, h, HS[hf]], hf)
        A.free("mla_wq", "mla_wqr", "mla_cqn")
        if mstop(3):
            return
        wkv = A.alloc("mla_wkv", [128, 2, 1024], BF16)
        wload(wkv, W["mla_w_ukv"][l].rearrange("(kc p) n -> p kc n", p=128))
        wv = A.alloc("mla_wv", [128, 2, 512], BF16)
        for kc in range(2):
            S.copy(wv[:, kc, :].rearrange("p (h d) -> p h d", h=4),
                   wkv[:, kc, :].rearrange("p (h t d) -> p h t d", h=4, t=2)[:, :, 1, :], e="pool")
        KT = A.alloc("mla_KT", [128, 4, 2 * T], BF16)
        V = A.alloc("mla_V", [128, 16, 512], BF16)
        for h in range(4):
            for blk in range(4):
                ps = PS()
                for kc in range(2):
                    S.mm(ps, wkv[:, kc, h * 256:h * 256 + 128], ckvn[:, kc, blk * 512:(blk + 1) * 512],
                         start=(kc == 0), stop=(kc == 1))
                S.copy(KT[:, h, blk * 512:(blk + 1) * 512], ps, e="act" if blk % 2 else "dve")
        for j in range(16):
            ps = PS()
            for kc in range(2):
                S.mm(ps, ckvn[:, kc, j * 128:(j + 1) * 128], wv[:, kc, :], start=(kc == 0), stop=(kc == 1))
            S.copy(V[:, j, :], ps, e="act" if j % 2 else "dve")
        A.free("mla_wkv", "mla_wv", "mla_ckvn")
        if mstop(4):
            return
        sc = 192.0 ** -0.5
        rd = A.alloc("mla_rd", [128, 512], F32)
        items = [(h, hf, j) for h in range(4) for hf in range(2) for j in range(8 + (hf + 1) * 4)]

        def q0_of(hf, j):
            return 0 if j < 8 else max((j - 8) * 128 - hf * 512, 0)

        def scores(it):
            h, hf, j = it
            q0 = q0_of(hf, j)
            qs = slice(hf * 512 + q0, (hf + 1) * 512)
            ps = PS()
            S.mm(ps[:, q0:512], KT[:, h, j * 128:(j + 1) * 128], qT[:, h, qs], start=True, stop=False)
            S.mm(ps[:, q0:512], krall[:, j * 128:(j + 1) * 128], qrT[:, h, qs], start=False, stop=True)
            return ps
        pend = scores(items[0])
        po = pd = None
        for idx, (h, hf, j) in enumerate(items):
            nkt = 8 + (hf + 1) * 4
            jl = j - 8
            q0 = q0_of(hf, j)
            ps = pend
            if idx + 1 < len(items):
                pend = scores(items[idx + 1])
            if j == 0:
                po, pd = PSX(), PSX()
            PT = A.alloc("mla_PT%d" % (idx % 3), [128, 512], BF16)
            if j < 8:
                S.act(PT[:, q0:512], ps[:, q0:512], AF.Exp, scale=sc, bias=nb)
            else:
                S.act(PT[:, q0:512], ps[:, q0:512], AF.Exp, scale=sc)
                if jl * 128 >= hf * 512:
                    S.tt(PT[:, q0:q0 + 128], PT[:, q0:q0 + 128], tri_b, ALU.mult, e="pool")
            S.mm(po[:, q0:512], V[:, j, h * 128:(h + 1) * 128], PT[:, q0:512], start=(j == 0), stop=(j == nkt - 1))
            S.mm(pd[:, q0:512], ones_b, PT[:, q0:512], start=(j == 0), stop=(j == nkt - 1))
            A.free("mla_PT%d" % (idx % 3))
            if j == nkt - 1:
                S.recip(rd, pd)
                S.tt(yT[:, h, HS[hf]], po, rd, ALU.mult)
        A.free("mla_rd", "mla_KT", "mla_V", "mla_qT", "mla_qrT", "mla_kr")

    GSTOP = int(os.environ.get("GSTOP", "99"))

    def cla(tag, qtT, ktT, khT, Edec, v_tok, Wd, cin, cout, onum):
        nwb = Wd // 128
        if GSTOP <= 3:
            return
        kh_tok = A.alloc(tag + "khtok", [128, 8, 256], BF16)
        for i in range(8):
            psb = PS().bitcast(BF16)
            for fc in range(2):
                S.tr(psb[:, fc * 128:(fc + 1) * 128], khT[:, fc, i * 128:(i + 1) * 128], ident_b)
            S.copy(kh_tok[:, i, :], psb[:, 0:256], e="act" if i % 2 else "dve")
        St = A.alloc(tag + "S", [128, 2, Wd], F32)
        Sb = A.alloc(tag + "Sb", [128, 2, Wd], BF16)
        S.memset(St, 0.0, e="dve")

        def upd(c, fc):
            i, r0 = c // 2, (c % 2) * 64
            ps = PS()
            S.mm(ps[:, 0:2 * Wd], kh_tok[r0:r0 + 64, i, fc * 128:(fc + 1) * 128],
                 v_tok[r0:r0 + 64, i, 2 * fc:2 * fc + 2, :].rearrange("p a b -> p (a b)"))
            for hh in range(2):
                pr = slice(hh * 64, (hh + 1) * 64)
                S.stt(St[pr, fc, :], St[pr, fc, :], Edec[pr, fc, c:c + 1], ps[pr, hh * Wd:(hh + 1) * Wd],
                      ALU.mult, ALU.add)

        if GSTOP <= 4:
            A.free(tag + "khtok", tag + "S", tag + "Sb")
            return
        for c in range(16):
            for fc in range(2):
                upd(c, fc)
        if GSTOP <= 5:
            A.free(tag + "khtok", tag + "S", tag + "Sb")
            return
        for fc in range(2):
            S.dma(cin[fc * 128:(fc + 1) * 128, :], St[:, fc, :])
        S.cc(cout, cin, PAIRS)
        for fc in range(2):
            S.dma(St[:, fc, :], cout[fc * 128:(fc + 1) * 128, :])
        S.ts(St, St, pf, None, op0=ALU.mult)
        S.copy(Sb, St, e="act")
        if GSTOP <= 6:
            A.free(tag + "khtok", tag + "S", tag + "Sb")
            return
        for i in range(8):
            tsl = slice(i * 128, (i + 1) * 128)
            attm = [A.alloc(tag + "attm%d" % fc, [128, 2, 128], BF16) for fc in range(2)]
            for fc in range(2):
                for hh in range(2):
                    pr = slice(hh * 64, (hh + 1) * 64)
                    pa = PS()
                    S.mm(pa[:, 0:128], ktT[pr, fc, tsl], qtT[pr, fc, tsl])
                    S.tt(attm[fc][:, hh, :], pa[:, 0:128], bd_b, ALU.mult)
            po = [[PSX() for hh in range(2)] for fc in range(2)]
            for fc in range(2):
                for hh in range(2):
                    for wb in range(nwb):
                        S.mm(po[fc][hh][:, wb * 128:(wb + 1) * 128], v_tok[:, i, 2 * fc + hh, wb * 128:(wb + 1) * 128],
                             attm[fc][:, hh, :], start=(wb == 0), stop=False)
            for cc in range(2):
                c = 2 * i + cc
                qsl = slice(i * 128 + cc * 64, i * 128 + cc * 64 + 64)
                for fc in range(2):
                    for hh in range(2):
                        pr = slice(hh * 64, (hh + 1) * 64)
                        for wb in range(nwb):
                            S.mm(po[fc][hh][:, wb * 128 + cc * 64: wb * 128 + (cc + 1) * 64],
                                 Sb[pr, fc, wb * 128:(wb + 1) * 128], qtT[pr, fc, qsl], start=False,
                                 stop=(cc == 1 and wb == nwb - 1))
                for fc in range(2):
                    upd(c, fc)
                for fc in range(2):
                    S.copy(Sb[:, fc, :], St[:, fc, :], e="act")
            for fc in range(2):
                for hh in range(2):
                    h = 2 * fc + hh
                    if nwb == 1:
                        S.copy(onum[:, h, tsl], po[fc][hh][:, 0:128], e="dve")
                    else:
                        dd = A.alloc(tag + "dd", [128, 128], F32)
                        S.act(dd, po[fc][hh][:, 128:256], AF.Abs)
                        S.ts(dd, dd, 1.0, None, op0=ALU.max)
                        S.recip(dd, dd)
                        S.tt(onum[:, h, tsl], po[fc][hh][:, 0:128], dd, ALU.mult)
                        A.free(tag + "dd")
            A.free(tag + "attm0", tag + "attm1")
        A.free(tag + "khtok", tag + "S", tag + "Sb")

    def headnorm_gate(onum, gcol, gateT, yT):
        sq = A.alloc("hn_sq", [128, 512], BF16)
        rs = A.alloc("hn_rs", [128, 512], F32)
        for h in range(4):
            for hf in range(2):
                S.act(sq, onum[:, h, HS[hf]], AF.Square)
                ps = PS()
                S.mm(ps, ones_b, sq)
                S.act(rs, ps, AF.Sqrt, bias=epsc, scale=1.0 / 128.0)
                S.recip(rs, rs)
                S.tt(rs, rs, onum[:, h, HS[hf]], ALU.mult)
                S.stt(yT[:, h, HS[hf]], rs, gcol, gateT[:, h, HS[hf]], ALU.mult, ALU.mult)
        A.free("hn_sq", "hn_rs")

    def mixer_gla(l, yT, pcol):
        wb1 = win_blk_c(l, 1728, 2240, "wblk0")
        qk = A.alloc("gla_qk", [128, 4, T], F32)
        for j in range(4):
            proj_fm(wb1, j * 128, 128, lambda ps, hf, j=j: S.copy(qk[:, j, HS[hf]], ps, e="act"))
        A.free("wblk0")
        wb3 = win_blk_c(l, 2752, 3280, "wblk1")
        soT = A.alloc("gla_so", [128, 4, T], BF16)
        for j in range(4):
            proj_fm(wb3, j * 128, 128, lambda ps, hf, j=j: S.act(soT[:, j, HS[hf]], ps, AF.Silu))
        glr = A.alloc("gla_lr", [16, T], BF16)
        proj_fm(wb3, 512, 16, lambda ps, hf: S.copy(glr[:, HS[hf]], ps, e="act"))
        A.free("wblk1")
        wg = A.alloc("gla_wg", [16, 256], BF16)
        wload(wg, W["gla_w_gate"][l])
        nbg = A.alloc("gla_nbg", [128, 2], F32)
        S.ts(nbg, pcol[:, 5:7], -1.0, None, op0=ALU.mult)
        bneg = A.alloc("gla_bneg", [128, 2, T], F32)
        et = A.alloc("gla_et", [128, 2, T], F32)
        for fc in range(2):
            for hf in range(2):
                ps = PS()
                S.mm(ps, wg[:, fc * 128:(fc + 1) * 128], glr[:, HS[hf]])
                S.act(et[:, fc, HS[hf]], ps, AF.Exp, scale=-1.0, bias=nbg[:, fc:fc + 1])
            S.act(et[:, fc, :], et[:, fc, :], AF.Ln, bias=onec)
            S.ts(et[:, fc, :], et[:, fc, :], 1.0 / 16.0, None, op0=ALU.mult)
            S.scan(bneg[:, fc, :], rmask, et[:, fc, :], 0.0, ALU.mult, ALU.add)
        A.free("gla_wg", "gla_nbg", "gla_lr")
        Edec = A.alloc("gla_Edec", [128, 2, 16], F32)
        qtT = A.alloc("gla_qt", [128, 2, T], BF16)
        ktT = A.alloc("gla_kt", [128, 2, T], BF16)
        khT = A.alloc("gla_kh", [128, 2, T], BF16)
        for fc in range(2):
            bl = bneg[:, fc, :].rearrange("p (c t) -> p c t", t=64)[:, :, 63:64]
            S.act(Edec[:, fc, :], bl.rearrange("p c o -> p (c o)"), AF.Exp, scale=-1.0)
            S.act(et[:, fc, :], bneg[:, fc, :], AF.Exp, scale=-1.0)
            S.stt(qtT[:, fc, :], qk[:, fc, :], 0.125, et[:, fc, :], ALU.mult, ALU.mult)
            S.act(et[:, fc, :], bneg[:, fc, :], AF.Exp)
            S.tt(ktT[:, fc, :], qk[:, 2 + fc, :], et[:, fc, :], ALU.mult)
            S.tt(et[:, fc, :].rearrange("p (c t) -> p c t", t=64), bneg[:, fc, :].rearrange("p (c t) -> p c t", t=64),
                 bl.to_broadcast([128, 16, 64]), ALU.subtract)
            S.act(et[:, fc, :], et[:, fc, :], AF.Exp)
            S.tt(khT[:, fc, :], qk[:, 2 + fc, :], et[:, fc, :], ALU.mult)
        A.free("gla_qk", "gla_bneg", "gla_et")
        wb2 = win_blk(l, 2240, 2752, "wblk0")
        v_tok = A.alloc("gla_v", [128, 8, 4, 128], BF16)
        for i in range(8):
            ps = PS()
            for kc in range(16):
                S.mm(ps, hb[:, kc, i * 128:(i + 1) * 128], wb2[:, kc, :], start=(kc == 0), stop=(kc == 15))
            S.copy(v_tok[:, i, :, :].rearrange("p a b -> p (a b)"), ps, e="act" if i % 2 else "dve")
        A.free("wblk0")
        onum = A.alloc("gla_on", [128, 4, T], F32)
        cla("gla_", qtT, ktT, khT, Edec, v_tok, 128, cin_g, cout_g, onum)
        A.free("gla_qt", "gla_kt", "gla_kh", "gla_v", "gla_Edec")
        headnorm_gate(onum, pcol[:, 7:8], soT, yT)
        A.free("gla_on", "gla_so")

    def mixer_ml(l, yT, pcol):
        wbA = win_blk_c(l, 3280, 3792, "wblk0")
        mqk = A.alloc("ml_mqk", [128, 4, T + 4], F32)
        for j in range(4):
            proj_fm(wbA, j * 128, 128, lambda ps, hf, j=j: S.copy(mqk[:, j, 3 + hf * 512: 3 + (hf + 1) * 512], ps, e="act"))
        A.free("wblk0")
        for j in range(4):
            S.dma(cin_tail[:, j * 4:(j + 1) * 4], mqk[:, j, T - 1:T + 3])
        S.cc(cout_tail, cin_tail, PAIRS)
        for j in range(4):
            S.dma(mqk[:, j, 0:3], cout_tail[0:128, j * 4 + 1:j * 4 + 4])
        for j in range(4):
            S.ts(mqk[:, j, 0:3], mqk[:, j, 0:3], pf, None, op0=ALU.mult)
        cv = A.alloc("ml_cv", [128, 4, T], F32)
        for j in range(4):
            S.ts(cv[:, j, :], mqk[:, j, 3:3 + T], pcol[:, 9 + 12 + j: 9 + 12 + j + 1], pcol[:, 25 + j:25 + j + 1],
                 op0=ALU.mult, op1=ALU.add, e="dve" if j % 2 else "pool")
            for k in range(3):
                S.stt(cv[:, j, :], mqk[:, j, k:k + T], pcol[:, 9 + 4 * k + j: 9 + 4 * k + j + 1], cv[:, j, :],
                      ALU.mult, ALU.add)
            S.act(cv[:, j, :], cv[:, j, :], AF.Silu)
        A.free("ml_mqk")
        wbC = win_blk_c(l, 4304, 4824, "wblk1")
        soT = A.alloc("ml_so", [128, 4, T], BF16)
        for j in range(4):
            proj_fm(wbC, j * 128, 128, lambda ps, hf, j=j: S.act(soT[:, j, HS[hf]], ps, AF.Sigmoid))
        gb = A.alloc("ml_gb", [4, 4], F32)
        S.dma(gb[:, 0:2], W["ml_gate_b"][l].rearrange("(two h) -> h two", two=2), allow_slow_non_contiguous=True)
        S.ts(gb[:, 2:3], gb[:, 1:2], -1.0, None, op0=ALU.mult)
        r_b = A.alloc("ml_rb", [4, T], F32)
        r_t = A.alloc("ml_rt", [4, T], F32)
        r_e = A.alloc("ml_re", [4, T], F32)
        for hf in range(2):
            ps = proj_ps(wbC, 512, 4, hf)
            S.act(r_t[:, HS[hf]], ps, AF.Identity, bias=gb[:, 0:1])
            ps = proj_ps(wbC, 516, 4, hf)
            S.act(r_e[:, HS[hf]], ps, AF.Exp, scale=-1.0, bias=gb[:, 2:3])
        A.free("wblk1")
        S.act(r_e, r_e, AF.Ln, bias=onec[0:4, :])
        S.scan(r_b, rmask[0:4, :], r_e, 0.0, ALU.mult, ALU.add)
        S.tt(r_t, r_t, r_b, ALU.add)
        bl = r_b.rearrange("p (c t) -> p c t", t=64)[:, :, 63:64]
        Edec = A.alloc("ml_Edec", [128, 2, 16], F32)
        rd = A.alloc("ml_rd", [4, 16], F32)
        S.act(rd, bl.rearrange("p c o -> p (c o)"), AF.Exp, scale=-1.0)
        for fc in range(2):
            ps = PS()
            S.mm(ps[:, 0:16], sel_f[:, fc, :], rd)
            S.copy(Edec[:, fc, :], ps[:, 0:16])
        qtT = A.alloc("ml_qt", [128, 2, T], BF16)
        ktT = A.alloc("ml_kt", [128, 2, T], BF16)
        khT = A.alloc("ml_kh", [128, 2, T], BF16)

        def bc_mul(row, dst, src, scale=None):
            for fc in range(2):
                for hf in range(2):
                    ps = PS()
                    S.mm(ps, sel_f[:, fc, :], row[:, HS[hf]])
                    if scale is None:
                        S.tt(dst[:, fc, HS[hf]], ps, src[:, fc, HS[hf]], ALU.mult)
                    else:
                        S.stt(dst[:, fc, HS[hf]], src[:, fc, HS[hf]], scale, ps, ALU.mult, ALU.mult)

        S.act(r_e, r_b, AF.Exp, scale=-1.0)
        bc_mul(r_e, qtT, cv[:, 0:2, :], scale=0.125)
        S.act(r_e, r_t, AF.Exp)
        bc_mul(r_e, ktT, cv[:, 2:4, :])
        S.tt(r_e.rearrange("p (c t) -> p c t", t=64), r_t.rearrange("p (c t) -> p c t", t=64),
             bl.to_broadcast([4, 16, 64]), ALU.subtract)
        S.act(r_e, r_e, AF.Exp)
        bc_mul(r_e, khT, cv[:, 2:4, :])
        A.free("ml_cv", "ml_rb", "ml_rt", "ml_re", "ml_rd", "ml_gb")
        wbB = win_blk(l, 3792, 4304, "wblk0")
        v_tok = A.alloc("ml_v", [128, 8, 4, 256], BF16)
        for i in range(8):
            S.memset(v_tok[:, i, :, 128:256], 1.0, e="pool")
            ps = PS()
            for kc in range(16):
                S.mm(ps, hb[:, kc, i * 128:(i + 1) * 128], wbB[:, kc, :], start=(kc == 0), stop=(kc == 15))
            S.copy(v_tok[:, i, :, 0:128], ps.rearrange("p (a b) -> p a b", a=4), e="act" if i % 2 else "dve")
        A.free("wblk0")
        onum = A.alloc("ml_on", [128, 4, T], F32)
        cla("ml_", qtT, ktT, khT, Edec, v_tok, 256, cin_m, cout_m, onum)
        A.free("ml_qt", "ml_kt", "ml_kh", "ml_v", "ml_Edec")
        headnorm_gate(onum, pcol[:, 8:9], soT, yT)
        A.free("ml_on", "ml_so")

    def pipeline(steps):
        n = len(steps)
        hs_ = [None] * n
        if n:
            hs_[0] = steps[0][0](0)
        for s in range(n):
            if s + 1 < n:
                hs_[s + 1] = steps[s + 1][0]((s + 1) % 2)
            steps[s][1](hs_[s])
            A.free(*steps[s][2](s % 2))

    def resid_evac(dt):
        return lambda ps, hf: S.stt(hT[:, dt, HS[hf]], hT[:, dt, HS[hf]], ALPHA, ps, ALU.mult, ALU.add)

    def merge_out(l, y):
        mT = A.alloc("mT", [128, 16, T], BF16)
        macc = A.alloc("macc", [128, 2, T], F32)
        steps = []
        for G in range(8):
            for n in range(4):
                def load(slot, G=G, n=n):
                    gw = A.alloc("gw%d" % slot, [128, 16, 256], BF16)
                    c0 = 4824 + n * 2048 + G * 256
                    wload(gw, W["w_in"][l, :, c0:c0 + 256].rearrange("(kc p) n -> p kc n", p=128))
                    bw = A.alloc("bw%d" % slot, [128, 4, 256], BF16)
                    wload(bw, W["w_branch"][l, n, :, G * 256:(G + 1) * 256].rearrange("(kc p) n -> p kc n", p=128))
                    return gw, bw

                def comp(hd, G=G, n=n):
                    gw, bw = hd
                    for dt in range(2):
                        for hf in range(2):
                            pg = proj_ps(gw, dt * 128, 128, hf)
                            pp = proj_ps(bw, dt * 128, 128, hf, kcn=4, rhs=y[n])
                            sg = A.alloc("mg_sg", [128, 512], F32)
                            S.act(sg, pg, AF.Sigmoid)
                            if n == 0:
                                S.tt(macc[:, dt, HS[hf]], sg, pp, ALU.mult)
                            else:
                                S.tt(sg, sg, pp, ALU.mult)
                                S.tt(macc[:, dt, HS[hf]], macc[:, dt, HS[hf]], sg, ALU.add, e="pool")
                            A.free("mg_sg")
                    if n == 3:
                        S.copy(mT[:, 2 * G:2 * G + 2, :], macc, e="act")
                steps.append((load, comp, lambda slot: ("gw%d" % slot, "bw%d" % slot)))
        pipeline(steps)
        A.free("macc", "yA", "yB", "yC", "yD")
        steps = []
        for blk in range(4):
            def load(slot, blk=blk):
                wo = A.alloc("wo%d" % slot, [128, 16, 512], BF16)
                wload(wo, W["w_out"][l, :, blk * 512:(blk + 1) * 512].rearrange("(kc p) n -> p kc n", p=128))
                return wo

            def comp(wo, blk=blk):
                for j in range(4):
                    proj_fm(wo, j * 128, 128, resid_evac(blk * 4 + j), rhs=mT)
            steps.append((load, comp, lambda slot: ("wo%d" % slot,)))
        pipeline(steps)
        A.free("mT")

    def xattn(l):
        memT = A.alloc("xa_memT", [128, 16, 256], BF16)
        for i in range(2):
            mt = A.alloc("xa_mt", [128, D], F32)
            S.dma(mt, mem_d[i * 128:(i + 1) * 128, :])
            for c4 in range(4):
                ps = PS()
                for j in range(4):
                    S.tr(ps[:, j * 128:(j + 1) * 128], mt[:, (c4 * 4 + j) * 128:(c4 * 4 + j + 1) * 128], ident_f)
                S.copy(memT[:, c4 * 4:(c4 + 1) * 4, i * 128:(i + 1) * 128], ps.rearrange("p (a b) -> p a b", a=4),
                       e="act" if c4 % 2 else "dve")
            A.free("xa_mt")

        def wl(name, src):
            wb = A.alloc(name, [128, 16, 512], BF16)
            wload(wb, src.rearrange("(kc p) n -> p kc n", p=128))
            return wb
        def wlc(name, src):
            wb = A.alloc(name, [128, 4, 16, 128], BF16)
            for ch in range(4):
                wload(wb[:, ch, :, :], src[:, ch * 128:(ch + 1) * 128].rearrange("(kc p) n -> p kc n", p=128))
            return wb
        wq = wlc("xa_wq", W["x_w_q"][l])
        wk = wlc("xa_wk", W["x_w_kv"][l, :, 0:512])
        qT = A.alloc("xa_qT", [128, 4, T], BF16)
        for h in range(4):
            proj_fm(wq, h * 128, 128, lambda ps, hf, h=h: S.copy(qT[:, h, HS[hf]], ps, e="act"))
        A.free("xa_wq")
        wv = wl("xa_wv", W["x_w_kv"][l, :, 512:1024])
        KT = A.alloc("xa_KT", [128, 4, 256], BF16)
        for h in range(4):
            ps = PS()
            for kc in range(16):
                S.mm(ps[:, 0:256], wk[:, h, kc, :], memT[:, kc, :], start=(kc == 0), stop=(kc == 15))
            S.copy(KT[:, h, :], ps[:, 0:256])
        A.free("xa_wk")
        wo = A.alloc("xa_wo", [128, 4, D], BF16)
        wload(wo, W["x_w_o"][l].rearrange("(kc p) n -> p kc n", p=128))
        V = A.alloc("xa_V", [128, 2, 512], BF16)
        for jc in range(2):
            ps = PS()
            for kc in range(16):
                S.mm(ps, memT[:, kc, jc * 128:(jc + 1) * 128], wv[:, kc, :], start=(kc == 0), stop=(kc == 15))
            S.copy(V[:, jc, :], ps, e="act")
        A.free("xa_wv", "xa_memT")
        oT = A.alloc("xa_oT", [128, 4, T], BF16)
        rd = A.alloc("xa_rd", [128, 512], F32)
        sc = 128.0 ** -0.5
        for h in range(4):
            for hf in range(2):
                po, pd = PSX(), PSX()
                for jc in range(2):
                    ps = PS()
                    S.mm(ps, KT[:, h, jc * 128:(jc + 1) * 128], qT[:, h, HS[hf]])
                    PT = A.alloc("xa_PT%d" % jc, [128, 512], BF16)
                    S.act(PT, ps, AF.Exp, scale=sc)
                    S.mm(po, V[:, jc, h * 128:(h + 1) * 128], PT, start=(jc == 0), stop=(jc == 1))
                    S.mm(pd, ones_b, PT, start=(jc == 0), stop=(jc == 1))
                    A.free("xa_PT%d" % jc)
                S.recip(rd, pd)
                S.tt(oT[:, h, HS[hf]], po, rd, ALU.mult)
        A.free("xa_rd", "xa_KT", "xa_V", "xa_qT")
        for dt in range(16):
            proj_fm(wo, dt * 128, 128, resid_evac(dt), kcn=4, rhs=oT)
        A.free("xa_wo", "xa_oT")

    def moe(l):
        wr = A.alloc("mo_wr", [128, 16, 16], F32)
        S.dma(wr, W["w_router"].rearrange("(kc p) e -> p kc e", p=128))
        rb = bcast_load("mo_rb", W["router_bias"], 16)
        aff = A.alloc("mo_aff", [128, 8, 16], F32)
        for i in range(8):
            ps = PS()
            for kc in range(16):
                S.mm(ps[:, 0:16], hT[:, kc, i * 128:(i + 1) * 128], wr[:, kc, :], start=(kc == 0), stop=(kc == 15))
            S.act(aff[:, i, :], ps[:, 0:16], AF.Sigmoid)
        Bt = A.alloc("mo_B", [128, 8, 16], F32)
        B2 = A.alloc("mo_B2", [128, 8, 16], F32)
        eq = A.alloc("mo_eq", [128, 8, 16], F32)
        m1 = A.alloc("mo_m1", [128, 32, 1], F32)
        m2 = A.alloc("mo_m2", [128, 32, 1], F32)
        gm = A.alloc("mo_gm", [128, 8, 1], F32)

        def g4(t):
            return t.rearrange("p a (g e) -> p (a g) e", e=4)

        def red(out, in_, op):
            S.add("dve", lambda g: g.tensor_reduce(out, in_, AX.X, op), [in_], [out])
        S.tt(Bt, aff, rb[:, None, :].to_broadcast([128, 8, 16]), ALU.add)
        red(m1.rearrange("p a o -> p (a o)"), g4(Bt), ALU.max)
        S.tt(g4(eq), g4(Bt), m1.to_broadcast([128, 32, 4]), ALU.is_equal)
        S.stt(B2, eq, -1.0e9, Bt, ALU.mult, ALU.add)
        red(m2.rearrange("p a o -> p (a o)"), g4(B2), ALU.max)
        S.tt(m1, m1, m2, ALU.add)
        red(gm.rearrange("p a o -> p (a o)"), m1.rearrange("p (a g) o -> p a (g o)", g=4), ALU.max)
        S.tt(m1.rearrange("p (a g) o -> p a (g o)", g=4), m1.rearrange("p (a g) o -> p a (g o)", g=4),
             gm.to_broadcast([128, 8, 4]), ALU.is_equal)
        S.tt(g4(eq), g4(Bt), m2.to_broadcast([128, 32, 4]), ALU.is_ge)
        S.tt(g4(eq), g4(eq), m1.to_broadcast([128, 32, 4]), ALU.mult)
        S.tt(eq, eq, aff, ALU.mult)
        red(gm.rearrange("p a o -> p (a o)"), eq, ALU.add)
        S.recip(gm, gm)
        S.tt(eq, eq, gm.to_broadcast([128, 8, 16]), ALU.mult)
        gT = A.alloc("mo_gT", [16, T], F32)
        for i4 in range(2):
            ps = PS()
            for j in range(4):
                S.tr(ps[0:16, j * 128:(j + 1) * 128], eq[:, i4 * 4 + j, :], ident_f)
            S.copy(gT[:, i4 * 512:(i4 + 1) * 512], ps[0:16, :])
        A.free("mo_wr", "mo_rb", "mo_aff", "mo_B", "mo_B2", "mo_eq", "mo_m1", "mo_m2", "mo_gm")
        hid = A.alloc("mo_hid", [128, 4, T], BF16)
        gbc = A.alloc("mo_gbc", [128, T], F32)
        selt = A.alloc("mo_sel", [16, 128], F32)
        steps = []
        for e in range(16):
            for fp in range(2):
                def load(slot, e=e, fp=fp):
                    wg = A.alloc("mo_wg%d" % slot, [128, 16, 256], BF16)
                    wload(wg, W["moe_w_gate"][l, e, :, fp * 256:(fp + 1) * 256].rearrange("(kc p) n -> p kc n", p=128))
                    wu = A.alloc("mo_wu%d" % slot, [128, 16, 256], BF16)
                    wload(wu, W["moe_w_up"][l, e, :, fp * 256:(fp + 1) * 256].rearrange("(kc p) n -> p kc n", p=128))
                    return wg, wu

                def comp(hd, e=e, fp=fp):
                    wg, wu = hd
                    if fp == 0:
                        S.copy(selt, ident_f[0:16, e:e + 1].to_broadcast([16, 128]))
                        for hf in range(2):
                            ps = PS()
                            S.mm(ps, selt, gT[:, HS[hf]])
                            S.copy(gbc[:, HS[hf]], ps, e="act")
                    for j in range(2):
                        for hf in range(2):
                            pg = proj_ps(wg, j * 128, 128, hf)
                            pu = proj_ps(wu, j * 128, 128, hf)
                            sg = A.alloc("mo_sg", [128, 512], F32)
                            S.act(sg, pg, AF.Silu)
                            S.tt(sg, sg, pu, ALU.mult)
                            S.tt(hid[:, fp * 2 + j, HS[hf]], sg, gbc[:, HS[hf]], ALU.mult, e="pool")
                            A.free("mo_sg")
                steps.append((load, comp, lambda slot: ("mo_wg%d" % slot, "mo_wu%d" % slot)))
            for dh in range(2):
                def load(slot, e=e, dh=dh):
                    wd = A.alloc("mo_wd%d" % slot, [128, 4, 1024], BF16)
                    wload(wd, W["moe_w_down"][l, e, :, dh * 1024:(dh + 1) * 1024].rearrange("(kc p) n -> p kc n", p=128))
                    return wd

                def comp(wd, e=e, dh=dh):
                    for j in range(8):
                        dt = dh * 8 + j
                        proj_fm(wd, j * 128, 128,
                                (resid_evac(dt) if e == 0 else
                                 (lambda ps, hf, dt=dt: S.tt(hT[:, dt, HS[hf]], hT[:, dt, HS[hf]], ps, ALU.add))),
                                kcn=4, rhs=hid)
                steps.append((load, comp, lambda slot: ("mo_wd%d" % slot,)))
        pipeline(steps)
        A.free("mo_hid", "mo_gbc", "mo_sel", "mo_gT")

    for l in range(depth):
        pcol = layer_params(l)
        y = [None] * 4
        y[1] = A.alloc("yB", [128, 4, T], BF16)
        mixer_mla(l, y[1], pcol)
        dump("y_b", y[1])
        y[2] = A.alloc("yC", [128, 4, T], BF16)
        mixer_gla(l, y[2], pcol)
        dump("y_c", y[2])
        y[3] = A.alloc("yD", [128, 4, T], BF16)
        mixer_ml(l, y[3], pcol)
        dump("y_d", y[3])
        y[0] = A.alloc("yA", [128, 4, T], BF16)
        mixer_sg(l, y[0])
        dump("y_a", y[0])
        A.free("pcol")
        merge_out(l, y)
        if l == 0:
            dump("pre1", hT)
        ln_fm(1 + 3 * l)
        if l == 0:
            dump("h1", hT)
        xattn(l)
        ln_fm(2 + 3 * l)
        if l == 0:
            dump("h2", hT)
        moe(l)
        ln_fm(3 + 3 * l)

    def write_out():
        for i in range(8):
            ot = A.alloc("ot%d" % (i % 2), [128, D], F32)
            for c4 in range(4):
                ps = PS()
                for j in range(4):
                    c = c4 * 4 + j
                    S.tr(ps[:, j * 128:(j + 1) * 128], hT[:, c, i * 128:(i + 1) * 128], ident_f)
                S.copy(ot[:, c4 * 512:(c4 + 1) * 512], ps, e="act" if c4 % 2 else "dve")
            S.dma(out_d[i * 128:(i + 1) * 128, :], ot, e="sp" if i % 2 == 0 else "act")
            A.free("ot%d" % (i % 2))
    write_out()
    S.finish()
    S.final_wait()
    nc._keep = (st, S)
    nc._depth = depth
    return nc, list(W.keys()), dump_d


_CACHE = {}


def kernel(**inputs):
    depth = DEPTH
    if "prog" not in _CACHE:
        _CACHE["prog"] = build(depth)
    nc, wnames, _ = _CACHE["prog"]
    return run_prog(nc, wnames, inputs)[0]


def run_prog(nc, wnames, inputs, dump_names=()):
    cst = make_consts()
    x = np.ascontiguousarray(inputs["x"], dtype=np.float32)
    mem = np.ascontiguousarray(inputs["mem"], dtype=np.float32)
    pos = np.ascontiguousarray(inputs["positions"], dtype=np.int32)
    in_maps = []
    depth = nc._depth
    wcache = {}
    for k in wnames:
        a = inputs[k]
        if k in LAYERED and depth < 4:
            a = a[:depth]
        wcache[k] = np.ascontiguousarray(a, dtype=np.float32)
    for c in range(8):
        b, hf = divmod(c, 2)
        m = {"x": x[b, hf * T:(hf + 1) * T], "mem": mem[b], "pos": pos[b:b + 1, hf * T:(hf + 1) * T],
             "pflag": np.full((128, 1), float(hf), np.float32), "cst": cst}
        for k in wnames:
            m[k] = wcache[k]
        in_maps.append(m)
    res = run_bass_kernel_spmd(nc, in_maps, core_ids=list(range(8)))
    out = np.empty((4, 2 * T, D), np.float32)
    for c in range(8):
        b, hf = divmod(c, 2)
        out[b, hf * T:(hf + 1) * T] = res.results[c]["out"]
    dumps = {n: [res.results[c]["dbg_" + n] for c in range(8)] for n in dump_names}
    return out, dumps
```

```python
import math
from concourse.bass_utils import run_bass_kernel_spmd
import numpy as np
import concourse.bass as bass
import concourse.mybir as mybir

F32 = mybir.dt.float32
BF16 = mybir.dt.bfloat16
I32 = mybir.dt.int32
AF = mybir.ActivationFunctionType
ALU = mybir.AluOpType
AX = mybir.AxisListType


def _region(ap):
    t = ap.tensor
    esz = mybir.dt.size(ap.dtype)
    pat = ap.ap
    off = ap.offset
    space = str(ap.space)
    if space == "DRAM" or "DRAM" in space.upper() or "HBM" in space.upper():
        lo = off
        hi = off + sum((c - 1) * abs(s) for s, c in pat) + 1
        return (t.name, 0, 1, lo * esz, hi * esz)
    pstride, pcount = pat[0]
    if pstride == 0:
        pstride = 1 << 40
    p0 = off // pstride if pstride < (1 << 40) else 0
    lo = off - p0 * pstride if pstride < (1 << 40) else off
    hi = lo + sum((c - 1) * abs(s) for s, c in pat[1:]) + 1
    if space == "PSUM":
        return (t.name, 0, 128, (lo * esz) // 2048 * 2048, ((hi * esz) + 2047) // 2048 * 2048)
    return (t.name, p0, p0 + pcount, lo * esz, hi * esz)


class _Op:
    __slots__ = ("e", "fn", "r", "w", "dma", "deps", "need", "tok", "slot")

    def __init__(self, e, fn, r, w, dma):
        self.e, self.fn, self.r, self.w, self.dma = e, fn, r, w, dma
        self.deps = None
        self.need = False
        self.tok = None
        self.slot = None


class Sched:
    GEN = 20000

    def __init__(self, nc, n_dma_slots=6):
        self.nc = nc
        self.engs = {"pe": nc.tensor, "act": nc.scalar, "dve": nc.vector,
                     "pool": nc.gpsimd, "sp": nc.sync}
        self.ops = []
        self.n_dma_slots = n_dma_slots

    def add(self, e, fn, reads=(), writes=(), dma=False):
        r = [_region(a) for a in reads if a is not None and hasattr(a, "ap")]
        w = [_region(a) for a in writes if a is not None and hasattr(a, "ap")]
        self.ops.append(_Op(e, fn, r, w, dma))

    def mm(self, out, lhsT, rhs, start=True, stop=True, **kw):
        self.add("pe", lambda g: g.matmul(out, lhsT, rhs, start=start, stop=stop, **kw),
                 [lhsT, rhs], [out])

    def tr(self, out, in_, ident):
        self.add("pe", lambda g: g.transpose(out, in_, ident), [in_, ident], [out])

    def act(self, out, in_, func, bias=None, scale=None, accum_out=None, e="act"):
        kw = {}
        if bias is not None:
            kw["bias"] = bias
        if scale is not None:
            kw["scale"] = scale
        if accum_out is not None:
            kw["accum_out"] = accum_out
        self.add(e, lambda g: g.activation(out, in_, func, **kw),
                 [in_, bias, scale], [out, accum_out])

    def tt(self, out, in0, in1, op, e="dve"):
        self.add(e, lambda g: g.tensor_tensor(out, in0, in1, op), [in0, in1], [out])

    def ts(self, out, in0, s1, s2=None, op0=ALU.mult, op1=None, e="dve", accum_out=None):
        kw = {}
        if op1 is not None:
            kw["op1"] = op1
        if accum_out is not None:
            kw["accum_out"] = accum_out
        self.add(e, lambda g: g.tensor_scalar(out, in0, s1, s2, op0, **kw),
                 [in0, s1, s2], [out, accum_out])

    def stt(self, out, in0, scalar, in1, op0, op1, e="dve"):
        self.add(e, lambda g: g.scalar_tensor_tensor(out, in0, scalar, in1, op0, op1),
                 [in0, scalar, in1], [out])

    def copy(self, out, in_, e="dve"):
        if e == "act":
            self.add(e, lambda g: g.copy(out, in_), [in_], [out])
        else:
            self.add(e, lambda g: g.tensor_copy(out, in_), [in_], [out])

    def memset(self, out, val, e="pool"):
        self.add(e, lambda g: g.memset(out, val), [], [out])

    def dma(self, out, in_, e="sp", **kw):
        self.add(e, lambda g: g.dma_start(out=out, in_=in_, **kw), [in_], [out], dma=True)

    def finish(self):
        nc = self.nc
        ops = self.ops
        recs = {}
        for i, op in enumerate(ops):
            deps = set()
            for (name, p0, p1, lo, hi) in op.r:
                for rc in recs.get(name, ()):
                    if rc[5] and rc[0] < p1 and p0 < rc[1] and rc[2] < hi and lo < rc[3]:
                        deps.add(rc[4])
            for (name, p0, p1, lo, hi) in op.w:
                for rc in recs.get(name, ()):
                    if rc[0] < p1 and p0 < rc[1] and rc[2] < hi and lo < rc[3]:
                        deps.add(rc[4])
            deps.discard(i)
            if op.e == "pe":
                deps = {d for d in deps if ops[d].e != "pe" or ops[d].dma}
            op.deps = deps
            for d in deps:
                ops[d].need = True
            for (name, p0, p1, lo, hi) in op.w:
                L = recs.setdefault(name, [])
                L[:] = [rc for rc in L if not (p0 <= rc[0] and rc[1] <= p1 and lo <= rc[2] and rc[3] <= hi)]
                L.append([p0, p1, lo, hi, i, True, op.e])
            for (name, p0, p1, lo, hi) in op.r:
                L = recs.setdefault(name, [])
                if not op.dma:
                    L[:] = [rc for rc in L if not ((not rc[5]) and rc[6] == op.e and not ops[rc[4]].dma
                                                   and p0 <= rc[0] and rc[1] <= p1 and lo <= rc[2] and rc[3] <= hi)]
                L.append([p0, p1, lo, hi, i, False, op.e])
        ticks = {e: 0 for e in self.engs}
        ndma = {e: 0 for e in self.engs}
        ncc = 0
        for op in ops:
            if op.dma == "cc":
                op.tok = ("k", "cc", ncc, 1)
                ncc += 1
            elif op.dma:
                k = ndma[op.e]
                ndma[op.e] += 1
                op.slot = (op.e, k % self.n_dma_slots)
                op.tok = ("d", op.e, k % self.n_dma_slots, 16 * (k // self.n_dma_slots + 1))
            elif op.need:
                ticks[op.e] += 1
                t = ticks[op.e]
                op.tok = ("c", op.e, (t - 1) // self.GEN, (t - 1) % self.GEN + 1)
        self.sems = {}
        import contextlib
        self._stack = contextlib.ExitStack()

        def sem(key):
            if key not in self.sems:
                self.sems[key] = self._stack.enter_context(nc.semaphore("s_%s_%s_%s" % key))
            return self.sems[key]

        waited = {e: {} for e in self.engs}
        last_on_slot = {}
        self.n_waits = 0
        for op in ops:
            g = self.engs[op.e]
            need = {}
            for d in op.deps:
                tk = ops[d].tok
                key = tk[:3]
                if need.get(key, 0) < tk[3]:
                    need[key] = tk[3]
            if op.dma and op.dma != "cc":
                prev = last_on_slot.get(op.slot)
                if prev is not None:
                    key = prev[:3]
                    if need.get(key, 0) < prev[3]:
                        need[key] = prev[3]
            for key, v in need.items():
                if waited[op.e].get(key, 0) < v:
                    g.wait_ge(sem(key), v)
                    waited[op.e][key] = v
                    self.n_waits += 1
            ins = op.fn(g)
            if op.dma == "cc":
                ins.then_inc(sem(op.tok[:3]), 1)
            elif op.dma:
                ins.then_inc(sem(op.tok[:3]), 16)
                last_on_slot[op.slot] = op.tok
            elif op.tok is not None:
                ins.then_inc(sem(op.tok[:3]), 1)
        self.ticks = ticks
        return self

    def wait_all_dma_on(self, e="sp"):
        raise NotImplementedError


def _finish_tail(self, e="sp"):
    g = self.engs[e]
    seen = {}
    for op in self.ops:
        if op.dma:
            seen[op.tok[:3]] = max(seen.get(op.tok[:3], 0), op.tok[3])
    for key, v in seen.items():
        g.wait_ge(self.sems[key], v)


Sched.final_wait = _finish_tail


def _recip(self, out, in_, e="dve"):
    self.add(e, lambda g: g.reciprocal(out, in_), [in_], [out])


def _scan(self, out, d0, d1, init, op0, op1):
    self.add("dve", lambda g: g.tensor_tensor_scan(out, d0, d1, init, op0, op1), [d0, d1, init], [out])


def _bnstats(self, out, in_):
    self.add("dve", lambda g: g.bn_stats(out, in_), [in_], [out])


def _bnaggr(self, out, in_):
    self.add("dve", lambda g: g.bn_aggr(out, in_), [in_], [out])


def _cc(self, out, in_, groups):
    self.add("pool", lambda g: g.collective_compute("AllGather", ALU.bypass, replica_groups=groups,
                                                    ins=[in_], outs=[out]), [in_], [out], dma="cc")


Sched.recip = _recip
Sched.scan = _scan
Sched.bnstats = _bnstats
Sched.bnaggr = _bnaggr
Sched.cc = _cc


class Arena:
    def __init__(self, nc, stack, nbytes):
        self.nb = nbytes
        self.t = stack.enter_context(nc.sbuf_tensor("arena", [128, nbytes // 2], BF16))
        self.free_list = [(0, nbytes)]
        self.live = {}

    def alloc(self, name, shape, dt, parts=None):
        P = shape[0]
        n = 1
        for s in shape[1:]:
            n *= s
        nbytes = n * mybir.dt.size(dt)
        nbytes = (nbytes + 63) // 64 * 64
        for i, (o, sz) in enumerate(self.free_list):
            if sz >= nbytes:
                if sz == nbytes:
                    self.free_list.pop(i)
                else:
                    self.free_list[i] = (o + nbytes, sz - nbytes)
                self.live[name] = (o, nbytes)
                ap = self.t[0:P, o // 2:(o + n * mybir.dt.size(dt)) // 2]
                if dt != BF16:
                    ap = ap.bitcast(dt)
                if len(shape) == 3:
                    ap = ap.rearrange("p (a b) -> p a b", a=shape[1])
                elif len(shape) == 4:
                    ap = ap.rearrange("p (a b c) -> p a b c", a=shape[1], b=shape[2])
                return ap
        raise RuntimeError("arena full: %s %d free=%s" % (name, nbytes, self.free_list))

    def free(self, *names):
        for name in names:
            o, sz = self.live.pop(name)
            self.free_list.append((o, sz))
        self.free_list.sort()
        m = []
        for o, sz in self.free_list:
            if m and m[-1][0] + m[-1][1] == o:
                m[-1] = (m[-1][0], m[-1][1] + sz)
            else:
                m.append((o, sz))
        self.free_list = m


T = 1024
DEPTH = 4
D = 2048
ALPHA = (2.0 * DEPTH) ** 0.25
EPS = 1e-5
PAIRS = [[0, 1], [2, 3], [4, 5], [6, 7]]
NCST = 128 * 3 + 1024 + 256 + 1
ARENA_BYTES = 202 * 1024

PARAM_SHAPES = dict(
    ln_in_g=[2048], ln_in_b=[2048], w_in=[4, 2048, 13016], sg_vnorm_g=[4, 512], sg_vnorm_b=[4, 512],
    sg_w_s=[4, 4, 128, 128], sg_b_s=[4, 4, 128], mla_qnorm_g=[4, 384], mla_kvnorm_g=[4, 256],
    mla_w_uq=[4, 384, 768], mla_w_ukv=[4, 256, 1024], gla_w_gate=[4, 16, 256], gla_b_gate=[4, 256],
    gla_norm_g=[4, 128], ml_conv_w=[4, 4, 512], ml_conv_b=[4, 512], ml_gate_b=[4, 8], ml_norm_g=[4, 128],
    w_branch=[4, 4, 512, 2048], w_out=[4, 2048, 2048], ln1_g=[4, 2048], ln1_b=[4, 2048],
    x_w_q=[4, 2048, 512], x_w_kv=[4, 2048, 1024], x_w_o=[4, 512, 2048], ln2_g=[4, 2048], ln2_b=[4, 2048],
    w_router=[2048, 16], router_bias=[16], moe_w_gate=[4, 16, 2048, 512], moe_w_up=[4, 16, 2048, 512],
    moe_w_down=[4, 16, 512, 2048], ln3_g=[4, 2048], ln3_b=[4, 2048])


LAYERED = [k for k, v in PARAM_SHAPES.items() if v[0] == 4 and len(v) >= 2 and k not in ('w_router',)]


def make_consts():
    c = np.zeros((128, NCST), np.float32)
    p = np.arange(128)[:, None]
    f = np.arange(128)[None, :]
    c[:, 0:128] = (p == f)
    c[:, 128:256] = (p <= f)
    c[:, 256:384] = (p <= f) & ((p // 64) == (f // 64))
    t = np.arange(1024)
    c[:, 384:1408] = (t % 64 != 0)[None, :]
    for k in range(4):
        for fc in range(2):
            for m in range(128):
                c[k, 1408 + fc * 128 + m] = 1.0 if k == 2 * fc + m // 64 else 0.0
    fr = (np.float32(10000.0) ** (-np.arange(32, dtype=np.float32) / np.float32(32))).astype(np.float32)
    c[0:32, 1664] = fr
    c[32:64, 1664] = fr
    return c


def build(depth=DEPTH, stop_after=None, dumps=()):
    import contextlib
    nc = bass.Bass("TRN2", target_bir_lowering=False)

    def din(name, shape, dt=F32):
        return nc.dram_tensor(name, list(shape), dt, kind="ExternalInput").ap()

    x_d = din("x", [T, D])
    mem_d = din("mem", [256, D])
    pos_d = din("pos", [1, T], I32)
    pf_d = din("pflag", [128, 1])
    cst_d = din("cst", [128, NCST])
    class _W(dict):
        def __missing__(self, k):
            shp = list(PARAM_SHAPES[k])
            if k in LAYERED:
                shp[0] = depth
            self[k] = din(k, shp)
            return self[k]
    W = _W()
    out_d = nc.dram_tensor("out", [T, D], F32, kind="ExternalOutput").ap()
    dump_d = {}

    def internal(name, shape, dt=F32):
        return nc.dram_tensor(name, list(shape), dt, kind="Internal").ap()

    cin_tail, cout_tail = internal("cin_tail", [128, 16]), internal("cout_tail", [256, 16])
    cin_kv, cout_kv = internal("cin_kv", [320, T], BF16), internal("cout_kv", [640, T], BF16)
    cin_g, cout_g = internal("cin_g", [256, 128]), internal("cout_g", [512, 128])
    cin_m, cout_m = internal("cin_m", [256, 256]), internal("cout_m", [512, 256])

    S = Sched(nc)
    st = contextlib.ExitStack()
    A = Arena(nc, st, ARENA_BYTES)
    pst = st.enter_context(nc.psum_tensor("ps", [128, 8, 512], F32))
    psn = [0]

    def PS():
        b = psn[0] % 4
        psn[0] += 1
        return pst[:, b, :]

    pxn = [0]

    def PSX():
        b = 4 + pxn[0] % 4
        pxn[0] += 1
        return pst[:, b, :]

    def dump(name, ap):
        if name in dumps:
            dd = nc.dram_tensor("dbg_" + name, list(ap.shape), ap.dtype, kind="ExternalOutput").ap()
            dump_d[name] = dd
            S.dma(dd, ap)

    def wload(dst, src):
        S.dma(dst, src, e="pool")

    HS = [slice(0, 512), slice(512, 1024)]

    hT = A.alloc("hT", [128, 16, T], F32)
    hb = A.alloc("hb", [128, 16, T], BF16)
    ident_f = A.alloc("ident_f", [128, 128], F32)
    ident_b = A.alloc("ident_b", [128, 128], BF16)
    ones_b = A.alloc("ones_b", [128, 128], BF16)
    tri_b = A.alloc("tri_b", [128, 128], BF16)
    bd_b = A.alloc("bd_b", [128, 128], BF16)
    rmask = A.alloc("rmask", [128, T], BF16)
    sel_f = A.alloc("sel_f", [4, 2, 128], F32)
    small = A.alloc("small", [128, 8], F32)
    pf, nb, epsc, freq, onec = (small[:, i:i + 1] for i in range(5))
    lnp = A.alloc("lnp", [128, 512], F32)
    cosT = A.alloc("cosT", [64, T], BF16)
    sinT = A.alloc("sinT", [64, T], BF16)

    cst = A.alloc("cst", [128, NCST], F32)
    S.dma(cst, cst_d)
    S.dma(pf, pf_d)
    S.copy(ident_f, cst[:, 0:128])
    S.copy(ident_b, cst[:, 0:128])
    S.copy(tri_b, cst[:, 128:256])
    S.copy(bd_b, cst[:, 256:384])
    S.copy(rmask, cst[:, 384:1408])
    S.copy(sel_f, cst[0:4, 1408:1664].rearrange("p (a b) -> p a b", a=2))
    S.copy(freq, cst[:, 1664:1665])
    S.memset(ones_b, 1.0, e="dve")
    S.memset(epsc, EPS, e="dve")
    S.memset(onec, 1.0, e="dve")
    S.ts(nb, pf, 30000.0, -30000.0, op0=ALU.mult, op1=ALU.add)

    posi = A.alloc("posi", [64, T], I32)
    S.dma(posi, pos_d.partition_broadcast(64) if False else pos_d[0:1, :].to_broadcast([64, T]))
    ang = A.alloc("ang", [64, T], F32)
    S.copy(ang, posi)
    S.ts(ang, ang, freq[0:64, :], 1.0 / (2.0 * math.pi), op0=ALU.mult, op1=ALU.mult)
    ki = A.alloc("ki", [64, T], I32)
    kf = A.alloc("kf", [64, T], F32)
    for tab, shift in ((sinT, 0.0), (cosT, 0.25)):
        uu = A.alloc("uu", [64, T], F32)
        S.ts(uu, ang, shift, None, op0=ALU.add)
        S.copy(ki, uu)
        S.copy(kf, ki)
        S.tt(uu, uu, kf, ALU.subtract)
        S.ts(kf, uu, 0.5, None, op0=ALU.is_gt)
        S.tt(uu, uu, kf, ALU.subtract)
        S.ts(kf, uu, -0.5, None, op0=ALU.is_lt)
        S.tt(uu, uu, kf, ALU.add)
        S.act(tab, uu, AF.Sin, scale=2.0 * math.pi)
        A.free("uu")
    A.free("posi", "ang", "ki", "kf")

    rows = A.alloc("rows", [128, 4, 128], F32)
    S.memset(rows, 0.0, e="dve")

    def ln_src(li, which):
        if li == 0:
            return W["ln_in_" + which]
        l, j = divmod(li - 1, 3)
        return W["ln%d_%s" % (j + 1, which)][l]
    for wi, which in enumerate("gb"):
        for li in range(1 + 3 * depth):
            q = li * 16
            slot, r = divmod(q, 128)
            S.dma(rows[r:r + 16, 2 * wi + slot, :], ln_src(li, which).rearrange("(c p) -> c p", p=128))
    ps = PS()
    for s4 in range(4):
        S.tr(ps[:, s4 * 128:(s4 + 1) * 128], rows[:, s4, :], ident_f)
    S.copy(lnp, ps)
    A.free("rows")
    A.free("cst")

    def lng(li, c):
        return lnp[:, li * 16 + c: li * 16 + c + 1]

    def lnb(li, c):
        return lnp[:, 256 + li * 16 + c: 256 + li * 16 + c + 1]

    for i in range(8):
        xt = A.alloc("xt%d" % (i % 2), [128, D], F32)
        S.dma(xt, x_d[i * 128:(i + 1) * 128, :], e="sp" if i % 2 == 0 else "act")
        for c4 in range(4):
            ps = PS()
            for j in range(4):
                c = c4 * 4 + j
                S.tr(ps[:, j * 128:(j + 1) * 128], xt[:, c * 128:(c + 1) * 128], ident_f)
            S.copy(hT[:, c4 * 4:(c4 + 1) * 4, i * 128:(i + 1) * 128],
                   ps.rearrange("p (a b) -> p a b", a=4), e="act" if c4 % 2 else "dve")
        A.free("xt%d" % (i % 2))

    def ln_fm(li):
        sq = A.alloc("ln_sq", [128, 16, 512], BF16)
        mean = A.alloc("ln_mean", [128, 1, 512], F32)
        rstd = A.alloc("ln_rstd", [128, 1, 512], F32)
        m2 = A.alloc("ln_m2", [128, 512], F32)
        for hf in range(2):
            hs = HS[hf]
            S.copy(hb[:, :, hs], hT[:, :, hs], e="dve")
            S.act(sq, hT[:, :, hs], AF.Square)
            pm, pq = PS(), PS()
            for c in range(16):
                S.mm(pm, ones_b, hb[:, c, hs], start=(c == 0), stop=(c == 15))
            for c in range(16):
                S.mm(pq, ones_b, sq[:, c, :], start=(c == 0), stop=(c == 15))
            S.act(mean[:, 0, :], pm, AF.Identity, scale=1.0 / D)
            S.tt(m2, mean[:, 0, :], mean[:, 0, :], ALU.mult)
            S.stt(m2, pq, 1.0 / D, m2, ALU.mult, ALU.subtract)
            S.act(m2, m2, AF.Sqrt, bias=epsc)
            S.recip(rstd[:, 0, :], m2)
            for (c0, c1, eng) in ((0, 11, "dve"), (11, 16, "pool")):
                S.tt(hT[:, c0:c1, hs], hT[:, c0:c1, hs], mean.to_broadcast([128, c1 - c0, 512]), ALU.subtract, e=eng)
                S.tt(hT[:, c0:c1, hs], hT[:, c0:c1, hs], rstd.to_broadcast([128, c1 - c0, 512]), ALU.mult, e=eng)
            for c in range(16):
                S.act(hb[:, c, hs], hT[:, c, hs], AF.Identity, scale=lng(li, c), bias=lnb(li, c))
                S.ts(hT[:, c, hs], hT[:, c, hs], lng(li, c), lnb(li, c), op0=ALU.mult, op1=ALU.add)
        A.free("ln_sq", "ln_mean", "ln_rstd", "ln_m2")

    ln_fm(0)
    dump("h0", hT)

    def win_blk(l, c0, c1, name):
        wb = A.alloc(name, [128, 16, c1 - c0], BF16)
        wload(wb, W["w_in"][l, :, c0:c1].rearrange("(kc p) n -> p kc n", p=128))
        return wb

    def win_blk_c(l, c0, c1, name):
        n = c1 - c0
        nch = (n + 127) // 128
        wb = A.alloc(name, [128, nch, 16, 128], BF16)
        for ch in range(nch):
            w = min(128, n - ch * 128)
            wload(wb[:, ch, :, 0:w], W["w_in"][l, :, c0 + ch * 128:c0 + ch * 128 + w].rearrange("(kc p) n -> p kc n", p=128))
        return wb

    def wsl(wb, kc, col0, ncols):
        if len(wb.shape) == 4:
            return wb[:, col0 // 128, kc, col0 % 128: col0 % 128 + ncols]
        return wb[:, kc, col0:col0 + ncols]

    def proj_fm(wb, col0, ncols, evac, kcn=16, rhs=None):
        rhs = hb if rhs is None else rhs
        for hf in range(2):
            ps = PS()
            for kc in range(kcn):
                S.mm(ps[0:ncols, :], wsl(wb, kc, col0, ncols), rhs[:, kc, HS[hf]],
                     start=(kc == 0), stop=(kc == kcn - 1))
            evac(ps[0:ncols, :], hf)

    def bcast_load(name, src1d, n):
        t = A.alloc(name, [128, n], F32)
        S.dma(t, src1d.rearrange("(o n) -> o n", o=1).to_broadcast([128, n]))
        return t

    def layer_params(l):
        rows = A.alloc("prow", [32, 128], F32)
        S.memset(rows, 0.0, e="dve")
        srcs = [(W["mla_qnorm_g"][l], 3), (W["mla_kvnorm_g"][l], 2), (W["gla_b_gate"][l], 2),
                (W["gla_norm_g"][l], 1), (W["ml_norm_g"][l], 1)]
        r = 0
        for src, n in srcs:
            S.dma(rows[r:r + n, :], src.rearrange("(c p) -> c p", p=128))
            r += n
        S.dma(rows[9:25, :], W["ml_conv_w"][l].rearrange("k (c p) -> (k c) p", p=128))
        S.dma(rows[25:29, :], W["ml_conv_b"][l].rearrange("(c p) -> c p", p=128))
        ps = PS()
        S.tr(ps[:, 0:32], rows, ident_f[0:32, 0:32])
        pc = A.alloc("pcol", [128, 32], F32)
        S.copy(pc, ps[:, 0:32])
        A.free("prow")
        return pc

    def mixer_sg(l, yT):
        wu = win_blk_c(l, 0, 512, "wblk0")
        wv = win_blk(l, 512, 1024, "wblk1")
        uT = A.alloc("sg_uT", [128, 4, T], BF16)
        for j in range(4):
            proj_fm(wu, j * 128, 128, lambda ps, hf, j=j: S.act(uT[:, j, HS[hf]], ps, AF.Gelu_apprx_tanh))
        vg = bcast_load("sg_vg", W["sg_vnorm_g"][l], 512)
        vb = bcast_load("sg_vb", W["sg_vnorm_b"][l], 512)
        bsr = bcast_load("sg_bs", W["sg_b_s"][l].rearrange("g t -> (g t)"), 512)
        ws = A.alloc("sg_ws", [128, 4, 128], F32)
        S.dma(ws, W["sg_w_s"][l].rearrange("g t s -> t g s"))
        wT = A.alloc("sg_wT", [128, 4, 128], BF16)
        ps = PS()
        for g in range(4):
            S.tr(ps[:, g * 128:(g + 1) * 128], ws[:, g, :], ident_f)
        S.tt(wT, ps.rearrange("p (a b) -> p a b", a=4), tri_b[:, None, :].to_broadcast([128, 4, 128]), ALU.mult)
        A.free("sg_ws")
        st6 = A.alloc("sg_st", [128, 8], F32)
        mv = A.alloc("sg_mv", [128, 4], F32)
        for i in range(8):
            ts_ = slice(i * 128, (i + 1) * 128)
            ps = PS()
            for kc in range(16):
                S.mm(ps, hb[:, kc, ts_], wv[:, kc, :], start=(kc == 0), stop=(kc == 15))
            vt = A.alloc("sg_vt", [128, 512], F32)
            S.act(vt, ps, AF.Gelu_apprx_tanh)
            S.bnstats(st6[:, 0:6], vt)
            S.bnaggr(mv[:, 0:2], st6[:, 0:6])
            S.act(mv[:, 2:3], mv[:, 1:2], AF.Sqrt, bias=epsc)
            S.recip(mv[:, 2:3], mv[:, 2:3])
            S.stt(mv[:, 3:4], mv[:, 0:1], -1.0, mv[:, 2:3], ALU.mult, ALU.mult)
            S.act(vt, vt, AF.Identity, scale=mv[:, 2:3], bias=mv[:, 3:4])
            S.tt(vt, vt, vg, ALU.mult)
            vnb = A.alloc("sg_vnb", [128, 512], BF16)
            S.tt(vnb, vt, vb, ALU.add)
            ps2 = PS()
            for g in range(4):
                S.mm(ps2[:, g * 128:(g + 1) * 128], vnb[:, g * 128:(g + 1) * 128], wT[:, g, :])
            mx = A.alloc("sg_mx", [128, 4, 128], F32)
            S.tt(mx, ps2.rearrange("p (a b) -> p a b", a=4), bsr.rearrange("p (a b) -> p a b", a=4), ALU.add)
            S.tt(yT[:, :, ts_], mx, uT[:, :, ts_], ALU.mult)
            A.free("sg_vt", "sg_vnb", "sg_mx")
        A.free("wblk0", "wblk1", "sg_uT", "sg_vg", "sg_vb", "sg_bs", "sg_wT", "sg_st", "sg_mv")

    def proj_ps(wb, col0, ncols, hf, kcn=16, rhs=None, ps=None):
        rhs = hb if rhs is None else rhs
        ps = PS() if ps is None else ps
        for kc in range(kcn):
            S.mm(ps[0:ncols, :], wsl(wb, kc, col0, ncols), rhs[:, kc, HS[hf]],
                 start=(kc == 0), stop=(kc == kcn - 1))
        return ps[0:ncols, :]

    def rms_fm(src, dst, nch, gcols, width, dsl=None):
        sq = A.alloc("rms_sq", [128, nch, 512], BF16)
        rs = A.alloc("rms_rs", [128, 512], F32)
        for hf in range(2):
            S.tt(sq, src[:, :, HS[hf]], src[:, :, HS[hf]], ALU.mult, e="pool")
            ps = PS()
            for j in range(nch):
                S.mm(ps, ones_b, sq[:, j, :], start=(j == 0), stop=(j == nch - 1))
            S.act(rs, ps, AF.Sqrt, bias=epsc, scale=1.0 / width)
            S.recip(rs, rs)
            for j in range(nch):
                d = dst[:, j, HS[hf]] if dsl is None else dst[:, j, dsl + hf * 512: dsl + (hf + 1) * 512]
                S.stt(d, src[:, j, HS[hf]], gcols[:, j:j + 1], rs, ALU.mult, ALU.mult)
        A.free("rms_sq", "rms_rs")

    def rope_evac(ps, psr, dst, hf):
        t1 = A.alloc("rp_t1", [64, 512], F32)
        t2 = A.alloc("rp_t2", [64, 512], F32)
        S.tt(t1, psr, sinT[:, HS[hf]], ALU.mult)
        S.tt(t2, ps, cosT[:, HS[hf]], ALU.mult)
        S.tt(dst, t1, t2, ALU.add, e="pool")
        A.free("rp_t1", "rp_t2")

    import os
    MSTOP = int(os.environ.get("MSTOP", "99"))

    def mstop(k):
        if MSTOP == k:
            for n in [n for n in A.live if n.startswith("mla_") or n.startswith("wblk")]:
                A.free(n)
            return True
        return False

    def mixer_mla(l, yT, pcol):
        wb = win_blk_c(l, 1024, 1728, "wblk0")
        wkr = A.alloc("mla_wkr", [128, 16, 64], BF16)
        S.ts(wkr[:, :, 0:32], wb[:, 5, :, 32:64], -1.0, None, op0=ALU.mult, e="pool")
        S.copy(wkr[:, :, 32:64], wb[:, 5, :, 0:32], e="pool")
        ckvn = A.alloc("mla_ckvn", [128, 2, 2 * T], BF16)
        krall = A.alloc("mla_kr", [64, 2 * T], BF16)
        cqn = A.alloc("mla_cqn", [128, 3, T], BF16)
        cq = A.alloc("mla_cq", [128, 3, T], F32)
        for j in range(3):
            proj_fm(wb, j * 128, 128, lambda ps, hf, j=j: S.copy(cq[:, j, HS[hf]], ps, e="act"))
        rms_fm(cq, cqn, 3, pcol[:, 0:3], 384.0)
        A.free("mla_cq")
        ckv = A.alloc("mla_ckv", [128, 2, T], F32)
        for j in range(2):
            proj_fm(wb, 384 + j * 128, 128, lambda ps, hf, j=j: S.copy(ckv[:, j, HS[hf]], ps, e="act"))
        rms_fm(ckv, ckvn, 2, pcol[:, 3:5], 256.0, dsl=T)
        A.free("mla_ckv")
        for hf in range(2):
            ps = proj_ps(wb, 640, 64, hf)
            psr = proj_ps(wkr, 0, 64, hf)
            rope_evac(ps, psr, krall[:, T + hf * 512: T + (hf + 1) * 512], hf)
        A.free("wblk0", "mla_wkr")
        if mstop(1):
            return
        S.dma(cin_kv[0:128, :], ckvn[:, 0, T:2 * T])
        S.dma(cin_kv[128:256, :], ckvn[:, 1, T:2 * T])
        S.dma(cin_kv[256:320, :], krall[:, T:2 * T])
        S.cc(cout_kv, cin_kv, PAIRS)
        S.dma(ckvn[:, 0, 0:T], cout_kv[0:128, :])
        S.dma(ckvn[:, 1, 0:T], cout_kv[128:256, :])
        S.dma(krall[:, 0:T], cout_kv[256:320, :])
        if mstop(2):
            return
        wq = A.alloc("mla_wq", [128, 3, 768], BF16)
        wload(wq, W["mla_w_uq"][l].rearrange("(kc p) n -> p kc n", p=128))
        wqr = A.alloc("mla_wqr", [128, 3, 256], BF16)
        wq4 = wq.rearrange("p k (h d) -> p k h d", h=4)
        wqr4 = wqr.rearrange("p k (h d) -> p k h d", h=4)
        for kc in range(3):
            S.ts(wqr4[:, kc, :, 0:32], wq4[:, kc, :, 160:192], -1.0, None, op0=ALU.mult, e="pool")
            S.copy(wqr4[:, kc, :, 32:64], wq4[:, kc, :, 128:160], e="pool")
        qT = A.alloc("mla_qT", [128, 4, T], BF16)
        qrT = A.alloc("mla_qrT", [64, 4, T], BF16)
        for h in range(4):
            proj_fm(wq, h * 192, 128, lambda ps, hf, h=h: S.copy(qT[:, h, HS[hf]], ps, e="act"), kcn=3, rhs=cqn)
            for hf in range(2):
                ps = proj_ps(wq, h * 192 + 128, 64, hf, kcn=3, rhs=cqn)
                psr = proj_ps(wqr, h * 64, 64, hf, kcn=3, rhs=cqn)
                rope_evac(ps, psr, qrT[:, h, HS[hf]], hf)
        A.free("mla_wq", "mla_wqr", "mla_cqn")
        if mstop(3):
            return
        wkv = A.alloc("mla_wkv", [128, 2, 1024], BF16)
        wload(wkv, W["mla_w_ukv"][l].rearrange("(kc p) n -> p kc n", p=128))
        wv = A.alloc("mla_wv", [128, 2, 512], BF16)
        for kc in range(2):
            S.copy(wv[:, kc, :].rearrange("p (h d) -> p h d", h=4),
                   wkv[:, kc, :].rearrange("p (h t d) -> p h t d", h=4, t=2)[:, :, 1, :], e="pool")
        KT = A.alloc("mla_KT", [128, 4, 2 * T], BF16)
        V = A.alloc("mla_V", [128, 16, 512], BF16)
        for h in range(4):
            for blk in range(4):
                ps = PS()
                for kc in range(2):
                    S.mm(ps, wkv[:, kc, h * 256:h * 256 + 128], ckvn[:, kc, blk * 512:(blk + 1) * 512],
                         start=(kc == 0), stop=(kc == 1))
                S.copy(KT[:, h, blk * 512:(blk + 1) * 512], ps, e="act" if blk % 2 else "dve")
        for j in range(16):
            ps = PS()
            for kc in range(2):
                S.mm(ps, ckvn[:, kc, j * 128:(j + 1) * 128], wv[:, kc, :], start=(kc == 0), stop=(kc == 1))
            S.copy(V[:, j, :], ps, e="act" if j % 2 else "dve")
        A.free("mla_wkv", "mla_wv", "mla_ckvn")
        if mstop(4):
            return
        sc = 192.0 ** -0.5
        rd = A.alloc("mla_rd", [128, 512], F32)
        items = [(h, hf, j) for h in range(4) for hf in range(2) for j in range(8 + (hf + 1) * 4)]

        def q0_of(hf, j):
            return 0 if j < 8 else max((j - 8) * 128 - hf * 512, 0)

        def scores(it):
            h, hf, j = it
            q0 = q0_of(hf, j)
            qs = slice(hf * 512 + q0, (hf + 1) * 512)
            ps = PS()
            S.mm(ps[:, q0:512], KT[:, h, j * 128:(j + 1) * 128], qT[:, h, qs], start=True, stop=False)
            S.mm(ps[:, q0:512], krall[:, j * 128:(j + 1) * 128], qrT[:, h, qs], start=False, stop=True)
            return ps
        pend = scores(items[0])
        po = pd = None
        for idx, (h, hf, j) in enumerate(items):
            nkt = 8 + (hf + 1) * 4
            jl = j - 8
            q0 = q0_of(hf, j)
            ps = pend
            if idx + 1 < len(items):
                pend = scores(items[idx + 1])
            if j == 0:
                po, pd = PSX(), PSX()
            PT = A.alloc("mla_PT%d" % (idx % 3), [128, 512], BF16)
            if j < 8:
                S.act(PT[:, q0:512], ps[:, q0:512], AF.Exp, scale=sc, bias=nb)
            else:
                S.act(PT[:, q0:512], ps[:, q0:512], AF.Exp, scale=sc)
                if jl * 128 >= hf * 512:
                    S.tt(PT[:, q0:q0 + 128], PT[:, q0:q0 + 128], tri_b, ALU.mult, e="pool")
            S.mm(po[:, q0:512], V[:, j, h * 128:(h + 1) * 128], PT[:, q0:512], start=(j == 0), stop=(j == nkt - 1))
            S.mm(pd[:, q0:512], ones_b, PT[:, q0:512], start=(j == 0), stop=(j == nkt - 1))
            A.free("mla_PT%d" % (idx % 3))
            if j == nkt - 1:
                S.recip(rd, pd)
                S.tt(yT[:, h, HS[hf]], po, rd, ALU.mult)
        A.free("mla_rd", "mla_KT", "mla_V", "mla_qT", "mla_qrT", "mla_kr")

    GSTOP = int(os.environ.get("GSTOP", "99"))

    def cla(tag, qtT, ktT, khT, Edec, v_tok, Wd, cin, cout, onum):
        nwb = Wd // 128
        if GSTOP <= 3:
            return
        kh_tok = A.alloc(tag + "khtok", [128, 8, 256], BF16)
        for i in range(8):
            psb = PS().bitcast(BF16)
            for fc in range(2):
                S.tr(psb[:, fc * 128:(fc + 1) * 128], khT[:, fc, i * 128:(i + 1) * 128], ident_b)
            S.copy(kh_tok[:, i, :], psb[:, 0:256], e="act" if i % 2 else "dve")
        St = A.alloc(tag + "S", [128, 2, Wd], F32)
        Sb = A.alloc(tag + "Sb", [128, 2, Wd], BF16)
        S.memset(St, 0.0, e="dve")

        def upd(c, fc):
            i, r0 = c // 2, (c % 2) * 64
            ps = PS()
            S.mm(ps[:, 0:2 * Wd], kh_tok[r0:r0 + 64, i, fc * 128:(fc + 1) * 128],
                 v_tok[r0:r0 + 64, i, 2 * fc:2 * fc + 2, :].rearrange("p a b -> p (a b)"))
            for hh in range(2):
                pr = slice(hh * 64, (hh + 1) * 64)
                S.stt(St[pr, fc, :], St[pr, fc, :], Edec[pr, fc, c:c + 1], ps[pr, hh * Wd:(hh + 1) * Wd],
                      ALU.mult, ALU.add)

        if GSTOP <= 4:
            A.free(tag + "khtok", tag + "S", tag + "Sb")
            return
        for c in range(16):
            for fc in range(2):
                upd(c, fc)
        if GSTOP <= 5:
            A.free(tag + "khtok", tag + "S", tag + "Sb")
            return
        for fc in range(2):
            S.dma(cin[fc * 128:(fc + 1) * 128, :], St[:, fc, :])
        S.cc(cout, cin, PAIRS)
        for fc in range(2):
            S.dma(St[:, fc, :], cout[fc * 128:(fc + 1) * 128, :])
        S.ts(St, St, pf, None, op0=ALU.mult)
        S.copy(Sb, St, e="act")
        if GSTOP <= 6:
            A.free(tag + "khtok", tag + "S", tag + "Sb")
            return
        for i in range(8):
            tsl = slice(i * 128, (i + 1) * 128)
            attm = [A.alloc(tag + "attm%d" % fc, [128, 2, 128], BF16) for fc in range(2)]
            for fc in range(2):
                for hh in range(2):
                    pr = slice(hh * 64, (hh + 1) * 64)
                    pa = PS()
                    S.mm(pa[:, 0:128], ktT[pr, fc, tsl], qtT[pr, fc, tsl])
                    S.tt(attm[fc][:, hh, :], pa[:, 0:128], bd_b, ALU.mult)
            po = [[PSX() for hh in range(2)] for fc in range(2)]
            for fc in range(2):
                for hh in range(2):
                    for wb in range(nwb):
                        S.mm(po[fc][hh][:, wb * 128:(wb + 1) * 128], v_tok[:, i, 2 * fc + hh, wb * 128:(wb + 1) * 128],
                             attm[fc][:, hh, :], start=(wb == 0), stop=False)
            for cc in range(2):
                c = 2 * i + cc
                qsl = slice(i * 128 + cc * 64, i * 128 + cc * 64 + 64)
                for fc in range(2):
                    for hh in range(2):
                        pr = slice(hh * 64, (hh + 1) * 64)
                        for wb in range(nwb):
                            S.mm(po[fc][hh][:, wb * 128 + cc * 64: wb * 128 + (cc + 1) * 64],
                                 Sb[pr, fc, wb * 128:(wb + 1) * 128], qtT[pr, fc, qsl], start=False,
                                 stop=(cc == 1 and wb == nwb - 1))
                for fc in range(2):
                    upd(c, fc)
                for fc in range(2):
                    S.copy(Sb[:, fc, :], St[:, fc, :], e="act")
            for fc in range(2):
                for hh in range(2):
                    h = 2 * fc + hh
                    if nwb == 1:
                        S.copy(onum[:, h, tsl], po[fc][hh][:, 0:128], e="dve")
                    else:
                        dd = A.alloc(tag + "dd", [128, 128], F32)
                        S.act(dd, po[fc][hh][:, 128:256], AF.Abs)
                        S.ts(dd, dd, 1.0, None, op0=ALU.max)
                        S.recip(dd, dd)
                        S.tt(onum[:, h, tsl], po[fc][hh][:, 0:128], dd, ALU.mult)
                        A.free(tag + "dd")
            A.free(tag + "attm0", tag + "attm1")
        A.free(tag + "khtok", tag + "S", tag + "Sb")

    def headnorm_gate(onum, gcol, gateT, yT):
        sq = A.alloc("hn_sq", [128, 512], BF16)
        rs = A.alloc("hn_rs", [128, 512], F32)
        for h in range(4):
            for hf in range(2):
                S.act(sq, onum[:, h, HS[hf]], AF.Square)
                ps = PS()
                S.mm(ps, ones_b, sq)
                S.act(rs, ps, AF.Sqrt, bias=epsc, scale=1.0 / 128.0)
                S.recip(rs, rs)
                S.tt(rs, rs, onum[:, h, HS[hf]], ALU.mult)
                S.stt(yT[:, h, HS[hf]], rs, gcol, gateT[:, h, HS[hf]], ALU.mult, ALU.mult)
        A.free("hn_sq", "hn_rs")

    def mixer_gla(l, yT, pcol):
        wb1 = win_blk_c(l, 1728, 2240, "wblk0")
        qk = A.alloc("gla_qk", [128, 4, T], F32)
        for j in range(4):
            proj_fm(wb1, j * 128, 128, lambda ps, hf, j=j: S.copy(qk[:, j, HS[hf]], ps, e="act"))
        A.free("wblk0")
        wb3 = win_blk_c(l, 2752, 3280, "wblk1")
        soT = A.alloc("gla_so", [128, 4, T], BF16)
        for j in range(4):
            proj_fm(wb3, j * 128, 128, lambda ps, hf, j=j: S.act(soT[:, j, HS[hf]], ps, AF.Silu))
        glr = A.alloc("gla_lr", [16, T], BF16)
        proj_fm(wb3, 512, 16, lambda ps, hf: S.copy(glr[:, HS[hf]], ps, e="act"))
        A.free("wblk1")
        wg = A.alloc("gla_wg", [16, 256], BF16)
        wload(wg, W["gla_w_gate"][l])
        nbg = A.alloc("gla_nbg", [128, 2], F32)
        S.ts(nbg, pcol[:, 5:7], -1.0, None, op0=ALU.mult)
        bneg = A.alloc("gla_bneg", [128, 2, T], F32)
        et = A.alloc("gla_et", [128, 2, T], F32)
        for fc in range(2):
            for hf in range(2):
                ps = PS()
                S.mm(ps, wg[:, fc * 128:(fc + 1) * 128], glr[:, HS[hf]])
                S.act(et[:, fc, HS[hf]], ps, AF.Exp, scale=-1.0, bias=nbg[:, fc:fc + 1])
            S.act(et[:, fc, :], et[:, fc, :], AF.Ln, bias=onec)
            S.ts(et[:, fc, :], et[:, fc, :], 1.0 / 16.0, None, op0=ALU.mult)
            S.scan(bneg[:, fc, :], rmask, et[:, fc, :], 0.0, ALU.mult, ALU.add)
        A.free("gla_wg", "gla_nbg", "gla_lr")
        Edec = A.alloc("gla_Edec", [128, 2, 16], F32)
        qtT = A.alloc("gla_qt", [128, 2, T], BF16)
        ktT = A.alloc("gla_kt", [128, 2, T], BF16)
        khT = A.alloc("gla_kh", [128, 2, T], BF16)
        for fc in range(2):
            bl = bneg[:, fc, :].rearrange("p (c t) -> p c t", t=64)[:, :, 63:64]
            S.act(Edec[:, fc, :], bl.rearrange("p c o -> p (c o)"), AF.Exp, scale=-1.0)
            S.act(et[:, fc, :], bneg[:, fc, :], AF.Exp, scale=-1.0)
            S.stt(qtT[:, fc, :], qk[:, fc, :], 0.125, et[:, fc, :], ALU.mult, ALU.mult)
            S.act(et[:, fc, :], bneg[:, fc, :], AF.Exp)
            S.tt(ktT[:, fc, :], qk[:, 2 + fc, :], et[:, fc, :], ALU.mult)
            S.tt(et[:, fc, :].rearrange("p (c t) -> p c t", t=64), bneg[:, fc, :].rearrange("p (c t) -> p c t", t=64),
                 bl.to_broadcast([128, 16, 64]), ALU.subtract)
            S.act(et[:, fc, :], et[:, fc, :], AF.Exp)
            S.tt(khT[:, fc, :], qk[:, 2 + fc, :], et[:, fc, :], ALU.mult)
        A.free("gla_qk", "gla_bneg", "gla_et")
        wb2 = win_blk(l, 2240, 2752, "wblk0")
        v_tok = A.alloc("gla_v", [128, 8, 4, 128], BF16)
        for i in range(8):
            ps = PS()
            for kc in range(16):
                S.mm(ps, hb[:, kc, i * 128:(i + 1) * 128], wb2[:, kc, :], start=(kc == 0), stop=(kc == 15))
            S.copy(v_tok[:, i, :, :].rearrange("p a b -> p (a b)"), ps, e="act" if i % 2 else "dve")
        A.free("wblk0")
        onum = A.alloc("gla_on", [128, 4, T], F32)
        cla("gla_", qtT, ktT, khT, Edec, v_tok, 128, cin_g, cout_g, onum)
        A.free("gla_qt", "gla_kt", "gla_kh", "gla_v", "gla_Edec")
        headnorm_gate(onum, pcol[:, 7:8], soT, yT)
        A.free("gla_on", "gla_so")

    def mixer_ml(l, yT, pcol):
        wbA = win_blk_c(l, 3280, 3792, "wblk0")
        mqk = A.alloc("ml_mqk", [128, 4, T + 4], F32)
        for j in range(4):
            proj_fm(wbA, j * 128, 128, lambda ps, hf, j=j: S.copy(mqk[:, j, 3 + hf * 512: 3 + (hf + 1) * 512], ps, e="act"))
        A.free("wblk0")
        for j in range(4):
            S.dma(cin_tail[:, j * 4:(j + 1) * 4], mqk[:, j, T - 1:T + 3])
        S.cc(cout_tail, cin_tail, PAIRS)
        for j in range(4):
            S.dma(mqk[:, j, 0:3], cout_tail[0:128, j * 4 + 1:j * 4 + 4])
        for j in range(4):
            S.ts(mqk[:, j, 0:3], mqk[:, j, 0:3], pf, None, op0=ALU.mult)
        cv = A.alloc("ml_cv", [128, 4, T], F32)
        for j in range(4):
            S.ts(cv[:, j, :], mqk[:, j, 3:3 + T], pcol[:, 9 + 12 + j: 9 + 12 + j + 1], pcol[:, 25 + j:25 + j + 1],
                 op0=ALU.mult, op1=ALU.add, e="dve" if j % 2 else "pool")
            for k in range(3):
                S.stt(cv[:, j, :], mqk[:, j, k:k + T], pcol[:, 9 + 4 * k + j: 9 + 4 * k + j + 1], cv[:, j, :],
                      ALU.mult, ALU.add)
            S.act(cv[:, j, :], cv[:, j, :], AF.Silu)
        A.free("ml_mqk")
        wbC = win_blk_c(l, 4304, 4824, "wblk1")
        soT = A.alloc("ml_so", [128, 4, T], BF16)
        for j in range(4):
            proj_fm(wbC, j * 128, 128, lambda ps, hf, j=j: S.act(soT[:, j, HS[hf]], ps, AF.Sigmoid))
        gb = A.alloc("ml_gb", [4, 4], F32)
        S.dma(gb[:, 0:2], W["ml_gate_b"][l].rearrange("(two h) -> h two", two=2), allow_slow_non_contiguous=True)
        S.ts(gb[:, 2:3], gb[:, 1:2], -1.0, None, op0=ALU.mult)
        r_b = A.alloc("ml_rb", [4, T], F32)
        r_t = A.alloc("ml_rt", [4, T], F32)
        r_e = A.alloc("ml_re", [4, T], F32)
        for hf in range(2):
            ps = proj_ps(wbC, 512, 4, hf)
            S.act(r_t[:, HS[hf]], ps, AF.Identity, bias=gb[:, 0:1])
            ps = proj_ps(wbC, 516, 4, hf)
            S.act(r_e[:, HS[hf]], ps, AF.Exp, scale=-1.0, bias=gb[:, 2:3])
        A.free("wblk1")
        S.act(r_e, r_e, AF.Ln, bias=onec[0:4, :])
        S.scan(r_b, rmask[0:4, :], r_e, 0.0, ALU.mult, ALU.add)
        S.tt(r_t, r_t, r_b, ALU.add)
        bl = r_b.rearrange("p (c t) -> p c t", t=64)[:, :, 63:64]
        Edec = A.alloc("ml_Edec", [128, 2, 16], F32)
        rd = A.alloc("ml_rd", [4, 16], F32)
        S.act(rd, bl.rearrange("p c o -> p (c o)"), AF.Exp, scale=-1.0)
        for fc in range(2):
            ps = PS()
            S.mm(ps[:, 0:16], sel_f[:, fc, :], rd)
            S.copy(Edec[:, fc, :], ps[:, 0:16])
        qtT = A.alloc("ml_qt", [128, 2, T], BF16)
        ktT = A.alloc("ml_kt", [128, 2, T], BF16)
        khT = A.alloc("ml_kh", [128, 2, T], BF16)

        def bc_mul(row, dst, src, scale=None):
            for fc in range(2):
                for hf in range(2):
                    ps = PS()
                    S.mm(ps, sel_f[:, fc, :], row[:, HS[hf]])
                    if scale is None:
                        S.tt(dst[:, fc, HS[hf]], ps, src[:, fc, HS[hf]], ALU.mult)
                    else:
                        S.stt(dst[:, fc, HS[hf]], src[:, fc, HS[hf]], scale, ps, ALU.mult, ALU.mult)

        S.act(r_e, r_b, AF.Exp, scale=-1.0)
        bc_mul(r_e, qtT, cv[:, 0:2, :], scale=0.125)
        S.act(r_e, r_t, AF.Exp)
        bc_mul(r_e, ktT, cv[:, 2:4, :])
        S.tt(r_e.rearrange("p (c t) -> p c t", t=64), r_t.rearrange("p (c t) -> p c t", t=64),
             bl.to_broadcast([4, 16, 64]), ALU.subtract)
        S.act(r_e, r_e, AF.Exp)
        bc_mul(r_e, khT, cv[:, 2:4, :])
        A.free("ml_cv", "ml_rb", "ml_rt", "ml_re", "ml_rd", "ml_gb")
        wbB = win_blk(l, 3792, 4304, "wblk0")
        v_tok = A.alloc("ml_v", [128, 8, 4, 256], BF16)
        for i in range(8):
            S.memset(v_tok[:, i, :, 128:256], 1.0, e="pool")
            ps = PS()
            for kc in range(16):
                S.mm(ps, hb[:, kc, i * 128:(i + 1) * 128], wbB[:, kc, :], start=(kc == 0), stop=(kc == 15))
            S.copy(v_tok[:, i, :, 0:128], ps.rearrange("p (a b) -> p a b", a=4), e="act" if i % 2 else "dve")
        A.free("wblk0")
        onum = A.alloc("ml_on", [128, 4, T], F32)
        cla("ml_", qtT, ktT, khT, Edec, v_tok, 256, cin_m, cout_m, onum)
        A.free("ml_qt", "ml_kt", "ml_kh", "ml_v", "ml_Edec")
        headnorm_gate(onum, pcol[:, 8:9], soT, yT)
        A.free("ml_on", "ml_so")

    def pipeline(steps):
        n = len(steps)
        hs_ = [None] * n
        if n:
            hs_[0] = steps[0][0](0)
        for s in range(n):
            if s + 1 < n:
                hs_[s + 1] = steps[s + 1][0]((s + 1) % 2)
            steps[s][1](hs_[s])
            A.free(*steps[s][2](s % 2))

    def resid_evac(dt):
        return lambda ps, hf: S.stt(hT[:, dt, HS[hf]], hT[:, dt, HS[hf]], ALPHA, ps, ALU.mult, ALU.add)

    def merge_out(l, y):
        mT = A.alloc("mT", [128, 16, T], BF16)
        macc = A.alloc("macc", [128, 2, T], F32)
        steps = []
        for G in range(8):
            for n in range(4):
                def load(slot, G=G, n=n):
                    gw = A.alloc("gw%d" % slot, [128, 16, 256], BF16)
                    c0 = 4824 + n * 2048 + G * 256
                    wload(gw, W["w_in"][l, :, c0:c0 + 256].rearrange("(kc p) n -> p kc n", p=128))
                    bw = A.alloc("bw%d" % slot, [128, 4, 256], BF16)
                    wload(bw, W["w_branch"][l, n, :, G * 256:(G + 1) * 256].rearrange("(kc p) n -> p kc n", p=128))
                    return gw, bw

                def comp(hd, G=G, n=n):
                    gw, bw = hd
                    for dt in range(2):
                        for hf in range(2):
                            pg = proj_ps(gw, dt * 128, 128, hf)
                            pp = proj_ps(bw, dt * 128, 128, hf, kcn=4, rhs=y[n])
                            sg = A.alloc("mg_sg", [128, 512], F32)
                            S.act(sg, pg, AF.Sigmoid)
                            if n == 0:
                                S.tt(macc[:, dt, HS[hf]], sg, pp, ALU.mult)
                            else:
                                S.tt(sg, sg, pp, ALU.mult)
                                S.tt(macc[:, dt, HS[hf]], macc[:, dt, HS[hf]], sg, ALU.add, e="pool")
                            A.free("mg_sg")
                    if n == 3:
                        S.copy(mT[:, 2 * G:2 * G + 2, :], macc, e="act")
                steps.append((load, comp, lambda slot: ("gw%d" % slot, "bw%d" % slot)))
        pipeline(steps)
        A.free("macc", "yA", "yB", "yC", "yD")
        steps = []
        for blk in range(4):
            def load(slot, blk=blk):
                wo = A.alloc("wo%d" % slot, [128, 16, 512], BF16)
                wload(wo, W["w_out"][l, :, blk * 512:(blk + 1) * 512].rearrange("(kc p) n -> p kc n", p=128))
                return wo

            def comp(wo, blk=blk):
                for j in range(4):
                    proj_fm(wo, j * 128, 128, resid_evac(blk * 4 + j), rhs=mT)
            steps.append((load, comp, lambda slot: ("wo%d" % slot,)))
        pipeline(steps)
        A.free("mT")

    def xattn(l):
        memT = A.alloc("xa_memT", [128, 16, 256], BF16)
        for i in range(2):
            mt = A.alloc("xa_mt", [128, D], F32)
            S.dma(mt, mem_d[i * 128:(i + 1) * 128, :])
            for c4 in range(4):
                ps = PS()
                for j in range(4):
                    S.tr(ps[:, j * 128:(j + 1) * 128], mt[:, (c4 * 4 + j) * 128:(c4 * 4 + j + 1) * 128], ident_f)
                S.copy(memT[:, c4 * 4:(c4 + 1) * 4, i * 128:(i + 1) * 128], ps.rearrange("p (a b) -> p a b", a=4),
                       e="act" if c4 % 2 else "dve")
            A.free("xa_mt")

        def wl(name, src):
            wb = A.alloc(name, [128, 16, 512], BF16)
            wload(wb, src.rearrange("(kc p) n -> p kc n", p=128))
            return wb
        def wlc(name, src):
            wb = A.alloc(name, [128, 4, 16, 128], BF16)
            for ch in range(4):
                wload(wb[:, ch, :, :], src[:, ch * 128:(ch + 1) * 128].rearrange("(kc p) n -> p kc n", p=128))
            return wb
        wq = wlc("xa_wq", W["x_w_q"][l])
        wk = wlc("xa_wk", W["x_w_kv"][l, :, 0:512])
        qT = A.alloc("xa_qT", [128, 4, T], BF16)
        for h in range(4):
            proj_fm(wq, h * 128, 128, lambda ps, hf, h=h: S.copy(qT[:, h, HS[hf]], ps, e="act"))
        A.free("xa_wq")
        wv = wl("xa_wv", W["x_w_kv"][l, :, 512:1024])
        KT = A.alloc("xa_KT", [128, 4, 256], BF16)
        for h in range(4):
            ps = PS()
            for kc in range(16):
                S.mm(ps[:, 0:256], wk[:, h, kc, :], memT[:, kc, :], start=(kc == 0), stop=(kc == 15))
            S.copy(KT[:, h, :], ps[:, 0:256])
        A.free("xa_wk")
        wo = A.alloc("xa_wo", [128, 4, D], BF16)
        wload(wo, W["x_w_o"][l].rearrange("(kc p) n -> p kc n", p=128))
        V = A.alloc("xa_V", [128, 2, 512], BF16)
        for jc in range(2):
            ps = PS()
            for kc in range(16):
                S.mm(ps, memT[:, kc, jc * 128:(jc + 1) * 128], wv[:, kc, :], start=(kc == 0), stop=(kc == 15))
            S.copy(V[:, jc, :], ps, e="act")
        A.free("xa_wv", "xa_memT")
        oT = A.alloc("xa_oT", [128, 4, T], BF16)
        rd = A.alloc("xa_rd", [128, 512], F32)
        sc = 128.0 ** -0.5
        for h in range(4):
            for hf in range(2):
                po, pd = PSX(), PSX()
                for jc in range(2):
                    ps = PS()
                    S.mm(ps, KT[:, h, jc * 128:(jc + 1) * 128], qT[:, h, HS[hf]])
                    PT = A.alloc("xa_PT%d" % jc, [128, 512], BF16)
                    S.act(PT, ps, AF.Exp, scale=sc)
                    S.mm(po, V[:, jc, h * 128:(h + 1) * 128], PT, start=(jc == 0), stop=(jc == 1))
                    S.mm(pd, ones_b, PT, start=(jc == 0), stop=(jc == 1))
                    A.free("xa_PT%d" % jc)
                S.recip(rd, pd)
                S.tt(oT[:, h, HS[hf]], po, rd, ALU.mult)
        A.free("xa_rd", "xa_KT", "xa_V", "xa_qT")
        for dt in range(16):
            proj_fm(wo, dt * 128, 128, resid_evac(dt), kcn=4, rhs=oT)
        A.free("xa_wo", "xa_oT")

    def moe(l):
        wr = A.alloc("mo_wr", [128, 16, 16], F32)
        S.dma(wr, W["w_router"].rearrange("(kc p) e -> p kc e", p=128))
        rb = bcast_load("mo_rb", W["router_bias"], 16)
        aff = A.alloc("mo_aff", [128, 8, 16], F32)
        for i in range(8):
            ps = PS()
            for kc in range(16):
                S.mm(ps[:, 0:16], hT[:, kc, i * 128:(i + 1) * 128], wr[:, kc, :], start=(kc == 0), stop=(kc == 15))
            S.act(aff[:, i, :], ps[:, 0:16], AF.Sigmoid)
        Bt = A.alloc("mo_B", [128, 8, 16], F32)
        B2 = A.alloc("mo_B2", [128, 8, 16], F32)
        eq = A.alloc("mo_eq", [128, 8, 16], F32)
        m1 = A.alloc("mo_m1", [128, 32, 1], F32)
        m2 = A.alloc("mo_m2", [128, 32, 1], F32)
        gm = A.alloc("mo_gm", [128, 8, 1], F32)

        def g4(t):
            return t.rearrange("p a (g e) -> p (a g) e", e=4)

        def red(out, in_, op):
            S.add("dve", lambda g: g.tensor_reduce(out, in_, AX.X, op), [in_], [out])
        S.tt(Bt, aff, rb[:, None, :].to_broadcast([128, 8, 16]), ALU.add)
        red(m1.rearrange("p a o -> p (a o)"), g4(Bt), ALU.max)
        S.tt(g4(eq), g4(Bt), m1.to_broadcast([128, 32, 4]), ALU.is_equal)
        S.stt(B2, eq, -1.0e9, Bt, ALU.mult, ALU.add)
        red(m2.rearrange("p a o -> p (a o)"), g4(B2), ALU.max)
        S.tt(m1, m1, m2, ALU.add)
        red(gm.rearrange("p a o -> p (a o)"), m1.rearrange("p (a g) o -> p a (g o)", g=4), ALU.max)
        S.tt(m1.rearrange("p (a g) o -> p a (g o)", g=4), m1.rearrange("p (a g) o -> p a (g o)", g=4),
             gm.to_broadcast([128, 8, 4]), ALU.is_equal)
        S.tt(g4(eq), g4(Bt), m2.to_broadcast([128, 32, 4]), ALU.is_ge)
        S.tt(g4(eq), g4(eq), m1.to_broadcast([128, 32, 4]), ALU.mult)
        S.tt(eq, eq, aff, ALU.mult)
        red(gm.rearrange("p a o -> p (a o)"), eq, ALU.add)
        S.recip(gm, gm)
        S.tt(eq, eq, gm.to_broadcast([128, 8, 16]), ALU.mult)
        gT = A.alloc("mo_gT", [16, T], F32)
        for i4 in range(2):
            ps = PS()
            for j in range(4):
                S.tr(ps[0:16, j * 128:(j + 1) * 128], eq[:, i4 * 4 + j, :], ident_f)
            S.copy(gT[:, i4 * 512:(i4 + 1) * 512], ps[0:16, :])
        A.free("mo_wr", "mo_rb", "mo_aff", "mo_B", "mo_B2", "mo_eq", "mo_m1", "mo_m2", "mo_gm")
        hid = A.alloc("mo_hid", [128, 4, T], BF16)
        gbc = A.alloc("mo_gbc", [128, T], F32)
        selt = A.alloc("mo_sel", [16, 128], F32)
        steps = []
        for e in range(16):
            for fp in range(2):
                def load(slot, e=e, fp=fp):
                    wg = A.alloc("mo_wg%d" % slot, [128, 16, 256], BF16)
                    wload(wg, W["moe_w_gate"][l, e, :, fp * 256:(fp + 1) * 256].rearrange("(kc p) n -> p kc n", p=128))
                    wu = A.alloc("mo_wu%d" % slot, [128, 16, 256], BF16)
                    wload(wu, W["moe_w_up"][l, e, :, fp * 256:(fp + 1) * 256].rearrange("(kc p) n -> p kc n", p=128))
                    return wg, wu

                def comp(hd, e=e, fp=fp):
                    wg, wu = hd
                    if fp == 0:
                        S.copy(selt, ident_f[0:16, e:e + 1].to_broadcast([16, 128]))
                        for hf in range(2):
                            ps = PS()
                            S.mm(ps, selt, gT[:, HS[hf]])
                            S.copy(gbc[:, HS[hf]], ps, e="act")
                    for j in range(2):
                        for hf in range(2):
                            pg = proj_ps(wg, j * 128, 128, hf)
                            pu = proj_ps(wu, j * 128, 128, hf)
                            sg = A.alloc("mo_sg", [128, 512], F32)
                            S.act(sg, pg, AF.Silu)
                            S.tt(sg, sg, pu, ALU.mult)
                            S.tt(hid[:, fp * 2 + j, HS[hf]], sg, gbc[:, HS[hf]], ALU.mult, e="pool")
                            A.free("mo_sg")
                steps.append((load, comp, lambda slot: ("mo_wg%d" % slot, "mo_wu%d" % slot)))
            for dh in range(2):
                def load(slot, e=e, dh=dh):
                    wd = A.alloc("mo_wd%d" % slot, [128, 4, 1024], BF16)
                    wload(wd, W["moe_w_down"][l, e, :, dh * 1024:(dh + 1) * 1024].rearrange("(kc p) n -> p kc n", p=128))
                    return wd

                def comp(wd, e=e, dh=dh):
                    for j in range(8):
                        dt = dh * 8 + j
                        proj_fm(wd, j * 128, 128,
                                (resid_evac(dt) if e == 0 else
                                 (lambda ps, hf, dt=dt: S.tt(hT[:, dt, HS[hf]], hT[:, dt, HS[hf]], ps, ALU.add))),
                                kcn=4, rhs=hid)
                steps.append((load, comp, lambda slot: ("mo_wd%d" % slot,)))
        pipeline(steps)
        A.free("mo_hid", "mo_gbc", "mo_sel", "mo_gT")

    for l in range(depth):
        pcol = layer_params(l)
        y = [None] * 4
        y[1] = A.alloc("yB", [128, 4, T], BF16)
        mixer_mla(l, y[1], pcol)
        dump("y_b", y[1])
        y[2] = A.alloc("yC", [128, 4, T], BF16)
        mixer_gla(l, y[2], pcol)
        dump("y_c", y[2])
        y[3] = A.alloc("yD", [128, 4, T], BF16)
        mixer_ml(l, y[3], pcol)
        dump("y_d", y[3])
        y[0] = A.alloc("yA", [128, 4, T], BF16)
        mixer_sg(l, y[0])
        dump("y_a", y[0])
        A.free("pcol")
        merge_out(l, y)
        if l == 0:
            dump("pre1", hT)
        ln_fm(1 + 3 * l)
        if l == 0:
            dump("h1", hT)
        xattn(l)
        ln_fm(2 + 3 * l)
        if l == 0:
            dump("h2", hT)
        moe(l)
        ln_fm(3 + 3 * l)

    def write_out():
        for i in range(8):
            ot = A.alloc("ot%d" % (i % 2), [128, D], F32)
            for c4 in range(4):
                ps = PS()
                for j in range(4):
                    c = c4 * 4 + j
                    S.tr(ps[:, j * 128:(j + 1) * 128], hT[:, c, i * 128:(i + 1) * 128], ident_f)
                S.copy(ot[:, c4 * 512:(c4 + 1) * 512], ps, e="act" if c4 % 2 else "dve")
            S.dma(out_d[i * 128:(i + 1) * 128, :], ot, e="sp" if i % 2 == 0 else "act")
            A.free("ot%d" % (i % 2))
    write_out()
    S.finish()
    S.final_wait()
    nc._keep = (st, S)
    nc._depth = depth
    return nc, list(W.keys()), dump_d


_CACHE = {}


def kernel(**inputs):
    depth = DEPTH
    if "prog" not in _CACHE:
        _CACHE["prog"] = build(depth)
    nc, wnames, _ = _CACHE["prog"]
    return run_prog(nc, wnames, inputs)[0]


def run_prog(nc, wnames, inputs, dump_names=()):
    cst = make_consts()
    x = np.ascontiguousarray(inputs["x"], dtype=np.float32)
    mem = np.ascontiguousarray(inputs["mem"], dtype=np.float32)
    pos = np.ascontiguousarray(inputs["positions"], dtype=np.int32)
    in_maps = []
    depth = nc._depth
    wcache = {}
    for k in wnames:
        a = inputs[k]
        if k in LAYERED and depth < 4:
            a = a[:depth]
        wcache[k] = np.ascontiguousarray(a, dtype=np.float32)
    for c in range(8):
        b, hf = divmod(c, 2)
        m = {"x": x[b, hf * T:(hf + 1) * T], "mem": mem[b], "pos": pos[b:b + 1, hf * T:(hf + 1) * T],
             "pflag": np.full((128, 1), float(hf), np.float32), "cst": cst}
        for k in wnames:
            m[k] = wcache[k]
        in_maps.append(m)
    res = run_bass_kernel_spmd(nc, in_maps, core_ids=list(range(8)))
    out = np.empty((4, 2 * T, D), np.float32)
    for c in range(8):
        b, hf = divmod(c, 2)
        out[b, hf * T:(hf + 1) * T] = res.results[c]["out"]
    dumps = {n: [res.results[c]["dbg_" + n] for c in range(8)] for n in dump_names}
    return out, dumps
```
